# Optimizing a Trainium2 kernel written in Bass

```python
import jax, jax.numpy as jnp
from jax import lax
import numpy as np

D_MODEL = 1024
BATCH = 8
SEQ = 2048
DEPTH = 1

CHUNK = 64
D_MIX = D_MODEL
D_LRU = D_MIX // 2
LRU_BLOCKS = 8
LRU_BLOCK_DIM = D_LRU // LRU_BLOCKS
CONV_WIDTH = 4
LRU_C = 8.0
N_SB_HEADS = 8
SB_HEAD_DIM = 64
D_SB = N_SB_HEADS * SB_HEAD_DIM
Q_BLOCK = 128
D_IN_PROJ = 2 * D_LRU + 3 * D_SB
N_EXPERTS = 32
TOP_K = 4
D_EXPERT = D_MODEL
SWIGLU_LIMIT = 7.0
SWIGLU_ALPHA = 1.702
MOE_ROW_BLOCK = 256
D_PLE = 256
RMS_EPS = 1e-6

kernel_name = "hymba_rglru_stickbreak_moe_ple"


def rms_norm(x, g):
    xf = x.astype(jnp.float32)
    y = xf * lax.rsqrt(jnp.mean(xf * xf, axis=-1, keepdims=True) + RMS_EPS)
    return (y * g.astype(jnp.float32)).astype(x.dtype)


def causal_depthwise_conv(x, w, b):
    out = lax.conv_general_dilated(
        x, w[:, None, :], window_strides=(1,), padding=[(CONV_WIDTH - 1, 0)],
        dimension_numbers=("NWC", "WIO", "NWC"), feature_group_count=x.shape[-1])
    return out + b


def rg_lru(x, w_a, b_a, w_x, b_x, lam):
    B, S, C = x.shape
    xf = x.astype(jnp.float32)
    xb = xf.reshape(B, S, LRU_BLOCKS, LRU_BLOCK_DIM)
    r = jax.nn.sigmoid(jnp.einsum("bsni,nij->bsnj", xb, w_a.astype(jnp.float32)).reshape(B, S, C) + b_a)
    i = jax.nn.sigmoid(jnp.einsum("bsni,nij->bsnj", xb, w_x.astype(jnp.float32)).reshape(B, S, C) + b_x)
    log_a = -LRU_C * r * jax.nn.softplus(-lam.astype(jnp.float32))
    a = jnp.exp(log_a)
    b = jnp.sqrt(-jnp.expm1(2.0 * log_a)) * (i * xf)

    def combine(lhs, rhs):
        a1, b1 = lhs
        a2, b2 = rhs
        return a1 * a2, a2 * b1 + b2

    _, h = lax.associative_scan(combine, (a, b), axis=1)
    return h.astype(x.dtype)


def stick_breaking_attention(q, k, v):
    B, S, H, Dh = q.shape
    scale = SB_HEAD_DIM ** -0.5
    qf = q.astype(jnp.float32).transpose(0, 2, 1, 3)
    kf = k.astype(jnp.float32).transpose(0, 2, 1, 3)
    vf = v.astype(jnp.float32).transpose(0, 2, 1, 3)
    outs = []
    for blk in range(S // Q_BLOCK):
        q0 = blk * Q_BLOCK
        kv_len = q0 + Q_BLOCK
        z = jnp.einsum("bhqd,bhkd->bhqk", qf[:, :, q0:kv_len], kf[:, :, :kv_len]) * scale
        q_pos = q0 + jnp.arange(Q_BLOCK)
        k_pos = jnp.arange(kv_len)
        visible = k_pos[None, :] < q_pos[:, None]
        log_keep = jnp.where(visible, jax.nn.log_sigmoid(-z), 0.0)
        between = lax.cumsum(log_keep, axis=3, reverse=True) - log_keep
        weights = jnp.where(visible, jnp.exp(jax.nn.log_sigmoid(z) + between), 0.0)
        outs.append(jnp.einsum("bhqk,bhkd->bhqd", weights, vf[:, :, :kv_len]))
    o = jnp.concatenate(outs, axis=2)
    return o.transpose(0, 2, 1, 3).reshape(B, S, H * Dh).astype(q.dtype)


def moe_ffn(x, w_router, b_router, w_up, b_up, w_down, b_down):
    B, S, D = x.shape
    T = B * S
    xt = x.reshape(T, D)
    logits = (xt @ w_router + b_router).astype(jnp.float32)
    top_vals, top_idx = lax.top_k(logits, TOP_K)
    gates = jax.nn.softmax(top_vals, axis=-1).astype(x.dtype)

    n_assign = T * TOP_K
    expert_flat = top_idx.reshape(n_assign)
    token_flat = jnp.arange(n_assign, dtype=jnp.int32) // TOP_K
    gate_flat = gates.reshape(n_assign)
    order = jnp.argsort(expert_flat)
    sorted_expert = expert_flat[order]
    counts = jnp.zeros((N_EXPERTS,), jnp.int32).at[expert_flat].add(1)
    start = jnp.cumsum(counts) - counts
    padded = (counts + MOE_ROW_BLOCK - 1) // MOE_ROW_BLOCK * MOE_ROW_BLOCK
    padded_end = jnp.cumsum(padded)
    padded_start = padded_end - padded
    dest = padded_start[sorted_expert] + (jnp.arange(n_assign, dtype=jnp.int32) - start[sorted_expert])
    n_blocks = -(-n_assign // MOE_ROW_BLOCK) + N_EXPERTS
    n_rows = n_blocks * MOE_ROW_BLOCK
    row_token = jnp.zeros((n_rows,), jnp.int32).at[dest].set(token_flat[order])
    row_gate = jnp.zeros((n_rows,), x.dtype).at[dest].set(gate_flat[order])
    block_expert = jnp.clip(
        jnp.searchsorted(padded_end, jnp.arange(n_blocks, dtype=jnp.int32) * MOE_ROW_BLOCK, side="right"),
        0, N_EXPERTS - 1)

    def expert_block(args):
        e, tok = args
        xb = xt[tok]
        hdn = xb @ w_up[e] + b_up[e]
        g = jnp.minimum(hdn[:, :D_EXPERT], SWIGLU_LIMIT)
        u = jnp.clip(hdn[:, D_EXPERT:], -SWIGLU_LIMIT, SWIGLU_LIMIT)
        glu = g * jax.nn.sigmoid(SWIGLU_ALPHA * g)
        return ((u + 1.0) * glu) @ w_down[e] + b_down[e]

    y = lax.map(expert_block, (block_expert, row_token.reshape(n_blocks, MOE_ROW_BLOCK)))
    y = y.reshape(n_rows, D) * row_gate[:, None]
    out = jnp.zeros((T, D), x.dtype).at[row_token].add(y)
    return out.reshape(B, S, D)


def setup_inputs(seed: int = 0) -> dict:
    key = jax.random.key(seed)
    ks = jax.random.split(key, 26)

    def nrm(k, shape, scale):
        return jax.random.normal(k, shape, jnp.float32) * scale

    def gain(k, shape):
        return 1.0 + nrm(k, shape, 0.05)

    a0 = jax.random.uniform(ks[10], (DEPTH, D_LRU), jnp.float32, minval=0.9, maxval=0.999)
    return {
        "x": nrm(ks[0], (BATCH, SEQ, D_MODEL), 1.0),
        "p": nrm(ks[1], (DEPTH, BATCH, SEQ, D_PLE), 1.0),
        "mix_norm_g": gain(ks[2], (DEPTH, D_MODEL)),
        "w_in": nrm(ks[3], (DEPTH, D_MODEL, D_IN_PROJ), D_MODEL ** -0.5),
        "conv_w": nrm(ks[4], (DEPTH, CONV_WIDTH, D_LRU), CONV_WIDTH ** -0.5),
        "conv_b": nrm(ks[5], (DEPTH, D_LRU), 0.01),
        "lru_w_a": nrm(ks[6], (DEPTH, LRU_BLOCKS, LRU_BLOCK_DIM, LRU_BLOCK_DIM), LRU_BLOCK_DIM ** -0.5),
        "lru_b_a": nrm(ks[7], (DEPTH, D_LRU), 0.01),
        "lru_w_x": nrm(ks[8], (DEPTH, LRU_BLOCKS, LRU_BLOCK_DIM, LRU_BLOCK_DIM), LRU_BLOCK_DIM ** -0.5),
        "lru_b_x": nrm(ks[9], (DEPTH, D_LRU), 0.01),
        "lru_lambda": jnp.log(a0) - jnp.log1p(-a0),
        "lru_out_g": gain(ks[11], (DEPTH, D_LRU)),
        "sb_out_g": gain(ks[12], (DEPTH, D_SB)),
        "w_out": nrm(ks[13], (DEPTH, D_MIX, D_MODEL), D_MIX ** -0.5),
        "ffn_norm_g": gain(ks[14], (DEPTH, D_MODEL)),
        "w_router": nrm(ks[15], (DEPTH, D_MODEL, N_EXPERTS), D_MODEL ** -0.5),
        "b_router": nrm(ks[16], (DEPTH, N_EXPERTS), 0.01),
        "w_up": nrm(ks[17], (DEPTH, N_EXPERTS, D_MODEL, 2 * D_EXPERT), D_MODEL ** -0.5),
        "b_up": nrm(ks[18], (DEPTH, N_EXPERTS, 2 * D_EXPERT), 0.01),
        "w_down": nrm(ks[19], (DEPTH, N_EXPERTS, D_EXPERT, D_MODEL), D_EXPERT ** -0.5),
        "b_down": nrm(ks[20], (DEPTH, N_EXPERTS, D_MODEL), 0.01),
        "ple_norm_g": gain(ks[21], (DEPTH, D_MODEL)),
        "w_ple_gate": nrm(ks[22], (DEPTH, D_MODEL, D_MODEL), D_MODEL ** -0.5),
        "w_ple": nrm(ks[23], (DEPTH, D_PLE, D_MODEL), D_PLE ** -0.5),
        "final_norm_g": gain(ks[24], (D_MODEL,)),
    }


def reference(x, p, mix_norm_g, w_in, conv_w, conv_b, lru_w_a, lru_b_a, lru_w_x, lru_b_x,
              lru_lambda, lru_out_g, sb_out_g, w_out, ffn_norm_g, w_router, b_router,
              w_up, b_up, w_down, b_down, ple_norm_g, w_ple_gate, w_ple, final_norm_g):
    B, S, _ = x.shape
    h = x
    for i in range(DEPTH):
        xn = rms_norm(h, mix_norm_g[i])
        proj = xn @ w_in[i]
        lru_x, lru_gate, q, k, v = jnp.split(
            proj, [D_LRU, 2 * D_LRU, 2 * D_LRU + D_SB, 2 * D_LRU + 2 * D_SB], axis=-1)
        lru_h = rg_lru(causal_depthwise_conv(lru_x, conv_w[i], conv_b[i]),
                       lru_w_a[i], lru_b_a[i], lru_w_x[i], lru_b_x[i], lru_lambda[i])
        lru_y = lru_h * jax.nn.gelu(lru_gate)
        sb_y = stick_breaking_attention(q.reshape(B, S, N_SB_HEADS, SB_HEAD_DIM),
                                        k.reshape(B, S, N_SB_HEADS, SB_HEAD_DIM),
                                        v.reshape(B, S, N_SB_HEADS, SB_HEAD_DIM))
        mixed = jnp.concatenate([rms_norm(lru_y, lru_out_g[i]), rms_norm(sb_y, sb_out_g[i])], axis=-1)
        h = h + mixed @ w_out[i]
        h = h + moe_ffn(rms_norm(h, ffn_norm_g[i]), w_router[i], b_router[i],
                        w_up[i], b_up[i], w_down[i], b_down[i])
        ple = p[i] @ w_ple[i]
        ple_gate = jax.nn.sigmoid(rms_norm(h, ple_norm_g[i]) @ w_ple_gate[i])
        h = h + ple * ple_gate
    return rms_norm(h, final_norm_g)
```

```python
import contextlib
import numpy as np
import concourse.bass as bass
import concourse.mybir as mybir
from concourse.bass_utils import run_bass_kernel_spmd

F32 = mybir.dt.float32
BF16 = mybir.dt.bfloat16
I32 = mybir.dt.int32
U32 = mybir.dt.uint32
AF = mybir.ActivationFunctionType
ALU = mybir.AluOpType

S = 2048
D = 1024
NT = 16
NE = 32
CAP = 448
NA = 4
LAST = CAP - 384
NSLOT = NE * CAP
EPS = 1e-6
NEG = -30000.0


class Res:
    __slots__ = ("w", "r", "const")

    def __init__(self, const=False):
        self.w = None
        self.r = {}
        self.const = const


def RL(n):
    return [Res() for _ in range(n)]


class KB:
    NDS = 48
    NPRE = 40

    def __init__(self, nc):
        self.nc = nc
        self.es = contextlib.ExitStack()
        self.E = {}
        for nm, h in (("pe", nc.tensor), ("act", nc.scalar), ("dve", nc.vector),
                      ("pool", nc.gpsimd), ("sp", nc.sync)):
            self.E[nm] = {"h": h, "sem": self.es.enter_context(nc.semaphore("c_" + nm)),
                          "n": 0, "seen": {}, "hist": []}
        self.ds = [[self.es.enter_context(nc.semaphore("d%d" % i)), 0] for i in range(self.NDS + self.NPRE)]
        self.dhist = {}
        self.dn = {"sp": 0, "pool": 0, "act": 0}
        self.drange = {"sp": (0, 28), "pool": (28, 44), "act": (44, 48)}
        self.pn = 0
        self.ninst = 0

    def _wait(self, e, ev):
        E = self.E[e]
        kind, key, val = ev
        if kind == "e":
            if key == e and e == "pe":
                return
            sem = self.E[key]["sem"]
            hist = self.E[key]["hist"]
            snap = hist[val - 1] if val - 1 < len(hist) else {}
        else:
            sem = self.ds[key][0]
            snap = self.dhist.get((key, val), {})
        k = (kind, key)
        if E["seen"].get(k, 0) >= val:
            return
        E["h"].wait_ge(sem, val)
        new = dict(E["seen"])
        for kk, vv in snap.items():
            if new.get(kk, 0) < vv:
                new[kk] = vv
        new[k] = val
        E["seen"] = new

    def op(self, e, fn, reads=(), writes=(), dma=False, inc=True, pre=False):
        E = self.E[e]
        evs = []
        for r in reads:
            if r.w is not None:
                evs.append(r.w)
        for w in writes:
            if w.w is not None:
                evs.append(w.w)
            for (kind, key), val in w.r.items():
                evs.append((kind, key, val))
        if dma:
            if pre:
                i = self.NDS + self.pn
                self.pn = (self.pn + 1) % self.NPRE
            else:
                lo, hi = self.drange[e]
                i = lo + self.dn[e]
                self.dn[e] = (self.dn[e] + 1) % (hi - lo)
            if self.ds[i][1] > 0:
                evs.append(("d", i, self.ds[i][1]))
        for ev in evs:
            self._wait(e, ev)
        inst = fn(E["h"])
        self.ninst += 1
        if dma:
            self.ds[i][1] += 16
            inst.then_inc(self.ds[i][0], 16)
            me = ("d", i, self.ds[i][1])
            self.dhist[(i, self.ds[i][1])] = E["seen"]
        elif inc:
            E["n"] += 1
            inst.then_inc(E["sem"], 1)
            me = ("e", e, E["n"])
            E["hist"].append(E["seen"])
        else:
            me = ("e", e, E["n"] + 1)
        for r in reads:
            if not r.const:
                k = (me[0], me[1])
                if r.r.get(k, 0) < me[2]:
                    r.r[k] = me[2]
        for w in writes:
            w.w = me
            w.r = {}
        return inst

    def barrier(self):
        evs = [("e", nm, E["n"]) for nm, E in self.E.items() if E["n"] > 0]
        evs += [("d", i, v) for i, (s, v) in enumerate(self.ds) if v > 0 and i < self.NDS]
        for e in self.E:
            for ev in evs:
                self._wait(e, ev)


def build(stop="FULL"):
    nc = bass.Bass("TRN2", target_bir_lowering=False)

    def din(name, shape, dtype=F32):
        return nc.dram_tensor(name, shape, dtype, kind="ExternalInput").ap()

    x = din("x", [S, D])
    pin = din("p", [S, 256])
    gains = din("gains", [4, D])
    w_in = din("w_in", [D, 2560])
    lruvec = din("lruvec", [9, 512])
    lru_w_a = din("lru_w_a", [8, 64, 64])
    lru_w_x = din("lru_w_x", [8, 64, 64])
    sb_out_g = din("sb_out_g", [1, 512])
    w_out = din("w_out", [D, D])
    w_router = din("w_router", [D, NE])
    b_router = din("b_router", [1, NE])
    w_up = din("w_up", [NE, D, 2 * D])
    b_up = din("b_up", [NE, 2 * D])
    w_down = din("w_down", [NE, D, D])
    b_down = din("b_down", [NE, D])
    w_ple_gate = din("w_ple_gate", [D, D])
    w_ple = din("w_ple", [256, D])
    out = nc.dram_tensor("out", [S, D], F32, kind="ExternalOutput").ap()
    xs_d = nc.dram_tensor("xs_scratch", [NSLOT, D], BF16, kind="Internal").ap()
    ys_d = nc.dram_tensor("ys_scratch", [NSLOT, D], F32, kind="Internal").ap()
    hsp_d = nc.dram_tensor("h_spill", [6 * 128, D], F32, kind="Internal").ap()
    dbg = {}

    kb = KB(nc)
    op = kb.op

    with kb.es:
        top = kb.es

        uniq = [0]

        def sbt(stack, name, shape, dtype):
            uniq[0] += 1
            return stack.enter_context(nc.sbuf_tensor("%s_%d" % (name, uniq[0]), shape, dtype))

        ps = [top.enter_context(nc.psum_tensor("ps%d" % i, [128, 512], F32)) for i in range(8)]
        psr = RL(8)

        cres = Res(const=True)
        dmat = sbt(top, "dmat", [128, 128], I32)
        ident_bf = sbt(top, "ident_bf", [128, 128], BF16)
        ident_f = sbt(top, "ident_f", [128, 128], F32)
        NU = sbt(top, "NU", [128, 128], BF16)
        NL = sbt(top, "NL", [128, 128], BF16)
        maskneg = sbt(top, "maskneg", [128, 128], BF16)
        SU = sbt(top, "SU", [128, 128], BF16)
        ones_bf = sbt(top, "ones_bf", [128, 128], BF16)
        zeros_bf = sbt(top, "zeros_bf", [128, 128], BF16)
        ones_f = sbt(top, "ones_f", [128, 1], F32)
        iota_e = sbt(top, "iota_e", [128, NE], F32)
        iota_ei = sbt(top, "iota_ei", [128, NE], I32)
        gb = [sbt(top, "gb%d" % i, [128, D], F32) for i in range(2)]
        gb_r = RL(2)
        hres = sbt(top, "hres", [128, NT, D], F32)
        h_r = RL(NT)
        xnT = sbt(top, "xnT", [128, 8, S], BF16)
        xnT_r = RL(NT)
        stat = sbt(top, "stat", [128, 8, NT], F32)
        stat_r = Res()

        op("pool", lambda g: g.iota(dmat[:], pattern=[[1, 128]], base=0, channel_multiplier=-1), writes=[cres])
        op("pool", lambda g: g.iota(iota_ei[:], pattern=[[1, NE]], base=0, channel_multiplier=0), writes=[cres])

        def cmat(dst, cmp_op, mul):
            op("dve", lambda v: v.tensor_scalar(out=dst[:], in0=dmat[:], scalar1=0.0, scalar2=mul,
                                                 op0=cmp_op, op1=ALU.mult), reads=[cres], writes=[cres])
        cmat(ident_bf, ALU.is_equal, 1.0)
        cmat(ident_f, ALU.is_equal, 1.0)
        cmat(NU, ALU.is_lt, -1.0)
        cmat(NL, ALU.is_ge, -1.0)
        cmat(maskneg, ALU.is_le, NEG)
        cmat(SU, ALU.is_gt, 1.0)
        op("dve", lambda v: v.memset(ones_bf[:], 1.0), writes=[cres])
        op("dve", lambda v: v.memset(zeros_bf[:], 0.0), writes=[cres])
        op("dve", lambda v: v.memset(ones_f[:], 1.0), writes=[cres])
        op("dve", lambda v: v.tensor_copy(out=iota_e[:], in_=iota_ei[:]), reads=[cres], writes=[cres])
        def load_gain(gidx):
            op("sp", lambda q: q.dma_start(out=gb[gidx % 2][:], in_=gains[gidx:gidx + 1, :].partition_broadcast(128)),
               writes=[gb_r[gidx % 2]], dma=True)

        breg = nc.gpsimd.to_reg(NSLOT - 1)

        def norm_T(src_fn, gidx, tok_out=None, banks=(0, 1), statrow=0, per_tile=None):
            load_gain(gidx)
            with contextlib.ExitStack() as st:
                junk = sbt(st, "nt_junk", [128, D], BF16)
                junk_r = Res()
                xnb = [sbt(st, "nt_xnb%d" % j, [128, D], BF16) for j in range(2)]
                xnb_r = RL(2)
                sq = sbt(st, "nt_sq", [128, NT], F32)
                sr = RL(NT)
                pend = []

                def flush():
                    while pend:
                        pi, pbank = pend.pop(0)
                        pbv = ps[pbank][:].bitcast(BF16)
                        o_ap = xnT[:, :, pi * 128:(pi + 1) * 128]
                        i_ap = pbv.rearrange("p (a b) -> p a b", a=8)
                        if pi % 2 == 0:
                            op("act", lambda a: a.copy(out=o_ap, in_=i_ap), reads=[psr[pbank]], writes=[xnT_r[pi]])
                        else:
                            op("dve", lambda v: v.tensor_copy(out=o_ap, in_=i_ap), reads=[psr[pbank]], writes=[xnT_r[pi]])

                for i in range(NT):
                    src, sres = src_fn(i)
                    ssc = stat[:, statrow, i:i + 1]
                    op("act", lambda a: a.activation(out=junk[:], in_=src, func=AF.Square, accum_out=ssc),
                       reads=sres + [cres], writes=[junk_r, sr[i]])
                    op("act", lambda a: a.activation(out=sq[:, i:i + 1], in_=ssc, func=AF.Sqrt,
                                                     scale=1.0 / D, bias=EPS),
                       reads=[sr[i]], writes=[sr[i]])
                    op("dve", lambda v: v.reciprocal(out=ssc, in_=sq[:, i:i + 1]), reads=[sr[i]], writes=[sr[i]])
                    if tok_out is not None:
                        dst, dres = tok_out(i)
                    else:
                        dst, dres = xnb[i % 2][:], [xnb_r[i % 2]]
                    op("dve", lambda v: v.scalar_tensor_tensor(out=dst, in0=src, scalar=ssc, in1=gb[gidx % 2][:],
                                                               op0=ALU.mult, op1=ALU.mult),
                       reads=sres + [sr[i], gb_r[gidx % 2]], writes=dres)
                    flush()
                    b = banks[i % 2]
                    pb = ps[b][:].bitcast(BF16)
                    for kc in range(8):
                        op("pe", lambda t, kc=kc: t.transpose(out=pb[:, kc * 128:(kc + 1) * 128],
                                                              in_=dst[:, kc * 128:(kc + 1) * 128],
                                                              identity=ident_bf[:]),
                           reads=dres + [cres], writes=[psr[b]], inc=(kc == 7))
                    pend.append((i, b))
                    if per_tile is not None:
                        per_tile(i)
                flush()
                if per_tile is not None:
                    per_tile(NT)
                kb.barrier()

        def halias(a0, a1, inner):
            v = hres[:, a0:a1, :].bitcast(BF16)
            return v.rearrange("p a (b c) -> p (a b) c", c=inner)

        mix = contextlib.ExitStack()
        top.enter_context(mix)
        w_in_v = w_in.rearrange("(kc p) n -> p kc n", p=128)
        wq2 = sbt(mix, "wq2", [128, 8, 512], BF16)
        wl = sbt(mix, "wl", [128, 8, 1024], BF16)
        wq = [halias(12, 14, 512), halias(14, 16, 512), wq2[:]]
        wq_r = RL(3)
        wl_r = Res()
        for j, c0 in enumerate((1024, 1536, 2048)):
            for kc in range(8):
                op("pool", lambda g, j=j, kc=kc, c0=c0: g.dma_start(out=wq[j][:, kc, :], in_=w_in_v[:, kc, c0:c0 + 512]),
                   writes=[wq_r[j]], dma=True, pre=True)
        for kc in range(8):
            op("pool", lambda g, kc=kc: g.dma_start(out=wl[:, kc, :], in_=w_in_v[:, kc, 0:1024]), writes=[wl_r], dma=True, pre=True)

        with contextlib.ExitStack() as st:
            xt = [sbt(st, "xt%d" % j, [128, D], F32) for j in range(3)]
            xt_r = RL(3)

            def src1(i):
                j = i % 3
                op("sp", lambda q: q.dma_start(out=xt[j][:], in_=x[i * 128:(i + 1) * 128, :]),
                   writes=[xt_r[j]], dma=True)
                return xt[j][:], [xt_r[j]]
            norm_T(src1, 0)

        if stop == "P1":
            dbg["xnT"] = nc.dram_tensor("dbg_xnT", [128, 8 * S], BF16, kind="ExternalOutput").ap()
            op("sp", lambda q: q.dma_start(out=dbg["xnT"], in_=xnT[:].rearrange("p a b -> p (a b)")),
               reads=xnT_r, dma=True)
            kb.barrier()
            return nc, dbg

        sbyT = sbt(mix, "sbyT", [128, 4, S], BF16)
        sby_r = Res()
        gsb = sbt(mix, "gsb", [128, 4], F32)
        gstg = sbt(mix, "gstg", [1, 512], F32)
        gsb_r = Res()
        op("sp", lambda q: q.dma_start(out=gstg[:], in_=sb_out_g), writes=[gsb_r], dma=True)
        for c in range(4):
            op("pe", lambda t, c=c: t.transpose(out=ps[0][:, c:c + 1], in_=gstg[0:1, c * 128:(c + 1) * 128],
                                                identity=ident_f[0:1, 0:1]),
               reads=[gsb_r, cres], writes=[psr[0]])
        op("dve", lambda v: v.tensor_copy(out=gsb[:], in_=ps[0][:, 0:4]), reads=[psr[0]], writes=[gsb_r])
        kb.barrier()

        lyT = sbt(mix, "lyT", [128, 4, S], BF16)
        ly_r = Res()
        with contextlib.ExitStack() as st:
            qT = halias(0, 4, S)
            kT = halias(4, 8, S)
            vtok = halias(8, 12, 512)
            qkv_r = Res()
            bi = 0
            for j, dstT in ((0, qT), (1, kT)):
                for ch in range(4):
                    for tb in range(4):
                        b = bi % 4
                        bi += 1
                        for kc in range(8):
                            op("pe", lambda t, kc=kc: t.matmul(ps[b][:], lhsT=wq[j][:, kc, ch * 128:(ch + 1) * 128],
                                                               rhs=xnT[:, kc, tb * 512:(tb + 1) * 512],
                                                               start=(kc == 0), stop=(kc == 7)),
                               reads=[wq_r[j]] + xnT_r[tb * 4:tb * 4 + 4], writes=[psr[b]], inc=(kc == 7))
                        sc = 0.125 if j == 0 else 1.0
                        if bi % 2 == 0:
                            op("act", lambda a: a.activation(out=dstT[:, ch, tb * 512:(tb + 1) * 512], in_=ps[b][:],
                                                             func=AF.Copy, scale=sc),
                               reads=[psr[b]], writes=[qkv_r])
                        else:
                            op("dve", lambda v: v.tensor_scalar(out=dstT[:, ch, tb * 512:(tb + 1) * 512], in0=ps[b][:],
                                                                scalar1=sc, scalar2=None, op0=ALU.mult),
                               reads=[psr[b]], writes=[qkv_r])
            for i in range(NT):
                b = bi % 4
                bi += 1
                for kc in range(8):
                    op("pe", lambda t, kc=kc: t.matmul(ps[b][:], lhsT=xnT[:, kc, i * 128:(i + 1) * 128],
                                                       rhs=wq[2][:, kc, :], start=(kc == 0), stop=(kc == 7)),
                       reads=[wq_r[2], xnT_r[i]], writes=[psr[b]], inc=(kc == 7))
                if i % 2 == 0:
                    op("act", lambda a: a.copy(out=vtok[:, i, :], in_=ps[b][:]), reads=[psr[b]], writes=[qkv_r])
                else:
                    op("dve", lambda v: v.tensor_copy(out=vtok[:, i, :], in_=ps[b][:]), reads=[psr[b]], writes=[qkv_r])
            kb.barrier()

            NSTR = 2
            wk = []
            for s_ in range(NSTR):
                per = []
                for par in range(2):
                    per.append({
                        "sp": sbt(st, "at_sp%d%d" % (s_, par), [128, 512], F32),
                        "spb": sbt(st, "at_spb%d%d" % (s_, par), [128, 512], BF16),
                        "d": sbt(st, "at_d%d%d" % (s_, par), [128, 512], F32),
                        "w": sbt(st, "at_w%d%d" % (s_, par), [128, 512], BF16),
                        "r": {k: Res() for k in ("sp", "spb", "d", "w")},
                    })
                wk.append(per)
            osq = sbt(st, "at_osq", [128, 512], F32)
            osq_r = Res()
            sqacc = sbt(st, "at_sqacc", [128, S], F32)
            sqacc_r = Res()
            steps = []
            for hp in range(4):
                for j in range(4):
                    cs = list(range(4 * j + 3, -1, -1))
                    for idx, c in enumerate(cs):
                        steps.append({"hp": hp, "j": j, "c": c, "first": idx == 0, "last": idx == len(cs) - 1,
                                      "par": len(steps) % 2})

            def geo(st_, s_):
                h = 2 * st_["hp"] + s_
                prt = slice((h % 2) * 64, (h % 2) * 64 + 64)
                m = st_["c"] - 4 * st_["j"]
                c0 = 128 * m if m > 0 else 0
                return h, prt, h // 2, m, c0, slice(c0, 512), st_["j"] * 512

            def stage1(s_, st_):
                h, prt, ch, m, c0, cols, t0 = geo(st_, s_)
                c, j = st_["c"], st_["j"]
                bz = 4 * s_ + st_["par"]
                diag = m >= 0
                op("pe", lambda t: t.matmul(ps[bz][:, cols], lhsT=kT[prt, ch, c * 128:(c + 1) * 128],
                                            rhs=qT[prt, ch, t0 + c0:t0 + 512], start=True, stop=not diag),
                   reads=[qkv_r], writes=[psr[bz]], inc=not diag)
                if diag:
                    op("pe", lambda t: t.matmul(ps[bz][:, c0:c0 + 128], lhsT=ident_bf[:], rhs=maskneg[:],
                                                start=False, stop=True),
                       reads=[cres], writes=[psr[bz]])

            def stage1b(s_, st_):
                h, prt, ch, m, c0, cols, t0 = geo(st_, s_)
                W = wk[s_][st_["par"]]
                R = W["r"]
                bz = 4 * s_ + st_["par"]
                op("act", lambda a: a.activation(out=W["sp"][:, cols], in_=ps[bz][:, cols], func=AF.Exp),
                   reads=[psr[bz]], writes=[R["sp"]])
                op("act", lambda a: a.activation(out=W["sp"][:, cols], in_=W["sp"][:, cols], func=AF.Ln, bias=1.0),
                   reads=[R["sp"]], writes=[R["sp"]])
                op("dve", lambda g: g.tensor_copy(out=W["spb"][:, cols], in_=W["sp"][:, cols]),
                   reads=[R["sp"]], writes=[R["spb"]])
                op("dve", lambda v: v.tensor_tensor(out=W["d"][:, cols], in0=ps[bz][:, cols], in1=W["sp"][:, cols],
                                                    op=ALU.subtract),
                   reads=[psr[bz], R["sp"]], writes=[R["d"]])

            def stage2a(s_, st_):
                h, prt, ch, m, c0, cols, t0 = geo(st_, s_)
                W = wk[s_][st_["par"]]
                R = W["r"]
                bx = 4 * s_ + 2
                if st_["first"]:
                    op("pe", lambda t: t.matmul(ps[bx][:, :], lhsT=zeros_bf[:, :], rhs=qT[:, 0, t0:t0 + 512],
                                                start=True, stop=False, skip_group_check=True),
                       reads=[cres, qkv_r], writes=[psr[bx]])
                op("pe", lambda t: t.matmul(ps[bx][:, cols], lhsT=NU[:], rhs=W["spb"][:, cols],
                                            start=False, stop=False, skip_group_check=True),
                   reads=[cres, R["spb"]], writes=[psr[bx]])
                op("dve", lambda v: v.tensor_tensor(out=W["d"][:, cols], in0=ps[bx][:, cols], in1=W["d"][:, cols],
                                                    op=ALU.add),
                   reads=[psr[bx], R["d"]], writes=[R["d"]])

            def stage2w(s_, st_):
                h, prt, ch, m, c0, cols, t0 = geo(st_, s_)
                W = wk[s_][st_["par"]]
                R = W["r"]
                op("act", lambda a: a.activation(out=W["w"][:, cols], in_=W["d"][:, cols], func=AF.Exp),
                   reads=[R["d"]], writes=[R["w"]])

            def stage2b(s_, st_):
                h, prt, ch, m, c0, cols, t0 = geo(st_, s_)
                W = wk[s_][st_["par"]]
                R = W["r"]
                bx = 4 * s_ + 2
                if st_["c"] > 0:
                    op("pe", lambda t: t.matmul(ps[bx][:, cols], lhsT=NL[:], rhs=W["spb"][:, cols],
                                                start=False, stop=False, skip_group_check=True),
                       reads=[cres, R["spb"]], writes=[psr[bx]])

            def stage2c(s_, st_):
                h, prt, ch, m, c0, cols, t0 = geo(st_, s_)
                W = wk[s_][st_["par"]]
                R = W["r"]
                bo = 4 * s_ + 3
                c, hp = st_["c"], st_["hp"]
                if st_["first"]:
                    op("pe", lambda t: t.matmul(ps[bo][prt, :], lhsT=zeros_bf[:, 0:64], rhs=qT[:, 0, t0:t0 + 512],
                                                start=True, stop=False, skip_group_check=True),
                       reads=[cres, qkv_r], writes=[psr[bo]])
                op("pe", lambda t: t.matmul(ps[bo][prt, cols], lhsT=vtok[:, c, h * 64:(h + 1) * 64],
                                            rhs=W["w"][:, cols], start=False, stop=(c == 0),
                                            skip_group_check=True),
                   reads=[qkv_r, R["w"]], writes=[psr[bo]])
                if st_["last"]:
                    op("act", lambda a: a.activation(out=sbyT[prt, hp, t0:t0 + 512], in_=ps[bo][prt, :], func=AF.Copy,
                                                     scale=gsb[prt, hp:hp + 1]),
                       reads=[psr[bo], gsb_r], writes=[sby_r])
                    if hp == 0:
                        op("act", lambda a: a.activation(out=sqacc[prt, t0:t0 + 512], in_=ps[bo][prt, :], func=AF.Square),
                           reads=[psr[bo]], writes=[sqacc_r])
                    else:
                        op("act", lambda a: a.activation(out=osq[prt, :], in_=ps[bo][prt, :], func=AF.Square),
                           reads=[psr[bo]], writes=[osq_r])
                        op("pool", lambda g: g.tensor_tensor(out=sqacc[prt, t0:t0 + 512], in0=sqacc[prt, t0:t0 + 512],
                                                             in1=osq[prt, :], op=ALU.add),
                           reads=[osq_r, sqacc_r], writes=[sqacc_r])

            nst = len(steps)
            for s_ in range(2):
                stage1(s_, steps[0])
            for r in range(nst + 1):
                if r > 0:
                    for s_ in range(2):
                        stage2a(s_, steps[r - 1])
                if r + 1 < nst:
                    for s_ in range(2):
                        stage1(s_, steps[r + 1])
                if r < nst:
                    for s_ in range(2):
                        stage1b(s_, steps[r])
                if r > 0:
                    for s_ in range(2):
                        stage2w(s_, steps[r - 1])
                    for s_ in range(2):
                        stage2b(s_, steps[r - 1])
                    for s_ in range(2):
                        stage2c(s_, steps[r - 1])
            kb.barrier()
            for i in range(NT):
                op("pe", lambda t, i=i: t.matmul(ps[0][:, i:i + 1], lhsT=sqacc[:, i * 128:(i + 1) * 128], rhs=ones_f[:, :],
                                                 start=True, stop=True),
                   reads=[sqacc_r, cres], writes=[psr[0]])
            op("dve", lambda v: v.tensor_copy(out=stat[:, 1, :], in_=ps[0][:, 0:NT]), reads=[psr[0]], writes=[stat_r])
            kb.barrier()

        if stop == "ATT":
            dbg["sbyT"] = nc.dram_tensor("dbg_sbyT", [128, 4 * S], BF16, kind="ExternalOutput").ap()
            op("sp", lambda q: q.dma_start(out=dbg["sbyT"], in_=sbyT[:].rearrange("p a b -> p (a b)")),
               reads=[sby_r], dma=True)
            dbg["stat"] = nc.dram_tensor("dbg_stat", [128, 8 * NT], F32, kind="ExternalOutput").ap()
            op("sp", lambda q: q.dma_start(out=dbg["stat"], in_=stat[:].rearrange("p a b -> p (a b)")),
               reads=[stat_r], dma=True)
            kb.barrier()
            return nc, dbg

        mix2 = contextlib.ExitStack()
        top.enter_context(mix2)
        wo = sbt(mix2, "wo", [128, 8, D], BF16)
        wo_r = Res()
        w_out_v = w_out.rearrange("(c p) n -> p c n", p=128)
        for c in range(8):
            op("pool", lambda g, c=c: g.dma_start(out=wo[:, c, :], in_=w_out_v[:, c, :]), writes=[wo_r], dma=True, pre=True)
        with contextlib.ExitStack() as st:
            lstg = sbt(st, "lstg", [9, 512], F32)
            lv = sbt(st, "lv", [128, 4, 9], F32)
            lsc = sbt(st, "lsc", [128, 4, 2], F32)
            ltmp = sbt(st, "ltmp", [128, 4], F32)
            lv_r = Res()
            op("sp", lambda q: q.dma_start(out=lstg[:], in_=lruvec), writes=[lv_r], dma=True)
            for cc in range(4):
                op("pe", lambda t, cc=cc: t.transpose(out=ps[0][:, cc * 16:cc * 16 + 9], in_=lstg[0:9, cc * 128:(cc + 1) * 128],
                                                      identity=ident_f[0:9, 0:9]),
                   reads=[lv_r, cres], writes=[psr[0]])
            op("dve", lambda v: v.tensor_copy(out=lv[:], in_=ps[0][:, 0:64].rearrange("p (a b) -> p a b", a=4)[:, :, 0:9]),
               reads=[psr[0]], writes=[lv_r])
            op("act", lambda a: a.activation(out=ltmp[:], in_=lv[:, :, 7], func=AF.Exp, scale=-1.0), reads=[lv_r], writes=[lv_r])
            op("act", lambda a: a.activation(out=ltmp[:], in_=ltmp[:], func=AF.Ln, bias=1.0), reads=[lv_r], writes=[lv_r])
            op("dve", lambda v: v.tensor_scalar(out=lsc[:, :, 0], in0=ltmp[:], scalar1=-8.0, scalar2=None, op0=ALU.mult),
               reads=[lv_r], writes=[lv_r])
            op("dve", lambda v: v.tensor_scalar(out=lsc[:, :, 1], in0=ltmp[:], scalar1=-16.0, scalar2=None, op0=ALU.mult),
               reads=[lv_r], writes=[lv_r])
            wabd = sbt(st, "wabd", [128, 4, 128], F32)
            wxbd = sbt(st, "wxbd", [128, 4, 128], F32)
            wbd_r = Res()
            op("pool", lambda g: g.memset(wabd[:], 0.0), writes=[wbd_r])
            op("pool", lambda g: g.memset(wxbd[:], 0.0), writes=[wbd_r])
            for n in range(8):
                pr = slice((n % 2) * 64, (n % 2) * 64 + 64)
                op("sp", lambda q, n=n, pr=pr: q.dma_start(out=wabd[pr, n // 2, pr], in_=lru_w_a[n]), writes=[wbd_r], dma=True)
                op("sp", lambda q, n=n, pr=pr: q.dma_start(out=wxbd[pr, n // 2, pr], in_=lru_w_x[n]), writes=[wbd_r], dma=True)

            lxp = sbt(st, "lxp", [128, S + 3], F32)
            def hf32(a0):
                return hres[:, a0:a0 + 2, :].rearrange("p a b -> p (a b)")
            lg, cx, b1, b2, b3, b4 = (hf32(a0) for a0 in (4, 6, 8, 10, 12, 14))
            gbuf = hf32(0)
            gbuf_r = Res()
            ysq = sbt(st, "ysq", [128, S], F32)
            lxp_r, lg_r, cx_r, b1_r, b2_r, b3_r, b4_r, ysq_r = RL(8)
            op("pool", lambda g: g.memset(lxp[:, 0:3], 0.0), writes=[lxp_r])
            bi = 0
            for cc in range(4):
                for which, col0 in ((0, cc * 128), (1, 512 + cc * 128)):
                    for tb in range(4):
                        b = bi % 4
                        bi += 1
                        for kc in range(8):
                            op("pe", lambda t, kc=kc: t.matmul(ps[b][:], lhsT=wl[:, kc, col0:col0 + 128],
                                                               rhs=xnT[:, kc, tb * 512:(tb + 1) * 512],
                                                               start=(kc == 0), stop=(kc == 7)),
                               reads=[wl_r] + xnT_r[tb * 4:tb * 4 + 4], writes=[psr[b]], inc=(kc == 7))
                        if which == 0:
                            op("act", lambda a: a.copy(out=lxp[:, 3 + tb * 512:3 + (tb + 1) * 512], in_=ps[b][:]),
                               reads=[psr[b]], writes=[lxp_r])
                        else:
                            op("act", lambda a: a.copy(out=lg[:, tb * 512:(tb + 1) * 512], in_=ps[b][:]),
                               reads=[psr[b]], writes=[lg_r])
                op("act", lambda a: a.activation(out=gbuf[:], in_=lg[:], func=AF.Square), reads=[lg_r], writes=[gbuf_r])
                op("dve", lambda v: v.tensor_scalar(out=cx[:], in0=lxp[:, 3:3 + S], scalar1=lv[:, cc, 3:4], scalar2=lv[:, cc, 4:5],
                                                    op0=ALU.mult, op1=ALU.add),
                   reads=[lxp_r, lv_r], writes=[cx_r])
                for jt in range(3):
                    op("dve", lambda v, jt=jt: v.scalar_tensor_tensor(out=cx[:], in0=lxp[:, jt:jt + S], scalar=lv[:, cc, jt:jt + 1],
                                                                      in1=cx[:], op0=ALU.mult, op1=ALU.add),
                       reads=[lxp_r, lv_r, cx_r], writes=[cx_r])
                op("dve", lambda g: g.tensor_scalar(out=gbuf[:], in0=gbuf[:], scalar1=0.044715, scalar2=1.0, op0=ALU.mult, op1=ALU.add),
                   reads=[gbuf_r], writes=[gbuf_r])
                op("dve", lambda g: g.tensor_tensor(out=gbuf[:], in0=gbuf[:], in1=lg[:], op=ALU.mult), reads=[gbuf_r, lg_r], writes=[gbuf_r])
                for wmat, bcol, dst, dres in ((wabd, 5, b1, b1_r), (wxbd, 6, b4, b4_r)):
                    for tb in range(4):
                        b = 4 + (bi % 4)
                        bi += 1
                        op("pe", lambda t: t.matmul(ps[b][:], lhsT=wmat[:, cc, :], rhs=cx[:, tb * 512:(tb + 1) * 512],
                                                    start=True, stop=True),
                           reads=[wbd_r, cx_r], writes=[psr[b]])
                        op("act", lambda a: a.activation(out=dst[:, tb * 512:(tb + 1) * 512], in_=ps[b][:], func=AF.Sigmoid,
                                                         bias=lv[:, cc, bcol:bcol + 1]),
                           reads=[psr[b], lv_r], writes=[dres])
                op("act", lambda a: a.activation(out=gbuf[:], in_=gbuf[:], func=AF.Sigmoid, scale=1.5957691216057308),
                   reads=[gbuf_r], writes=[gbuf_r])
                op("dve", lambda g: g.tensor_tensor(out=gbuf[:], in0=gbuf[:], in1=lg[:], op=ALU.mult), reads=[gbuf_r, lg_r], writes=[gbuf_r])
                op("act", lambda a: a.activation(out=b2[:], in_=b1[:], func=AF.Exp, scale=lsc[:, cc, 0:1]),
                   reads=[b1_r, lv_r], writes=[b2_r])
                op("act", lambda a: a.activation(out=b3[:], in_=b1[:], func=AF.Exp, scale=lsc[:, cc, 1:2]),
                   reads=[b1_r, lv_r], writes=[b3_r])
                op("act", lambda a: a.activation(out=b3[:], in_=b3[:], func=AF.Sqrt, scale=-1.0, bias=1.0),
                   reads=[b3_r], writes=[b3_r])
                op("dve", lambda g: g.tensor_tensor(out=b4[:], in0=b4[:], in1=cx[:], op=ALU.mult), reads=[b4_r, cx_r], writes=[b4_r])
                op("dve", lambda g: g.tensor_tensor(out=b4[:], in0=b4[:], in1=b3[:], op=ALU.mult), reads=[b4_r, b3_r], writes=[b4_r])
                op("dve", lambda v: v.tensor_tensor_scan(out=b1[:], data0=b2[:], data1=b4[:], initial=0.0,
                                                         op0=ALU.mult, op1=ALU.add),
                   reads=[b2_r, b4_r], writes=[b1_r])
                op("dve", lambda v: v.tensor_tensor(out=b1[:], in0=b1[:], in1=gbuf[:], op=ALU.mult), reads=[b1_r, gbuf_r], writes=[b1_r])
                if cc == 0:
                    op("act", lambda a: a.activation(out=ysq[:], in_=b1[:], func=AF.Square), reads=[b1_r], writes=[ysq_r])
                else:
                    op("act", lambda a: a.activation(out=b2[:], in_=b1[:], func=AF.Square), reads=[b1_r], writes=[b2_r])
                    op("dve", lambda g: g.tensor_tensor(out=ysq[:], in0=ysq[:], in1=b2[:], op=ALU.add), reads=[ysq_r, b2_r], writes=[ysq_r])
                op("dve", lambda v: v.tensor_scalar(out=lyT[:, cc, :], in0=b1[:], scalar1=lv[:, cc, 8:9], scalar2=None, op0=ALU.mult),
                   reads=[b1_r, lv_r], writes=[ly_r])
            kb.barrier()
            for i in range(NT):
                op("pe", lambda t, i=i: t.matmul(ps[0][:, i:i + 1], lhsT=ysq[:, i * 128:(i + 1) * 128], rhs=ones_f[:, :],
                                                 start=True, stop=True),
                   reads=[ysq_r, cres], writes=[psr[0]])
            op("dve", lambda v: v.tensor_copy(out=stat[:, 2, :], in_=ps[0][:, 0:NT]), reads=[psr[0]], writes=[stat_r])
            op("act", lambda a: a.activation(out=stat[:, 1:3, :], in_=stat[:, 1:3, :], func=AF.Sqrt, scale=1.0 / 512, bias=EPS),
               reads=[stat_r], writes=[stat_r])
            op("dve", lambda v: v.reciprocal(out=stat[:, 1:3, :], in_=stat[:, 1:3, :]), reads=[stat_r], writes=[stat_r])
            kb.barrier()

        if stop == "LRU":
            dbg["lyT"] = nc.dram_tensor("dbg_lyT", [128, 4 * S], BF16, kind="ExternalOutput").ap()
            op("sp", lambda q: q.dma_start(out=dbg["lyT"], in_=lyT[:].rearrange("p a b -> p (a b)")), reads=[ly_r], dma=True)
            dbg["stat"] = nc.dram_tensor("dbg_stat", [128, 8 * NT], F32, kind="ExternalOutput").ap()
            op("sp", lambda q: q.dma_start(out=dbg["stat"], in_=stat[:].rearrange("p a b -> p (a b)")), reads=[stat_r], dma=True)
            kb.barrier()
            return nc, dbg

        with contextlib.ExitStack() as st:
            xt = [sbt(st, "xo%d" % j, [128, D], F32) for j in range(2)]
            xt_r = RL(2)
            tmp = [sbt(st, "otmp%d" % j, [128, 512], F32) for j in range(2)]
            tmp_r = RL(2)
            for i in range(NT):
                j = i % 2
                op("sp", lambda q: q.dma_start(out=xt[j][:], in_=x[i * 128:(i + 1) * 128, :]), writes=[xt_r[j]], dma=True)
                for half in range(2):
                    n0 = half * 512
                    ba = 2 * ((2 * i + half) % 2)
                    bb = ba + 1
                    for c in range(4):
                        op("pe", lambda t, c=c: t.matmul(ps[ba][:], lhsT=lyT[:, c, i * 128:(i + 1) * 128], rhs=wo[:, c, n0:n0 + 512],
                                                         start=(c == 0), stop=(c == 3)),
                           reads=[ly_r, wo_r], writes=[psr[ba]], inc=(c == 3))
                    for c in range(4):
                        op("pe", lambda t, c=c: t.matmul(ps[bb][:], lhsT=sbyT[:, c, i * 128:(i + 1) * 128], rhs=wo[:, 4 + c, n0:n0 + 512],
                                                         start=(c == 0), stop=(c == 3)),
                           reads=[sby_r, wo_r], writes=[psr[bb]], inc=(c == 3))
                    op("dve", lambda v: v.scalar_tensor_tensor(out=tmp[half][:], in0=ps[ba][:], scalar=stat[:, 2, i:i + 1],
                                                               in1=xt[j][:, n0:n0 + 512], op0=ALU.mult, op1=ALU.add),
                       reads=[psr[ba], stat_r, xt_r[j]], writes=[tmp_r[half]])
                    op("dve", lambda v: v.scalar_tensor_tensor(out=hres[:, i, n0:n0 + 512], in0=ps[bb][:], scalar=stat[:, 1, i:i + 1],
                                                               in1=tmp[half][:], op0=ALU.mult, op1=ALU.add),
                       reads=[psr[bb], stat_r, tmp_r[half]], writes=[h_r[i]])
            kb.barrier()
        mix2.close()
        mix.close()

        def dump_h(name):
            dbg[name] = nc.dram_tensor("dbg_" + name, [S, D], F32, kind="ExternalOutput").ap()
            for i in range(NT):
                op("sp", lambda q, i=i: q.dma_start(out=dbg[name][i * 128:(i + 1) * 128, :], in_=hres[:, i, :]),
                   reads=[h_r[i]], dma=True)
            kb.barrier()

        if stop == "OUT":
            dump_h("h1")
            return nc, dbg

        moe = contextlib.ExitStack()
        top.enter_context(moe)
        G = sbt(moe, "G", [128, NT, NE], F32)
        dest = sbt(moe, "dest", [128, NT, 4], I32)
        gk = sbt(moe, "gk", [128, NT, 4], F32)
        bupT = sbt(moe, "bupT", [128, 16, NE], F32)
        rt_r = Res()
        G_r, dest_r, gk_r = RL(NT), RL(NT), RL(NT)
        with contextlib.ExitStack() as st:
            GT = [sbt(st, "GT%d" % j, [NE, 128], F32) for j in range(2)]
            GT_r = RL(2)
            bdn = sbt(st, "bdn", [NE, D], F32)
            xn2all = sbt(st, "xn2all", [128, NT, D], BF16)
            xn2all_r = RL(NT)
            wr = sbt(st, "wr", [128, 8, NE], BF16)
            brb = sbt(st, "brb", [128, NE], F32)
            bstg = sbt(st, "bstg", [NE, 2 * D], F32)
            op("pool", lambda g: g.dma_start(out=wr[:], in_=w_router.rearrange("(kc p) n -> p kc n", p=128)), writes=[rt_r], dma=True)
            op("sp", lambda q: q.dma_start(out=brb[:], in_=b_router.partition_broadcast(128)), writes=[rt_r], dma=True)
            op("sp", lambda q: q.dma_start(out=bstg[:], in_=b_up), writes=[rt_r], dma=True)
            op("sp", lambda q: q.dma_start(out=bdn[:], in_=b_down), writes=[rt_r], dma=True)
            for c in range(16):
                op("pe", lambda t, c=c: t.transpose(out=ps[4 + c // 8][:, (c % 8) * NE:(c % 8 + 1) * NE],
                                                    in_=bstg[0:NE, c * 128:(c + 1) * 128], identity=ident_f[0:NE, 0:NE]),
                   reads=[rt_r, cres], writes=[psr[4 + c // 8]])
            for hh in range(2):
                op("dve", lambda v, hh=hh: v.tensor_copy(out=bupT[:, hh * 8:(hh + 1) * 8, :],
                                                         in_=ps[4 + hh][:, 0:8 * NE].rearrange("p (a b) -> p a b", a=8)),
                   reads=[psr[4 + hh]], writes=[rt_r])
            op("dve", lambda v: v.tensor_scalar(out=bupT[:, 8:16, :], in0=bupT[:, 8:16, :], scalar1=1.0, scalar2=None, op0=ALU.add),
               reads=[rt_r], writes=[rt_r])
            mskb = sbt(st, "mskb", [128, NT, NE], BF16)
            mskb_r = RL(NT)
            T2 = []
            for par in range(2):
                T2.append({
                    "lgt": sbt(st, "lgt", [128, NE], F32), "v8": sbt(st, "v8", [128, 8], F32), "i8": sbt(st, "i8", [128, 8], U32),
                    "i8f": sbt(st, "i8f", [128, 8], F32), "nmx": sbt(st, "nmx", [128, 1], F32), "exl": sbt(st, "exl", [128, NE], F32),
                    "msk": sbt(st, "msk", [128, NE], F32), "den": sbt(st, "den", [128, 1], F32), "posf": sbt(st, "posf", [128, NE], F32),
                    "oh3": sbt(st, "oh3", [128, 4, NE], F32), "sc3": sbt(st, "sc3", [128, 4, NE], F32), "pk": sbt(st, "pk", [128, 4], F32),
                    "vk": sbt(st, "vk", [128, 4], F32), "dk": sbt(st, "dk", [128, 4], F32), "w_r": Res()})
            iota3 = iota_e[:].unsqueeze(1).to_broadcast([128, 4, NE])

            def route_tile(i):
                T = T2[i % 2]
                lgt, v8, i8, i8f, nmx, exl, msk, den = (T[k] for k in ("lgt", "v8", "i8", "i8f", "nmx", "exl", "msk", "den"))
                posf, oh3, sc3, pk, vk, dk, w_r = (T[k] for k in ("posf", "oh3", "sc3", "pk", "vk", "dk", "w_r"))
                lb, pbk = 2 + (i % 2), 6 + (i % 2)
                for kc in range(8):
                    op("pe", lambda t, kc=kc: t.matmul(ps[lb][:, 0:NE], lhsT=xnT[:, kc, i * 128:(i + 1) * 128], rhs=wr[:, kc, :],
                                                       start=(kc == 0), stop=(kc == 7)),
                       reads=[xnT_r[i], rt_r], writes=[psr[lb]], inc=(kc == 7))
                    yield
                op("dve", lambda v: v.tensor_tensor(out=lgt[:], in0=ps[lb][:, 0:NE], in1=brb[:], op=ALU.add),
                   reads=[psr[lb], rt_r], writes=[w_r])
                yield
                op("dve", lambda v: v.max(out=v8[:], in_=lgt[:]), reads=[w_r], writes=[w_r])
                yield
                op("dve", lambda v: v.max_index(out=i8[:], in_max=v8[:], in_values=lgt[:]), reads=[w_r], writes=[w_r])
                yield
                op("dve", lambda v: v.tensor_copy(out=i8f[:], in_=i8[:]), reads=[w_r], writes=[w_r])
                yield
                op("dve", lambda v: v.tensor_scalar(out=msk[:], in0=lgt[:], scalar1=v8[:, 3:4], scalar2=None, op0=ALU.is_ge),
                   reads=[w_r], writes=[w_r])
                yield
                op("dve", lambda v: v.tensor_copy(out=mskb[:, i, :], in_=msk[:]), reads=[w_r], writes=[mskb_r[i]])
                yield
                op("dve", lambda v: v.tensor_scalar(out=nmx[:], in0=v8[:, 0:1], scalar1=-1.0, scalar2=None, op0=ALU.mult),
                   reads=[w_r], writes=[w_r])
                yield
                op("act", lambda a: a.activation(out=exl[:], in_=lgt[:], func=AF.Exp, bias=nmx[:, 0:1]), reads=[w_r], writes=[w_r])
                yield
                op("pe", lambda t: t.matmul(ps[pbk][:, 0:NE], lhsT=SU[:], rhs=mskb[:, i, :], start=True, stop=(i == 0)),
                   reads=[mskb_r[i], cres], writes=[psr[pbk]], inc=(i == 0))
                yield
                for i2 in range(i):
                    op("pe", lambda t, i2=i2: t.matmul(ps[pbk][:, 0:NE], lhsT=ones_bf[:], rhs=mskb[:, i2, :], start=False,
                                                       stop=(i2 == i - 1)),
                       reads=[mskb_r[i2], cres], writes=[psr[pbk]], inc=(i2 == i - 1))
                    yield
                op("dve", lambda v: v.tensor_copy(out=posf[:], in_=ps[pbk][:, 0:NE]), reads=[psr[pbk]], writes=[w_r])
                yield
                op("dve", lambda v: v.tensor_tensor(out=exl[:], in0=exl[:], in1=msk[:], op=ALU.mult), reads=[w_r], writes=[w_r])
                yield
                op("dve", lambda v: v.reduce_sum(out=den[:], in_=exl[:], axis=mybir.AxisListType.X), reads=[w_r], writes=[w_r])
                yield
                op("dve", lambda v: v.reciprocal(out=den[:], in_=den[:]), reads=[w_r], writes=[w_r])
                yield
                op("dve", lambda v: v.tensor_scalar(out=G[:, i, :], in0=exl[:], scalar1=den[:, 0:1], scalar2=None, op0=ALU.mult),
                   reads=[w_r], writes=[G_r[i]])
                yield
                op("dve", lambda v: v.tensor_tensor(out=oh3[:], in0=iota3, in1=i8f[:, 0:4].unsqueeze(2).to_broadcast([128, 4, NE]),
                                                    op=ALU.is_equal),
                   reads=[w_r, cres], writes=[w_r])
                yield
                op("dve", lambda v: v.tensor_tensor(out=sc3[:], in0=oh3[:], in1=posf[:].unsqueeze(1).to_broadcast([128, 4, NE]), op=ALU.mult),
                   reads=[w_r], writes=[w_r])
                yield
                op("dve", lambda v: v.reduce_sum(out=pk[:], in_=sc3[:], axis=mybir.AxisListType.X), reads=[w_r], writes=[w_r])
                yield
                op("dve", lambda v: v.tensor_tensor(out=sc3[:], in0=oh3[:], in1=G[:, i, :].unsqueeze(1).to_broadcast([128, 4, NE]), op=ALU.mult),
                   reads=[w_r, G_r[i]], writes=[w_r])
                yield
                op("dve", lambda v: v.reduce_sum(out=gk[:, i, :], in_=sc3[:], axis=mybir.AxisListType.X), reads=[w_r], writes=[gk_r[i]])
                yield
                op("dve", lambda v: v.tensor_scalar(out=vk[:], in0=pk[:], scalar1=float(CAP), scalar2=None, op0=ALU.is_lt), reads=[w_r], writes=[w_r])
                yield
                op("dve", lambda v: v.tensor_tensor(out=gk[:, i, :], in0=gk[:, i, :], in1=vk[:], op=ALU.mult), reads=[w_r, gk_r[i]], writes=[gk_r[i]])
                yield
                op("dve", lambda v: v.scalar_tensor_tensor(out=dk[:], in0=i8f[:, 0:4], scalar=float(CAP), in1=pk[:], op0=ALU.mult, op1=ALU.add),
                   reads=[w_r], writes=[w_r])
                yield
                op("dve", lambda v: v.tensor_scalar(out=vk[:], in0=vk[:], scalar1=-1.0e6, scalar2=1.0e6, op0=ALU.mult, op1=ALU.add), reads=[w_r], writes=[w_r])
                yield
                op("dve", lambda v: v.tensor_tensor(out=dk[:], in0=dk[:], in1=vk[:], op=ALU.add), reads=[w_r], writes=[w_r])
                yield
                op("dve", lambda v: v.tensor_copy(out=dest[:, i, :], in_=dk[:]), reads=[w_r], writes=[dest_r[i]])
                yield
                for k in range(4):
                    op("pool", lambda g, k=k: g.indirect_dma_start(
                        out=xs_d, out_offset=bass.IndirectOffsetOnAxis(ap=dest[:, i, k:k + 1], axis=0),
                        in_=xn2all[:, i, :], in_offset=None, bounds_check=breg, oob_is_err=False),
                        reads=[dest_r[i], xn2all_r[i]], dma=True)
                    yield
                jj = i % 2
                op("pe", lambda t: t.transpose(out=ps[4 + jj][0:NE, 0:128], in_=G[:, i, :], identity=ident_f[:]),
                   reads=[G_r[i], cres], writes=[psr[4 + jj]])
                yield
                op("act", lambda a: a.copy(out=GT[jj][:], in_=ps[4 + jj][0:NE, 0:128]), reads=[psr[4 + jj]], writes=[GT_r[jj]])
                yield
                for half in range(2):
                    b = lb
                    op("pe", lambda t: t.matmul(ps[b][:], lhsT=GT[jj][:], rhs=bdn[:, half * 512:(half + 1) * 512], start=True, stop=True),
                       reads=[GT_r[jj], rt_r], writes=[psr[b]])
                    yield
                    op("dve", lambda v: v.tensor_tensor(out=hres[:, i, half * 512:(half + 1) * 512], in0=ps[b][:],
                                                        in1=hres[:, i, half * 512:(half + 1) * 512], op=ALU.add),
                       reads=[psr[b], h_r[i]], writes=[h_r[i]])
                    yield

            def route_pair(i0):
                gens = [route_tile(i) for i in (i0, i0 + 1)]
                alive = [True, True]
                while any(alive):
                    for gi, g in enumerate(gens):
                        if alive[gi]:
                            try:
                                next(g)
                            except StopIteration:
                                alive[gi] = False

            def hook5(i):
                if i >= 2 and i % 2 == 0:
                    route_pair(i - 2)

            def src5(i):
                return hres[:, i, :], [h_r[i]]
            norm_T(src5, 1, tok_out=lambda i: (xn2all[:, i, :], [xn2all_r[i]]), statrow=3, per_tile=hook5)
            kb.barrier()

        if stop == "ROUTE":
            dbg["G"] = nc.dram_tensor("dbg_G", [128, NT * NE], F32, kind="ExternalOutput").ap()
            op("sp", lambda q: q.dma_start(out=dbg["G"], in_=G[:].rearrange("p a b -> p (a b)")), reads=[rt_r], dma=True)
            dbg["dest"] = nc.dram_tensor("dbg_dest", [128, NT * 4], I32, kind="ExternalOutput").ap()
            op("sp", lambda q: q.dma_start(out=dbg["dest"], in_=dest[:].rearrange("p a b -> p (a b)")), reads=[rt_r], dma=True)
            dbg["gk"] = nc.dram_tensor("dbg_gk", [128, NT * 4], F32, kind="ExternalOutput").ap()
            op("sp", lambda q: q.dma_start(out=dbg["gk"], in_=gk[:].rearrange("p a b -> p (a b)")), reads=[rt_r], dma=True)
            kb.barrier()
            return nc, dbg

        with contextlib.ExitStack() as st:
            NR = 5
            NSTG = 8
            for t_ in range(10, 16):
                op("sp", lambda q, t_=t_: q.dma_start(out=hsp_d[(t_ - 10) * 128:(t_ - 9) * 128, :], in_=hres[:, t_, :]),
                   reads=[h_r[t_]], dma=True)
            kb.barrier()
            ring = [sbt(st, "ring%d" % j, [128, 8, D], BF16) for j in range(NR)]
            ring_r = [RL(8) for _ in range(NR)]
            xflat = xnT[:].rearrange("p a b -> p (a b)")
            xe = xflat[:, 0:4096].rearrange("p (a b) -> p a b", a=NA)
            xe_r = Res()
            xeT = xflat[:, 4096:8192].rearrange("p (a b) -> p a b", a=8)
            xeT_r = Res()
            actT = xflat[:, 8192:12288].rearrange("p (a b) -> p a b", a=8)
            actT_r = RL(8)
            stg = [sbt(st, "stg%d" % j, [128, D], F32) for j in range(2)]
            stg = [t[:] for t in stg] + [xflat[:, 12288 + j * 2048:12288 + (j + 1) * 2048].bitcast(F32) for j in range(2)]
            stg += [hres[:, 12 + j, :] for j in range(4)]
            stg_r = RL(NSTG)
            gc = sbt(st, "gc", [128, CAP], F32)
            sg = sbt(st, "sg", [128, CAP], F32)
            uc = sbt(st, "uc", [128, CAP], F32)
            gc_r, sg_r, uc_r = Res(), Res(), Res()
            yst = [gb[j][:] for j in range(2)] + [hres[:, 10 + j, :] for j in range(2)]
            yst_r = RL(4)
            w_up_v = w_up.rearrange("e (kc p) n -> e p kc n", p=128)
            w_dn_v = w_down.rearrange("e (kc p) n -> e p kc n", p=128)
            ys_res = Res()
            npiece = [0]

            def load_piece(mi, kc):
                e_, part = divmod(mi, 3)
                if e_ >= NE:
                    return
                j = mi % NR
                n = npiece[0]
                npiece[0] += 1
                sj = n % NSTG
                src = w_up_v[e_, :, kc, part * D:(part + 1) * D] if part < 2 else w_dn_v[e_, :, kc, :]
                op("sp", lambda q: q.dma_start(out=stg[sj], in_=src), writes=[stg_r[sj]], dma=True)
                op("act", lambda a: a.copy(out=ring[j][:, kc, :], in_=stg[sj]), reads=[stg_r[sj]], writes=[ring_r[j][kc]])

            def load_xe(e_):
                op("pool", lambda q: q.dma_start(out=xe[:, 0:3, :], in_=xs_d[e_ * CAP:e_ * CAP + 384, :].rearrange("(a p) n -> p a n", p=128)),
                   writes=[xe_r], dma=True)
                op("pool", lambda q: q.dma_start(out=xe[0:LAST, 3, :], in_=xs_d[e_ * CAP + 384:(e_ + 1) * CAP, :]),
                   writes=[xe_r], dma=True)

            load_xe(0)
            for mi in range(3):
                for kc in range(8):
                    load_piece(mi, kc)
            for e in range(NE):
                jg, ju, jd = (3 * e) % NR, (3 * e + 1) % NR, (3 * e + 2) % NR
                for a_ in range(NA):
                    b = a_ % 2
                    pb = ps[b][:].bitcast(BF16)
                    rows = 128
                    for kc in range(8):
                        op("pe", lambda t, kc=kc: t.transpose(out=pb[:, kc * 128:kc * 128 + rows], in_=xe[0:rows, a_, kc * 128:(kc + 1) * 128],
                                                              identity=ident_bf[0:rows, 0:rows]),
                           reads=[xe_r, cres], writes=[psr[b]], inc=(kc == 7))
                    o_ap = xeT[:, :, a_ * 128:a_ * 128 + rows]
                    i_ap = pb.rearrange("p (a b) -> p a b", a=8)[:, :, 0:rows]
                    op("dve", lambda v: v.tensor_copy(out=o_ap, in_=i_ap), reads=[psr[b]], writes=[xeT_r])
                if e + 1 < NE:
                    load_xe(e + 1)
                for nch in range(8):
                    bg = 2 + (nch % 2) * 2
                    bu = bg + 1
                    for kc in range(8):
                        op("pe", lambda t, kc=kc: t.matmul(ps[bg][:, 0:CAP], lhsT=ring[jg][:, kc, nch * 128:(nch + 1) * 128], rhs=xeT[:, kc, 0:CAP],
                                                           start=(kc == 0), stop=(kc == 7)),
                           reads=[ring_r[jg][kc], xeT_r], writes=[psr[bg]], inc=(kc == 7))
                    for kc in range(8):
                        op("pe", lambda t, kc=kc: t.matmul(ps[bu][:, 0:CAP], lhsT=ring[ju][:, kc, nch * 128:(nch + 1) * 128], rhs=xeT[:, kc, 0:CAP],
                                                           start=(kc == 0), stop=(kc == 7)),
                           reads=[ring_r[ju][kc], xeT_r], writes=[psr[bu]], inc=(kc == 7))
                    op("dve", lambda v: v.tensor_scalar(out=gc[:], in0=ps[bg][:, 0:CAP], scalar1=bupT[:, nch, e:e + 1], scalar2=7.0,
                                                        op0=ALU.add, op1=ALU.min),
                       reads=[psr[bg], rt_r], writes=[gc_r])
                    op("act", lambda a: a.activation(out=sg[:], in_=gc[:], func=AF.Sigmoid, scale=1.702),
                       reads=[gc_r], writes=[sg_r])
                    load_piece(3 * e + 3 + (nch // 4), (2 * nch) % 8)
                    load_piece(3 * e + 3 + (nch // 4), (2 * nch + 1) % 8)
                    op("dve", lambda v: v.tensor_scalar(out=uc[:], in0=ps[bu][:, 0:CAP], scalar1=bupT[:, 8 + nch, e:e + 1], scalar2=8.0,
                                                        op0=ALU.add, op1=ALU.min),
                       reads=[psr[bu], rt_r], writes=[uc_r])
                    op("dve", lambda v: v.tensor_tensor(out=sg[:], in0=gc[:], in1=sg[:], op=ALU.mult),
                       reads=[gc_r, sg_r], writes=[sg_r])
                    op("dve", lambda v: v.scalar_tensor_tensor(out=actT[:, nch, 0:CAP], in0=uc[:], scalar=-6.0, in1=sg[:],
                                                               op0=ALU.max, op1=ALU.mult),
                       reads=[sg_r, uc_r], writes=[actT_r[nch]])
                gi = 0
                for a_ in range(NA):
                    yp = (e * NA + a_) % 4
                    rows = 128
                    for half in range(2):
                        b = 6 + half
                        for nch in range(8):
                            op("pe", lambda t, nch=nch: t.matmul(ps[b][0:rows, :], lhsT=actT[:, nch, a_ * 128:a_ * 128 + rows],
                                                                 rhs=ring[jd][:, nch, half * 512:(half + 1) * 512],
                                                                 start=(nch == 0), stop=(nch == 7)),
                               reads=[actT_r[nch], ring_r[jd][nch]], writes=[psr[b]], inc=(nch == 7))
                        op("dve", lambda v: v.tensor_copy(out=yst[yp][0:rows, half * 512:(half + 1) * 512], in_=ps[b][0:rows, :]),
                           reads=[psr[b]], writes=[yst_r[yp]])
                        load_piece(3 * e + 5, gi)
                        gi += 1
                    r0 = e * CAP + a_ * 128
                    srows = 128 if a_ < 3 else LAST
                    op("pool", lambda q: q.dma_start(out=ys_d[r0:r0 + srows, :], in_=yst[yp][0:srows, :]), reads=[yst_r[yp]], dma=True)
            kb.barrier()
            for t_ in range(10, 16):
                op("sp", lambda q, t_=t_: q.dma_start(out=hres[:, t_, :], in_=hsp_d[(t_ - 10) * 128:(t_ - 9) * 128, :]),
                   writes=[h_r[t_]], dma=True)

        with contextlib.ExitStack() as st:
            NYG = 8
            yg = [sbt(st, "yg%d" % j, [128, D], F32) for j in range(NYG)]
            yg_r = RL(NYG)
            for j in range(NYG):
                op("dve", lambda v, j=j: v.memset(yg[j][:], 0.0), writes=[yg_r[j]])
            load_gain(2)
            load_gain(3)
            junk = sbt(st, "fjunk", [128, D], BF16)
            junk_r = Res()
            xnb = [sbt(st, "fxnb%d" % j, [128, D], BF16) for j in range(2)]
            xnb_r = RL(2)
            fsq = sbt(st, "fsq", [128, 2 * NT], F32)
            wg = sbt(st, "wg", [128, 8, D], BF16)
            wp = sbt(st, "wp", [128, 2, D], BF16)
            wg_r = RL(10)
            wstg = [sbt(st, "wstg%d" % j, [128, D], F32) for j in range(2)]
            wstg_r = RL(2)
            wgv = w_ple_gate.rearrange("(kc p) n -> p kc n", p=128)
            wpv = w_ple.rearrange("(kc p) n -> p kc n", p=128)
            for n in range(10):
                src = wgv[:, n, :] if n < 8 else wpv[:, n - 8, :]
                dstw = wg[:, n, :] if n < 8 else wp[:, n - 8, :]
                op("sp", lambda q: q.dma_start(out=wstg[n % 2][:], in_=src), writes=[wstg_r[n % 2]], dma=True)
                op("act", lambda a: a.copy(out=dstw, in_=wstg[n % 2][:]), reads=[wstg_r[n % 2]], writes=[wg_r[n]])
            pf = [sbt(st, "pf%d" % j, [128, 256], F32) for j in range(2)]
            pf_r = RL(2)
            pt = [sbt(st, "pt%d" % j, [128, 256], BF16) for j in range(2)]
            pt_r = RL(2)
            pT = [sbt(st, "pT%d" % j, [128, 2, 128], BF16) for j in range(2)]
            pT_r = RL(2)
            sgt = [sbt(st, "sgt%d" % j, [128, 512], F32) for j in range(2)]
            sgt_r = RL(2)
            ot = [sbt(st, "ot%d" % j, [128, D], F32) for j in range(2)]
            ot_r = RL(2)

            def combine(i):
                for k in range(4):
                    y = (4 * i + k) % NYG
                    op("pool", lambda g: g.indirect_dma_start(
                        out=yg[y][:, :], out_offset=None, in_=ys_d,
                        in_offset=bass.IndirectOffsetOnAxis(ap=dest[:, i, k:k + 1], axis=0),
                        bounds_check=breg, oob_is_err=False),
                        reads=[dest_r[i], ys_res], writes=[yg_r[y]], dma=True)
                    op("dve", lambda v: v.scalar_tensor_tensor(out=hres[:, i, :], in0=yg[y][:], scalar=gk[:, i, k:k + 1],
                                                               in1=hres[:, i, :], op0=ALU.mult, op1=ALU.add),
                       reads=[yg_r[y], gk_r[i], h_r[i]], writes=[h_r[i]])

            sA_r, sC_r = RL(NT), RL(NT)

            def pleA(i):
                j = i % 2
                ssc = stat[:, 4, i:i + 1]
                op("act", lambda a: a.activation(out=junk[:], in_=hres[:, i, :], func=AF.Square, accum_out=ssc),
                   reads=[h_r[i]], writes=[junk_r, sA_r[i]])
                op("act", lambda a: a.activation(out=fsq[:, i:i + 1], in_=ssc, func=AF.Sqrt, scale=1.0 / D, bias=EPS),
                   reads=[sA_r[i]], writes=[sA_r[i]])
                op("dve", lambda v: v.reciprocal(out=ssc, in_=fsq[:, i:i + 1]), reads=[sA_r[i]], writes=[sA_r[i]])
                op("dve", lambda v: v.scalar_tensor_tensor(out=xnb[j][:], in0=hres[:, i, :], scalar=ssc, in1=gb[0][:],
                                                           op0=ALU.mult, op1=ALU.mult),
                   reads=[h_r[i], sA_r[i], gb_r[0]], writes=[xnb_r[j]])
                op("sp", lambda q: q.dma_start(out=pf[j][:], in_=pin[i * 128:(i + 1) * 128, :]), writes=[pf_r[j]], dma=True)
                op("act", lambda a: a.copy(out=pt[j][:], in_=pf[j][:]), reads=[pf_r[j]], writes=[pt_r[j]])

            def pleA2(i):
                j = i % 2
                pb = ps[j][:].bitcast(BF16)
                for kc in range(8):
                    op("pe", lambda t, kc=kc: t.transpose(out=pb[:, kc * 128:(kc + 1) * 128], in_=xnb[j][:, kc * 128:(kc + 1) * 128],
                                                          identity=ident_bf[:]),
                       reads=[xnb_r[j], cres], writes=[psr[j]], inc=(kc == 7))
                op("act", lambda a: a.copy(out=xnT[:, :, i * 128:(i + 1) * 128], in_=pb.rearrange("p (a b) -> p a b", a=8)),
                   reads=[psr[j]], writes=[xnT_r[i]])
                pb2 = ps[2 + j][:].bitcast(BF16)
                for kc in range(2):
                    op("pe", lambda t, kc=kc: t.transpose(out=pb2[:, kc * 128:(kc + 1) * 128], in_=pt[j][:, kc * 128:(kc + 1) * 128],
                                                          identity=ident_bf[:]),
                       reads=[pt_r[j], cres], writes=[psr[2 + j]], inc=(kc == 1))
                op("act", lambda a: a.copy(out=pT[j][:], in_=pb2[:, 0:256].rearrange("p (a b) -> p a b", a=2)),
                   reads=[psr[2 + j]], writes=[pT_r[j]])

            def pleB(i):
                j = i % 2
                for half in range(2):
                    n0 = half * 512
                    bg = 4 + 2 * half
                    bp = bg + 1
                    for kc in range(8):
                        op("pe", lambda t, kc=kc: t.matmul(ps[bg][:], lhsT=xnT[:, kc, i * 128:(i + 1) * 128], rhs=wg[:, kc, n0:n0 + 512],
                                                           start=(kc == 0), stop=(kc == 7)),
                           reads=[xnT_r[i], wg_r[kc]], writes=[psr[bg]], inc=(kc == 7))
                    for kc in range(2):
                        op("pe", lambda t, kc=kc: t.matmul(ps[bp][:], lhsT=pT[j][:, kc, :], rhs=wp[:, kc, n0:n0 + 512],
                                                           start=(kc == 0), stop=(kc == 1)),
                           reads=[pT_r[j], wg_r[8 + kc]], writes=[psr[bp]], inc=(kc == 1))

            def pleB2(i):
                for half in range(2):
                    n0 = half * 512
                    bg = 4 + 2 * half
                    bp = bg + 1
                    op("act", lambda a: a.activation(out=sgt[half][:], in_=ps[bg][:], func=AF.Sigmoid), reads=[psr[bg]], writes=[sgt_r[half]])
                    op("dve", lambda v: v.tensor_tensor(out=sgt[half][:], in0=ps[bp][:], in1=sgt[half][:], op=ALU.mult),
                       reads=[psr[bp], sgt_r[half]], writes=[sgt_r[half]])
                    op("dve", lambda v: v.tensor_tensor(out=hres[:, i, n0:n0 + 512], in0=hres[:, i, n0:n0 + 512], in1=sgt[half][:], op=ALU.add),
                       reads=[sgt_r[half], h_r[i]], writes=[h_r[i]])

            def pleC(i):
                j = i % 2
                ssf = stat[:, 5, i:i + 1]
                op("act", lambda a: a.activation(out=junk[:], in_=hres[:, i, :], func=AF.Square, accum_out=ssf),
                   reads=[h_r[i]], writes=[junk_r, sC_r[i]])
                op("act", lambda a: a.activation(out=fsq[:, NT + i:NT + i + 1], in_=ssf, func=AF.Sqrt, scale=1.0 / D, bias=EPS),
                   reads=[sC_r[i]], writes=[sC_r[i]])
                op("dve", lambda v: v.reciprocal(out=ssf, in_=fsq[:, NT + i:NT + i + 1]), reads=[sC_r[i]], writes=[sC_r[i]])
                op("dve", lambda v: v.scalar_tensor_tensor(out=ot[j][:], in0=hres[:, i, :], scalar=ssf, in1=gb[1][:], op0=ALU.mult, op1=ALU.mult),
                   reads=[h_r[i], sC_r[i], gb_r[1]], writes=[ot_r[j]])
                op("sp", lambda q: q.dma_start(out=out[i * 128:(i + 1) * 128, :], in_=ot[j][:]), reads=[ot_r[j]], dma=True)

            combine(0)
            for r in range(NT + 2):
                if 0 <= r - 1 < NT:
                    pleB(r - 1)
                if r < NT:
                    pleA(r)
                if r + 1 < NT:
                    combine(r + 1)
                if 0 <= r - 1 < NT:
                    pleB2(r - 1)
                if r < NT:
                    pleA2(r)
                if 0 <= r - 2 < NT:
                    pleC(r - 2)
            kb.barrier()
        moe.close()
    return nc, dbg


_CACHE = {}


def _prep(inputs, b):
    f = lambda a: np.ascontiguousarray(np.asarray(a, dtype=np.float32))
    g = inputs
    lruvec = np.concatenate([g["conv_w"][0], g["conv_b"][0][None], g["lru_b_a"][0][None], g["lru_b_x"][0][None],
                             g["lru_lambda"][0][None], g["lru_out_g"][0][None]], axis=0)
    gains = np.stack([g["mix_norm_g"][0], g["ffn_norm_g"][0], g["ple_norm_g"][0], g["final_norm_g"]], axis=0)
    return {
        "x": f(g["x"][b]), "p": f(g["p"][0, b]), "gains": f(gains), "w_in": f(g["w_in"][0]), "lruvec": f(lruvec),
        "lru_w_a": f(g["lru_w_a"][0]), "lru_w_x": f(g["lru_w_x"][0]), "sb_out_g": f(g["sb_out_g"][0][None]),
        "w_out": f(g["w_out"][0]), "w_router": f(g["w_router"][0]), "b_router": f(g["b_router"][0][None]),
        "w_up": f(g["w_up"][0]), "b_up": f(g["b_up"][0]), "w_down": f(g["w_down"][0]), "b_down": f(g["b_down"][0]),
        "w_ple_gate": f(g["w_ple_gate"][0]), "w_ple": f(g["w_ple"][0]),
    }


def kernel(**inputs):
    inputs = {k: np.asarray(v) for k, v in inputs.items()}
    if "nc" not in _CACHE:
        _CACHE["nc"] = build("FULL")[0]
    nc = _CACHE["nc"]
    shared = _prep(inputs, 0)
    in_maps = []
    for b in range(8):
        m = dict(shared)
        m["x"] = np.ascontiguousarray(inputs["x"][b], dtype=np.float32)
        m["p"] = np.ascontiguousarray(inputs["p"][0, b], dtype=np.float32)
        in_maps.append(m)
    res = run_bass_kernel_spmd(nc, in_maps, core_ids=list(range(8)))
    return np.stack([np.asarray(r["out"], dtype=np.float32) for r in res.results], axis=0)
```

```python
import contextlib
import numpy as np
import concourse.bass as bass
import concourse.mybir as mybir
from concourse.bass_utils import run_bass_kernel_spmd

F32 = mybir.dt.float32
BF16 = mybir.dt.bfloat16
I32 = mybir.dt.int32
U32 = mybir.dt.uint32
AF = mybir.ActivationFunctionType
ALU = mybir.AluOpType

S = 2048
D = 1024
NT = 16
NE = 32
CAP = 448
NA = 4
LAST = CAP - 384
NSLOT = NE * CAP
EPS = 1e-6
NEG = -30000.0


class Res:
    __slots__ = ("w", "r", "const")

    def __init__(self, const=False):
        self.w = None
        self.r = {}
        self.const = const


def RL(n):
    return [Res() for _ in range(n)]


class KB:
    NDS = 48
    NPRE = 40

    def __init__(self, nc):
        self.nc = nc
        self.es = contextlib.ExitStack()
        self.E = {}
        for nm, h in (("pe", nc.tensor), ("act", nc.scalar), ("dve", nc.vector),
                      ("pool", nc.gpsimd), ("sp", nc.sync)):
            self.E[nm] = {"h": h, "sem": self.es.enter_context(nc.semaphore("c_" + nm)),
                          "n": 0, "seen": {}, "hist": []}
        self.ds = [[self.es.enter_context(nc.semaphore("d%d" % i)), 0] for i in range(self.NDS + self.NPRE)]
        self.dhist = {}
        self.dn = {"sp": 0, "pool": 0, "act": 0}
        self.drange = {"sp": (0, 28), "pool": (28, 44), "act": (44, 48)}
        self.pn = 0
        self.ninst = 0

    def _wait(self, e, ev):
        E = self.E[e]
        kind, key, val = ev
        if kind == "e":
            if key == e and e == "pe":
                return
            sem = self.E[key]["sem"]
            hist = self.E[key]["hist"]
            snap = hist[val - 1] if val - 1 < len(hist) else {}
        else:
            sem = self.ds[key][0]
            snap = self.dhist.get((key, val), {})
        k = (kind, key)
        if E["seen"].get(k, 0) >= val:
            return
        E["h"].wait_ge(sem, val)
        new = dict(E["seen"])
        for kk, vv in snap.items():
            if new.get(kk, 0) < vv:
                new[kk] = vv
        new[k] = val
        E["seen"] = new

    def op(self, e, fn, reads=(), writes=(), dma=False, inc=True, pre=False):
        E = self.E[e]
        evs = []
        for r in reads:
            if r.w is not None:
                evs.append(r.w)
        for w in writes:
            if w.w is not None:
                evs.append(w.w)
            for (kind, key), val in w.r.items():
                evs.append((kind, key, val))
        if dma:
            if pre:
                i = self.NDS + self.pn
                self.pn = (self.pn + 1) % self.NPRE
            else:
                lo, hi = self.drange[e]
                i = lo + self.dn[e]
                self.dn[e] = (self.dn[e] + 1) % (hi - lo)
            if self.ds[i][1] > 0:
                evs.append(("d", i, self.ds[i][1]))
        for ev in evs:
            self._wait(e, ev)
        inst = fn(E["h"])
        self.ninst += 1
        if dma:
            self.ds[i][1] += 16
            inst.then_inc(self.ds[i][0], 16)
            me = ("d", i, self.ds[i][1])
            self.dhist[(i, self.ds[i][1])] = E["seen"]
        elif inc:
            E["n"] += 1
            inst.then_inc(E["sem"], 1)
            me = ("e", e, E["n"])
            E["hist"].append(E["seen"])
        else:
            me = ("e", e, E["n"] + 1)
        for r in reads:
            if not r.const:
                k = (me[0], me[1])
                if r.r.get(k, 0) < me[2]:
                    r.r[k] = me[2]
        for w in writes:
            w.w = me
            w.r = {}
        return inst

    def barrier(self):
        evs = [("e", nm, E["n"]) for nm, E in self.E.items() if E["n"] > 0]
        evs += [("d", i, v) for i, (s, v) in enumerate(self.ds) if v > 0 and i < self.NDS]
        for e in self.E:
            for ev in evs:
                self._wait(e, ev)


def build(stop="FULL"):
    nc = bass.Bass("TRN2", target_bir_lowering=False)

    def din(name, shape, dtype=F32):
        return nc.dram_tensor(name, shape, dtype, kind="ExternalInput").ap()

    x = din("x", [S, D])
    pin = din("p", [S, 256])
    gains = din("gains", [4, D])
    w_in = din("w_in", [D, 2560])
    lruvec = din("lruvec", [9, 512])
    lru_w_a = din("lru_w_a", [8, 64, 64])
    lru_w_x = din("lru_w_x", [8, 64, 64])
    sb_out_g = din("sb_out_g", [1, 512])
    w_out = din("w_out", [D, D])
    w_router = din("w_router", [D, NE])
    b_router = din("b_router", [1, NE])
    w_up = din("w_up", [NE, D, 2 * D])
    b_up = din("b_up", [NE, 2 * D])
    w_down = din("w_down", [NE, D, D])
    b_down = din("b_down", [NE, D])
    w_ple_gate = din("w_ple_gate", [D, D])
    w_ple = din("w_ple", [256, D])
    out = nc.dram_tensor("out", [S, D], F32, kind="ExternalOutput").ap()
    xs_d = nc.dram_tensor("xs_scratch", [NSLOT, D], BF16, kind="Internal").ap()
    ys_d = nc.dram_tensor("ys_scratch", [NSLOT, D], F32, kind="Internal").ap()
    hsp_d = nc.dram_tensor("h_spill", [6 * 128, D], F32, kind="Internal").ap()
    dbg = {}

    kb = KB(nc)
    op = kb.op

    with kb.es:
        top = kb.es

        uniq = [0]

        def sbt(stack, name, shape, dtype):
            uniq[0] += 1
            return stack.enter_context(nc.sbuf_tensor("%s_%d" % (name, uniq[0]), shape, dtype))

        ps = [top.enter_context(nc.psum_tensor("ps%d" % i, [128, 512], F32)) for i in range(8)]
        psr = RL(8)

        cres = Res(const=True)
        dmat = sbt(top, "dmat", [128, 128], I32)
        ident_bf = sbt(top, "ident_bf", [128, 128], BF16)
        ident_f = sbt(top, "ident_f", [128, 128], F32)
        NU = sbt(top, "NU", [128, 128], BF16)
        NL = sbt(top, "NL", [128, 128], BF16)
        maskneg = sbt(top, "maskneg", [128, 128], BF16)
        SU = sbt(top, "SU", [128, 128], BF16)
        ones_bf = sbt(top, "ones_bf", [128, 128], BF16)
        zeros_bf = sbt(top, "zeros_bf", [128, 128], BF16)
        ones_f = sbt(top, "ones_f", [128, 1], F32)
        iota_e = sbt(top, "iota_e", [128, NE], F32)
        iota_ei = sbt(top, "iota_ei", [128, NE], I32)
        gb = [sbt(top, "gb%d" % i, [128, D], F32) for i in range(2)]
        gb_r = RL(2)
        hres = sbt(top, "hres", [128, NT, D], F32)
        h_r = RL(NT)
        xnT = sbt(top, "xnT", [128, 8, S], BF16)
        xnT_r = RL(NT)
        stat = sbt(top, "stat", [128, 8, NT], F32)
        stat_r = Res()

        op("pool", lambda g: g.iota(dmat[:], pattern=[[1, 128]], base=0, channel_multiplier=-1), writes=[cres])
        op("pool", lambda g: g.iota(iota_ei[:], pattern=[[1, NE]], base=0, channel_multiplier=0), writes=[cres])

        def cmat(dst, cmp_op, mul):
            op("dve", lambda v: v.tensor_scalar(out=dst[:], in0=dmat[:], scalar1=0.0, scalar2=mul,
                                                 op0=cmp_op, op1=ALU.mult), reads=[cres], writes=[cres])
        cmat(ident_bf, ALU.is_equal, 1.0)
        cmat(ident_f, ALU.is_equal, 1.0)
        cmat(NU, ALU.is_lt, -1.0)
        cmat(NL, ALU.is_ge, -1.0)
        cmat(maskneg, ALU.is_le, NEG)
        cmat(SU, ALU.is_gt, 1.0)
        op("dve", lambda v: v.memset(ones_bf[:], 1.0), writes=[cres])
        op("dve", lambda v: v.memset(zeros_bf[:], 0.0), writes=[cres])
        op("dve", lambda v: v.memset(ones_f[:], 1.0), writes=[cres])
        op("dve", lambda v: v.tensor_copy(out=iota_e[:], in_=iota_ei[:]), reads=[cres], writes=[cres])
        def load_gain(gidx):
            op("sp", lambda q: q.dma_start(out=gb[gidx % 2][:], in_=gains[gidx:gidx + 1, :].partition_broadcast(128)),
               writes=[gb_r[gidx % 2]], dma=True)

        breg = nc.gpsimd.to_reg(NSLOT - 1)

        def norm_T(src_fn, gidx, tok_out=None, banks=(0, 1), statrow=0, per_tile=None):
            load_gain(gidx)
            with contextlib.ExitStack() as st:
                junk = sbt(st, "nt_junk", [128, D], BF16)
                junk_r = Res()
                xnb = [sbt(st, "nt_xnb%d" % j, [128, D], BF16) for j in range(2)]
                xnb_r = RL(2)
                sq = sbt(st, "nt_sq", [128, NT], F32)
                sr = RL(NT)
                pend = []

                def flush():
                    while pend:
                        pi, pbank = pend.pop(0)
                        pbv = ps[pbank][:].bitcast(BF16)
                        o_ap = xnT[:, :, pi * 128:(pi + 1) * 128]
                        i_ap = pbv.rearrange("p (a b) -> p a b", a=8)
                        if pi % 2 == 0:
                            op("act", lambda a: a.copy(out=o_ap, in_=i_ap), reads=[psr[pbank]], writes=[xnT_r[pi]])
                        else:
                            op("dve", lambda v: v.tensor_copy(out=o_ap, in_=i_ap), reads=[psr[pbank]], writes=[xnT_r[pi]])

                for i in range(NT):
                    src, sres = src_fn(i)
                    ssc = stat[:, statrow, i:i + 1]
                    op("act", lambda a: a.activation(out=junk[:], in_=src, func=AF.Square, accum_out=ssc),
                       reads=sres + [cres], writes=[junk_r, sr[i]])
                    op("act", lambda a: a.activation(out=sq[:, i:i + 1], in_=ssc, func=AF.Sqrt,
                                                     scale=1.0 / D, bias=EPS),
                       reads=[sr[i]], writes=[sr[i]])
                    op("dve", lambda v: v.reciprocal(out=ssc, in_=sq[:, i:i + 1]), reads=[sr[i]], writes=[sr[i]])
                    if tok_out is not None:
                        dst, dres = tok_out(i)
                    else:
                        dst, dres = xnb[i % 2][:], [xnb_r[i % 2]]
                    op("dve", lambda v: v.scalar_tensor_tensor(out=dst, in0=src, scalar=ssc, in1=gb[gidx % 2][:],
                                                               op0=ALU.mult, op1=ALU.mult),
                       reads=sres + [sr[i], gb_r[gidx % 2]], writes=dres)
                    flush()
                    b = banks[i % 2]
                    pb = ps[b][:].bitcast(BF16)
                    for kc in range(8):
                        op("pe", lambda t, kc=kc: t.transpose(out=pb[:, kc * 128:(kc + 1) * 128],
                                                              in_=dst[:, kc * 128:(kc + 1) * 128],
                                                              identity=ident_bf[:]),
                           reads=dres + [cres], writes=[psr[b]], inc=(kc == 7))
                    pend.append((i, b))
                    if per_tile is not None:
                        per_tile(i)
                flush()
                if per_tile is not None:
                    per_tile(NT)
                kb.barrier()

        def halias(a0, a1, inner):
            v = hres[:, a0:a1, :].bitcast(BF16)
            return v.rearrange("p a (b c) -> p (a b) c", c=inner)

        mix = contextlib.ExitStack()
        top.enter_context(mix)
        w_in_v = w_in.rearrange("(kc p) n -> p kc n", p=128)
        wq2 = sbt(mix, "wq2", [128, 8, 512], BF16)
        wl = sbt(mix, "wl", [128, 8, 1024], BF16)
        wq = [halias(12, 14, 512), halias(14, 16, 512), wq2[:]]
        wq_r = RL(3)
        wl_r = Res()
        for j, c0 in enumerate((1024, 1536, 2048)):
            for kc in range(8):
                op("pool", lambda g, j=j, kc=kc, c0=c0: g.dma_start(out=wq[j][:, kc, :], in_=w_in_v[:, kc, c0:c0 + 512]),
                   writes=[wq_r[j]], dma=True, pre=True)
        for kc in range(8):
            op("pool", lambda g, kc=kc: g.dma_start(out=wl[:, kc, :], in_=w_in_v[:, kc, 0:1024]), writes=[wl_r], dma=True, pre=True)

        with contextlib.ExitStack() as st:
            xt = [sbt(st, "xt%d" % j, [128, D], F32) for j in range(3)]
            xt_r = RL(3)

            def src1(i):
                j = i % 3
                op("sp", lambda q: q.dma_start(out=xt[j][:], in_=x[i * 128:(i + 1) * 128, :]),
                   writes=[xt_r[j]], dma=True)
                return xt[j][:], [xt_r[j]]
            norm_T(src1, 0)

        if stop == "P1":
            dbg["xnT"] = nc.dram_tensor("dbg_xnT", [128, 8 * S], BF16, kind="ExternalOutput").ap()
            op("sp", lambda q: q.dma_start(out=dbg["xnT"], in_=xnT[:].rearrange("p a b -> p (a b)")),
               reads=xnT_r, dma=True)
            kb.barrier()
            return nc, dbg

        sbyT = sbt(mix, "sbyT", [128, 4, S], BF16)
        sby_r = Res()
        gsb = sbt(mix, "gsb", [128, 4], F32)
        gstg = sbt(mix, "gstg", [1, 512], F32)
        gsb_r = Res()
        op("sp", lambda q: q.dma_start(out=gstg[:], in_=sb_out_g), writes=[gsb_r], dma=True)
        for c in range(4):
            op("pe", lambda t, c=c: t.transpose(out=ps[0][:, c:c + 1], in_=gstg[0:1, c * 128:(c + 1) * 128],
                                                identity=ident_f[0:1, 0:1]),
               reads=[gsb_r, cres], writes=[psr[0]])
        op("dve", lambda v: v.tensor_copy(out=gsb[:], in_=ps[0][:, 0:4]), reads=[psr[0]], writes=[gsb_r])
        kb.barrier()

        lyT = sbt(mix, "lyT", [128, 4, S], BF16)
        ly_r = Res()
        with contextlib.ExitStack() as st:
            qT = halias(0, 4, S)
            kT = halias(4, 8, S)
            vtok = halias(8, 12, 512)
            qkv_r = Res()
            bi = 0
            for j, dstT in ((0, qT), (1, kT)):
                for ch in range(4):
                    for tb in range(4):
                        b = bi % 4
                        bi += 1
                        for kc in range(8):
                            op("pe", lambda t, kc=kc: t.matmul(ps[b][:], lhsT=wq[j][:, kc, ch * 128:(ch + 1) * 128],
                                                               rhs=xnT[:, kc, tb * 512:(tb + 1) * 512],
                                                               start=(kc == 0), stop=(kc == 7)),
                               reads=[wq_r[j]] + xnT_r[tb * 4:tb * 4 + 4], writes=[psr[b]], inc=(kc == 7))
                        sc = 0.125 if j == 0 else 1.0
                        if bi % 2 == 0:
                            op("act", lambda a: a.activation(out=dstT[:, ch, tb * 512:(tb + 1) * 512], in_=ps[b][:],
                                                             func=AF.Copy, scale=sc),
                               reads=[psr[b]], writes=[qkv_r])
                        else:
                            op("dve", lambda v: v.tensor_scalar(out=dstT[:, ch, tb * 512:(tb + 1) * 512], in0=ps[b][:],
                                                                scalar1=sc, scalar2=None, op0=ALU.mult),
                               reads=[psr[b]], writes=[qkv_r])
            for i in range(NT):
                b = bi % 4
                bi += 1
                for kc in range(8):
                    op("pe", lambda t, kc=kc: t.matmul(ps[b][:], lhsT=xnT[:, kc, i * 128:(i + 1) * 128],
                                                       rhs=wq[2][:, kc, :], start=(kc == 0), stop=(kc == 7)),
                       reads=[wq_r[2], xnT_r[i]], writes=[psr[b]], inc=(kc == 7))
                if i % 2 == 0:
                    op("act", lambda a: a.copy(out=vtok[:, i, :], in_=ps[b][:]), reads=[psr[b]], writes=[qkv_r])
                else:
                    op("dve", lambda v: v.tensor_copy(out=vtok[:, i, :], in_=ps[b][:]), reads=[psr[b]], writes=[qkv_r])
            kb.barrier()

            NSTR = 2
            wk = []
            for s_ in range(NSTR):
                per = []
                for par in range(2):
                    per.append({
                        "sp": sbt(st, "at_sp%d%d" % (s_, par), [128, 512], F32),
                        "spb": sbt(st, "at_spb%d%d" % (s_, par), [128, 512], BF16),
                        "d": sbt(st, "at_d%d%d" % (s_, par), [128, 512], F32),
                        "w": sbt(st, "at_w%d%d" % (s_, par), [128, 512], BF16),
                        "r": {k: Res() for k in ("sp", "spb", "d", "w")},
                    })
                wk.append(per)
            osq = sbt(st, "at_osq", [128, 512], F32)
            osq_r = Res()
            sqacc = sbt(st, "at_sqacc", [128, S], F32)
            sqacc_r = Res()
            steps = []
            for hp in range(4):
                for j in range(4):
                    cs = list(range(4 * j + 3, -1, -1))
                    for idx, c in enumerate(cs):
                        steps.append({"hp": hp, "j": j, "c": c, "first": idx == 0, "last": idx == len(cs) - 1,
                                      "par": len(steps) % 2})

            def geo(st_, s_):
                h = 2 * st_["hp"] + s_
                prt = slice((h % 2) * 64, (h % 2) * 64 + 64)
                m = st_["c"] - 4 * st_["j"]
                c0 = 128 * m if m > 0 else 0
                return h, prt, h // 2, m, c0, slice(c0, 512), st_["j"] * 512

            def stage1(s_, st_):
                h, prt, ch, m, c0, cols, t0 = geo(st_, s_)
                c, j = st_["c"], st_["j"]
                bz = 4 * s_ + st_["par"]
                diag = m >= 0
                op("pe", lambda t: t.matmul(ps[bz][:, cols], lhsT=kT[prt, ch, c * 128:(c + 1) * 128],
                                            rhs=qT[prt, ch, t0 + c0:t0 + 512], start=True, stop=not diag),
                   reads=[qkv_r], writes=[psr[bz]], inc=not diag)
                if diag:
                    op("pe", lambda t: t.matmul(ps[bz][:, c0:c0 + 128], lhsT=ident_bf[:], rhs=maskneg[:],
                                                start=False, stop=True),
                       reads=[cres], writes=[psr[bz]])

            def stage1b(s_, st_):
                h, prt, ch, m, c0, cols, t0 = geo(st_, s_)
                W = wk[s_][st_["par"]]
                R = W["r"]
                bz = 4 * s_ + st_["par"]
                op("act", lambda a: a.activation(out=W["sp"][:, cols], in_=ps[bz][:, cols], func=AF.Exp),
                   reads=[psr[bz]], writes=[R["sp"]])
                op("act", lambda a: a.activation(out=W["sp"][:, cols], in_=W["sp"][:, cols], func=AF.Ln, bias=1.0),
                   reads=[R["sp"]], writes=[R["sp"]])
                op("dve", lambda g: g.tensor_copy(out=W["spb"][:, cols], in_=W["sp"][:, cols]),
                   reads=[R["sp"]], writes=[R["spb"]])
                op("dve", lambda v: v.tensor_tensor(out=W["d"][:, cols], in0=ps[bz][:, cols], in1=W["sp"][:, cols],
                                                    op=ALU.subtract),
                   reads=[psr[bz], R["sp"]], writes=[R["d"]])

            def stage2a(s_, st_):
                h, prt, ch, m, c0, cols, t0 = geo(st_, s_)
                W = wk[s_][st_["par"]]
                R = W["r"]
                bx = 4 * s_ + 2
                if st_["first"]:
                    op("pe", lambda t: t.matmul(ps[bx][:, :], lhsT=zeros_bf[:, :], rhs=qT[:, 0, t0:t0 + 512],
                                                start=True, stop=False, skip_group_check=True),
                       reads=[cres, qkv_r], writes=[psr[bx]])
                op("pe", lambda t: t.matmul(ps[bx][:, cols], lhsT=NU[:], rhs=W["spb"][:, cols],
                                            start=False, stop=False, skip_group_check=True),
                   reads=[cres, R["spb"]], writes=[psr[bx]])
                op("dve", lambda v: v.tensor_tensor(out=W["d"][:, cols], in0=ps[bx][:, cols], in1=W["d"][:, cols],
                                                    op=ALU.add),
                   reads=[psr[bx], R["d"]], writes=[R["d"]])

            def stage2w(s_, st_):
                h, prt, ch, m, c0, cols, t0 = geo(st_, s_)
                W = wk[s_][st_["par"]]
                R = W["r"]
                op("act", lambda a: a.activation(out=W["w"][:, cols], in_=W["d"][:, cols], func=AF.Exp),
                   reads=[R["d"]], writes=[R["w"]])

            def stage2b(s_, st_):
                h, prt, ch, m, c0, cols, t0 = geo(st_, s_)
                W = wk[s_][st_["par"]]
                R = W["r"]
                bx = 4 * s_ + 2
                if st_["c"] > 0:
                    op("pe", lambda t: t.matmul(ps[bx][:, cols], lhsT=NL[:], rhs=W["spb"][:, cols],
                                                start=False, stop=False, skip_group_check=True),
                       reads=[cres, R["spb"]], writes=[psr[bx]])

            def stage2c(s_, st_):
                h, prt, ch, m, c0, cols, t0 = geo(st_, s_)
                W = wk[s_][st_["par"]]
                R = W["r"]
                bo = 4 * s_ + 3
                c, hp = st_["c"], st_["hp"]
                if st_["first"]:
                    op("pe", lambda t: t.matmul(ps[bo][prt, :], lhsT=zeros_bf[:, 0:64], rhs=qT[:, 0, t0:t0 + 512],
                                                start=True, stop=False, skip_group_check=True),
                       reads=[cres, qkv_r], writes=[psr[bo]])
                op("pe", lambda t: t.matmul(ps[bo][prt, cols], lhsT=vtok[:, c, h * 64:(h + 1) * 64],
                                            rhs=W["w"][:, cols], start=False, stop=(c == 0),
                                            skip_group_check=True),
                   reads=[qkv_r, R["w"]], writes=[psr[bo]])
                if st_["last"]:
                    op("act", lambda a: a.activation(out=sbyT[prt, hp, t0:t0 + 512], in_=ps[bo][prt, :], func=AF.Copy,
                                                     scale=gsb[prt, hp:hp + 1]),
                       reads=[psr[bo], gsb_r], writes=[sby_r])
                    if hp == 0:
                        op("act", lambda a: a.activation(out=sqacc[prt, t0:t0 + 512], in_=ps[bo][prt, :], func=AF.Square),
                           reads=[psr[bo]], writes=[sqacc_r])
                    else:
                        op("act", lambda a: a.activation(out=osq[prt, :], in_=ps[bo][prt, :], func=AF.Square),
                           reads=[psr[bo]], writes=[osq_r])
                        op("pool", lambda g: g.tensor_tensor(out=sqacc[prt, t0:t0 + 512], in0=sqacc[prt, t0:t0 + 512],
                                                             in1=osq[prt, :], op=ALU.add),
                           reads=[osq_r, sqacc_r], writes=[sqacc_r])

            nst = len(steps)
            for s_ in range(2):
                stage1(s_, steps[0])
            for r in range(nst + 1):
                if r > 0:
                    for s_ in range(2):
                        stage2a(s_, steps[r - 1])
                if r + 1 < nst:
                    for s_ in range(2):
                        stage1(s_, steps[r + 1])
                if r < nst:
                    for s_ in range(2):
                        stage1b(s_, steps[r])
                if r > 0:
                    for s_ in range(2):
                        stage2w(s_, steps[r - 1])
                    for s_ in range(2):
                        stage2b(s_, steps[r - 1])
                    for s_ in range(2):
                        stage2c(s_, steps[r - 1])
            kb.barrier()
            for i in range(NT):
                op("pe", lambda t, i=i: t.matmul(ps[0][:, i:i + 1], lhsT=sqacc[:, i * 128:(i + 1) * 128], rhs=ones_f[:, :],
                                                 start=True, stop=True),
                   reads=[sqacc_r, cres], writes=[psr[0]])
            op("dve", lambda v: v.tensor_copy(out=stat[:, 1, :], in_=ps[0][:, 0:NT]), reads=[psr[0]], writes=[stat_r])
            kb.barrier()

        if stop == "ATT":
            dbg["sbyT"] = nc.dram_tensor("dbg_sbyT", [128, 4 * S], BF16, kind="ExternalOutput").ap()
            op("sp", lambda q: q.dma_start(out=dbg["sbyT"], in_=sbyT[:].rearrange("p a b -> p (a b)")),
               reads=[sby_r], dma=True)
            dbg["stat"] = nc.dram_tensor("dbg_stat", [128, 8 * NT], F32, kind="ExternalOutput").ap()
            op("sp", lambda q: q.dma_start(out=dbg["stat"], in_=stat[:].rearrange("p a b -> p (a b)")),
               reads=[stat_r], dma=True)
            kb.barrier()
            return nc, dbg

        mix2 = contextlib.ExitStack()
        top.enter_context(mix2)
        wo = sbt(mix2, "wo", [128, 8, D], BF16)
        wo_r = Res()
        w_out_v = w_out.rearrange("(c p) n -> p c n", p=128)
        for c in range(8):
            op("pool", lambda g, c=c: g.dma_start(out=wo[:, c, :], in_=w_out_v[:, c, :]), writes=[wo_r], dma=True, pre=True)
        with contextlib.ExitStack() as st:
            lstg = sbt(st, "lstg", [9, 512], F32)
            lv = sbt(st, "lv", [128, 4, 9], F32)
            lsc = sbt(st, "lsc", [128, 4, 2], F32)
            ltmp = sbt(st, "ltmp", [128, 4], F32)
            lv_r = Res()
            op("sp", lambda q: q.dma_start(out=lstg[:], in_=lruvec), writes=[lv_r], dma=True)
            for cc in range(4):
                op("pe", lambda t, cc=cc: t.transpose(out=ps[0][:, cc * 16:cc * 16 + 9], in_=lstg[0:9, cc * 128:(cc + 1) * 128],
                                                      identity=ident_f[0:9, 0:9]),
                   reads=[lv_r, cres], writes=[psr[0]])
            op("dve", lambda v: v.tensor_copy(out=lv[:], in_=ps[0][:, 0:64].rearrange("p (a b) -> p a b", a=4)[:, :, 0:9]),
               reads=[psr[0]], writes=[lv_r])
            op("act", lambda a: a.activation(out=ltmp[:], in_=lv[:, :, 7], func=AF.Exp, scale=-1.0), reads=[lv_r], writes=[lv_r])
            op("act", lambda a: a.activation(out=ltmp[:], in_=ltmp[:], func=AF.Ln, bias=1.0), reads=[lv_r], writes=[lv_r])
            op("dve", lambda v: v.tensor_scalar(out=lsc[:, :, 0], in0=ltmp[:], scalar1=-8.0, scalar2=None, op0=ALU.mult),
               reads=[lv_r], writes=[lv_r])
            op("dve", lambda v: v.tensor_scalar(out=lsc[:, :, 1], in0=ltmp[:], scalar1=-16.0, scalar2=None, op0=ALU.mult),
               reads=[lv_r], writes=[lv_r])
            wabd = sbt(st, "wabd", [128, 4, 128], F32)
            wxbd = sbt(st, "wxbd", [128, 4, 128], F32)
            wbd_r = Res()
            op("pool", lambda g: g.memset(wabd[:], 0.0), writes=[wbd_r])
            op("pool", lambda g: g.memset(wxbd[:], 0.0), writes=[wbd_r])
            for n in range(8):
                pr = slice((n % 2) * 64, (n % 2) * 64 + 64)
                op("sp", lambda q, n=n, pr=pr: q.dma_start(out=wabd[pr, n // 2, pr], in_=lru_w_a[n]), writes=[wbd_r], dma=True)
                op("sp", lambda q, n=n, pr=pr: q.dma_start(out=wxbd[pr, n // 2, pr], in_=lru_w_x[n]), writes=[wbd_r], dma=True)

            lxp = sbt(st, "lxp", [128, S + 3], F32)
            def hf32(a0):
                return hres[:, a0:a0 + 2, :].rearrange("p a b -> p (a b)")
            lg, cx, b1, b2, b3, b4 = (hf32(a0) for a0 in (4, 6, 8, 10, 12, 14))
            gbuf = hf32(0)
            gbuf_r = Res()
            ysq = sbt(st, "ysq", [128, S], F32)
            lxp_r, lg_r, cx_r, b1_r, b2_r, b3_r, b4_r, ysq_r = RL(8)
            op("pool", lambda g: g.memset(lxp[:, 0:3], 0.0), writes=[lxp_r])
            bi = 0
            for cc in range(4):
                for which, col0 in ((0, cc * 128), (1, 512 + cc * 128)):
                    for tb in range(4):
                        b = bi % 4
                        bi += 1
                        for kc in range(8):
                            op("pe", lambda t, kc=kc: t.matmul(ps[b][:], lhsT=wl[:, kc, col0:col0 + 128],
                                                               rhs=xnT[:, kc, tb * 512:(tb + 1) * 512],
                                                               start=(kc == 0), stop=(kc == 7)),
                               reads=[wl_r] + xnT_r[tb * 4:tb * 4 + 4], writes=[psr[b]], inc=(kc == 7))
                        if which == 0:
                            op("act", lambda a: a.copy(out=lxp[:, 3 + tb * 512:3 + (tb + 1) * 512], in_=ps[b][:]),
                               reads=[psr[b]], writes=[lxp_r])
                        else:
                            op("act", lambda a: a.copy(out=lg[:, tb * 512:(tb + 1) * 512], in_=ps[b][:]),
                               reads=[psr[b]], writes=[lg_r])
                op("act", lambda a: a.activation(out=gbuf[:], in_=lg[:], func=AF.Square), reads=[lg_r], writes=[gbuf_r])
                op("dve", lambda v: v.tensor_scalar(out=cx[:], in0=lxp[:, 3:3 + S], scalar1=lv[:, cc, 3:4], scalar2=lv[:, cc, 4:5],
                                                    op0=ALU.mult, op1=ALU.add),
                   reads=[lxp_r, lv_r], writes=[cx_r])
                for jt in range(3):
                    op("dve", lambda v, jt=jt: v.scalar_tensor_tensor(out=cx[:], in0=lxp[:, jt:jt + S], scalar=lv[:, cc, jt:jt + 1],
                                                                      in1=cx[:], op0=ALU.mult, op1=ALU.add),
                       reads=[lxp_r, lv_r, cx_r], writes=[cx_r])
                op("dve", lambda g: g.tensor_scalar(out=gbuf[:], in0=gbuf[:], scalar1=0.044715, scalar2=1.0, op0=ALU.mult, op1=ALU.add),
                   reads=[gbuf_r], writes=[gbuf_r])
                op("dve", lambda g: g.tensor_tensor(out=gbuf[:], in0=gbuf[:], in1=lg[:], op=ALU.mult), reads=[gbuf_r, lg_r], writes=[gbuf_r])
                for wmat, bcol, dst, dres in ((wabd, 5, b1, b1_r), (wxbd, 6, b4, b4_r)):
                    for tb in range(4):
                        b = 4 + (bi % 4)
                        bi += 1
                        op("pe", lambda t: t.matmul(ps[b][:], lhsT=wmat[:, cc, :], rhs=cx[:, tb * 512:(tb + 1) * 512],
                                                    start=True, stop=True),
                           reads=[wbd_r, cx_r], writes=[psr[b]])
                        op("act", lambda a: a.activation(out=dst[:, tb * 512:(tb + 1) * 512], in_=ps[b][:], func=AF.Sigmoid,
                                                         bias=lv[:, cc, bcol:bcol + 1]),
                           reads=[psr[b], lv_r], writes=[dres])
                op("act", lambda a: a.activation(out=gbuf[:], in_=gbuf[:], func=AF.Sigmoid, scale=1.5957691216057308),
                   reads=[gbuf_r], writes=[gbuf_r])
                op("dve", lambda g: g.tensor_tensor(out=gbuf[:], in0=gbuf[:], in1=lg[:], op=ALU.mult), reads=[gbuf_r, lg_r], writes=[gbuf_r])
                op("act", lambda a: a.activation(out=b2[:], in_=b1[:], func=AF.Exp, scale=lsc[:, cc, 0:1]),
                   reads=[b1_r, lv_r], writes=[b2_r])
                op("act", lambda a: a.activation(out=b3[:], in_=b1[:], func=AF.Exp, scale=lsc[:, cc, 1:2]),
                   reads=[b1_r, lv_r], writes=[b3_r])
                op("act", lambda a: a.activation(out=b3[:], in_=b3[:], func=AF.Sqrt, scale=-1.0, bias=1.0),
                   reads=[b3_r], writes=[b3_r])
                op("dve", lambda g: g.tensor_tensor(out=b4[:], in0=b4[:], in1=cx[:], op=ALU.mult), reads=[b4_r, cx_r], writes=[b4_r])
                op("dve", lambda g: g.tensor_tensor(out=b4[:], in0=b4[:], in1=b3[:], op=ALU.mult), reads=[b4_r, b3_r], writes=[b4_r])
                op("dve", lambda v: v.tensor_tensor_scan(out=b1[:], data0=b2[:], data1=b4[:], initial=0.0,
                                                         op0=ALU.mult, op1=ALU.add),
                   reads=[b2_r, b4_r], writes=[b1_r])
                op("dve", lambda v: v.tensor_tensor(out=b1[:], in0=b1[:], in1=gbuf[:], op=ALU.mult), reads=[b1_r, gbuf_r], writes=[b1_r])
                if cc == 0:
                    op("act", lambda a: a.activation(out=ysq[:], in_=b1[:], func=AF.Square), reads=[b1_r], writes=[ysq_r])
                else:
                    op("act", lambda a: a.activation(out=b2[:], in_=b1[:], func=AF.Square), reads=[b1_r], writes=[b2_r])
                    op("dve", lambda g: g.tensor_tensor(out=ysq[:], in0=ysq[:], in1=b2[:], op=ALU.add), reads=[ysq_r, b2_r], writes=[ysq_r])
                op("dve", lambda v: v.tensor_scalar(out=lyT[:, cc, :], in0=b1[:], scalar1=lv[:, cc, 8:9], scalar2=None, op0=ALU.mult),
                   reads=[b1_r, lv_r], writes=[ly_r])
            kb.barrier()
            for i in range(NT):
                op("pe", lambda t, i=i: t.matmul(ps[0][:, i:i + 1], lhsT=ysq[:, i * 128:(i + 1) * 128], rhs=ones_f[:, :],
                                                 start=True, stop=True),
                   reads=[ysq_r, cres], writes=[psr[0]])
            op("dve", lambda v: v.tensor_copy(out=stat[:, 2, :], in_=ps[0][:, 0:NT]), reads=[psr[0]], writes=[stat_r])
            op("act", lambda a: a.activation(out=stat[:, 1:3, :], in_=stat[:, 1:3, :], func=AF.Sqrt, scale=1.0 / 512, bias=EPS),
               reads=[stat_r], writes=[stat_r])
            op("dve", lambda v: v.reciprocal(out=stat[:, 1:3, :], in_=stat[:, 1:3, :]), reads=[stat_r], writes=[stat_r])
            kb.barrier()

        if stop == "LRU":
            dbg["lyT"] = nc.dram_tensor("dbg_lyT", [128, 4 * S], BF16, kind="ExternalOutput").ap()
            op("sp", lambda q: q.dma_start(out=dbg["lyT"], in_=lyT[:].rearrange("p a b -> p (a b)")), reads=[ly_r], dma=True)
            dbg["stat"] = nc.dram_tensor("dbg_stat", [128, 8 * NT], F32, kind="ExternalOutput").ap()
            op("sp", lambda q: q.dma_start(out=dbg["stat"], in_=stat[:].rearrange("p a b -> p (a b)")), reads=[stat_r], dma=True)
            kb.barrier()
            return nc, dbg

        with contextlib.ExitStack() as st:
            xt = [sbt(st, "xo%d" % j, [128, D], F32) for j in range(2)]
            xt_r = RL(2)
            tmp = [sbt(st, "otmp%d" % j, [128, 512], F32) for j in range(2)]
            tmp_r = RL(2)
            for i in range(NT):
                j = i % 2
                op("sp", lambda q: q.dma_start(out=xt[j][:], in_=x[i * 128:(i + 1) * 128, :]), writes=[xt_r[j]], dma=True)
                for half in range(2):
                    n0 = half * 512
                    ba = 2 * ((2 * i + half) % 2)
                    bb = ba + 1
                    for c in range(4):
                        op("pe", lambda t, c=c: t.matmul(ps[ba][:], lhsT=lyT[:, c, i * 128:(i + 1) * 128], rhs=wo[:, c, n0:n0 + 512],
                                                         start=(c == 0), stop=(c == 3)),
                           reads=[ly_r, wo_r], writes=[psr[ba]], inc=(c == 3))
                    for c in range(4):
                        op("pe", lambda t, c=c: t.matmul(ps[bb][:], lhsT=sbyT[:, c, i * 128:(i + 1) * 128], rhs=wo[:, 4 + c, n0:n0 + 512],
                                                         start=(c == 0), stop=(c == 3)),
                           reads=[sby_r, wo_r], writes=[psr[bb]], inc=(c == 3))
                    op("dve", lambda v: v.scalar_tensor_tensor(out=tmp[half][:], in0=ps[ba][:], scalar=stat[:, 2, i:i + 1],
                                                               in1=xt[j][:, n0:n0 + 512], op0=ALU.mult, op1=ALU.add),
                       reads=[psr[ba], stat_r, xt_r[j]], writes=[tmp_r[half]])
                    op("dve", lambda v: v.scalar_tensor_tensor(out=hres[:, i, n0:n0 + 512], in0=ps[bb][:], scalar=stat[:, 1, i:i + 1],
                                                               in1=tmp[half][:], op0=ALU.mult, op1=ALU.add),
                       reads=[psr[bb], stat_r, tmp_r[half]], writes=[h_r[i]])
            kb.barrier()
        mix2.close()
        mix.close()

        def dump_h(name):
            dbg[name] = nc.dram_tensor("dbg_" + name, [S, D], F32, kind="ExternalOutput").ap()
            for i in range(NT):
                op("sp", lambda q, i=i: q.dma_start(out=dbg[name][i * 128:(i + 1) * 128, :], in_=hres[:, i, :]),
                   reads=[h_r[i]], dma=True)
            kb.barrier()

        if stop == "OUT":
            dump_h("h1")
            return nc, dbg

        moe = contextlib.ExitStack()
        top.enter_context(moe)
        G = sbt(moe, "G", [128, NT, NE], F32)
        dest = sbt(moe, "dest", [128, NT, 4], I32)
        gk = sbt(moe, "gk", [128, NT, 4], F32)
        bupT = sbt(moe, "bupT", [128, 16, NE], F32)
        rt_r = Res()
        G_r, dest_r, gk_r = RL(NT), RL(NT), RL(NT)
        with contextlib.ExitStack() as st:
            GT = [sbt(st, "GT%d" % j, [NE, 128], F32) for j in range(2)]
            GT_r = RL(2)
            bdn = sbt(st, "bdn", [NE, D], F32)
            xn2all = sbt(st, "xn2all", [128, NT, D], BF16)
            xn2all_r = RL(NT)
            wr = sbt(st, "wr", [128, 8, NE], BF16)
            brb = sbt(st, "brb", [128, NE], F32)
            bstg = sbt(st, "bstg", [NE, 2 * D], F32)
            op("pool", lambda g: g.dma_start(out=wr[:], in_=w_router.rearrange("(kc p) n -> p kc n", p=128)), writes=[rt_r], dma=True)
            op("sp", lambda q: q.dma_start(out=brb[:], in_=b_router.partition_broadcast(128)), writes=[rt_r], dma=True)
            op("sp", lambda q: q.dma_start(out=bstg[:], in_=b_up), writes=[rt_r], dma=True)
            op("sp", lambda q: q.dma_start(out=bdn[:], in_=b_down), writes=[rt_r], dma=True)
            for c in range(16):
                op("pe", lambda t, c=c: t.transpose(out=ps[4 + c // 8][:, (c % 8) * NE:(c % 8 + 1) * NE],
                                                    in_=bstg[0:NE, c * 128:(c + 1) * 128], identity=ident_f[0:NE, 0:NE]),
                   reads=[rt_r, cres], writes=[psr[4 + c // 8]])
            for hh in range(2):
                op("dve", lambda v, hh=hh: v.tensor_copy(out=bupT[:, hh * 8:(hh + 1) * 8, :],
                                                         in_=ps[4 + hh][:, 0:8 * NE].rearrange("p (a b) -> p a b", a=8)),
                   reads=[psr[4 + hh]], writes=[rt_r])
            op("dve", lambda v: v.tensor_scalar(out=bupT[:, 8:16, :], in0=bupT[:, 8:16, :], scalar1=1.0, scalar2=None, op0=ALU.add),
               reads=[rt_r], writes=[rt_r])
            mskb = sbt(st, "mskb", [128, NT, NE], BF16)
            mskb_r = RL(NT)
            T2 = []
            for par in range(2):
                T2.append({
                    "lgt": sbt(st, "lgt", [128, NE], F32), "v8": sbt(st, "v8", [128, 8], F32), "i8": sbt(st, "i8", [128, 8], U32),
                    "i8f": sbt(st, "i8f", [128, 8], F32), "nmx": sbt(st, "nmx", [128, 1], F32), "exl": sbt(st, "exl", [128, NE], F32),
                    "msk": sbt(st, "msk", [128, NE], F32), "den": sbt(st, "den", [128, 1], F32), "posf": sbt(st, "posf", [128, NE], F32),
                    "oh3": sbt(st, "oh3", [128, 4, NE], F32), "sc3": sbt(st, "sc3", [128, 4, NE], F32), "pk": sbt(st, "pk", [128, 4], F32),
                    "vk": sbt(st, "vk", [128, 4], F32), "dk": sbt(st, "dk", [128, 4], F32), "w_r": Res()})
            iota3 = iota_e[:].unsqueeze(1).to_broadcast([128, 4, NE])

            def route_tile(i):
                T = T2[i % 2]
                lgt, v8, i8, i8f, nmx, exl, msk, den = (T[k] for k in ("lgt", "v8", "i8", "i8f", "nmx", "exl", "msk", "den"))
                posf, oh3, sc3, pk, vk, dk, w_r = (T[k] for k in ("posf", "oh3", "sc3", "pk", "vk", "dk", "w_r"))
                lb, pbk = 2 + (i % 2), 6 + (i % 2)
                for kc in range(8):
                    op("pe", lambda t, kc=kc: t.matmul(ps[lb][:, 0:NE], lhsT=xnT[:, kc, i * 128:(i + 1) * 128], rhs=wr[:, kc, :],
                                                       start=(kc == 0), stop=(kc == 7)),
                       reads=[xnT_r[i], rt_r], writes=[psr[lb]], inc=(kc == 7))
                    yield
                op("dve", lambda v: v.tensor_tensor(out=lgt[:], in0=ps[lb][:, 0:NE], in1=brb[:], op=ALU.add),
                   reads=[psr[lb], rt_r], writes=[w_r])
                yield
                op("dve", lambda v: v.max(out=v8[:], in_=lgt[:]), reads=[w_r], writes=[w_r])
                yield
                op("dve", lambda v: v.max_index(out=i8[:], in_max=v8[:], in_values=lgt[:]), reads=[w_r], writes=[w_r])
                yield
                op("dve", lambda v: v.tensor_copy(out=i8f[:], in_=i8[:]), reads=[w_r], writes=[w_r])
                yield
                op("dve", lambda v: v.tensor_scalar(out=msk[:], in0=lgt[:], scalar1=v8[:, 3:4], scalar2=None, op0=ALU.is_ge),
                   reads=[w_r], writes=[w_r])
                yield
                op("dve", lambda v: v.tensor_copy(out=mskb[:, i, :], in_=msk[:]), reads=[w_r], writes=[mskb_r[i]])
                yield
                op("dve", lambda v: v.tensor_scalar(out=nmx[:], in0=v8[:, 0:1], scalar1=-1.0, scalar2=None, op0=ALU.mult),
                   reads=[w_r], writes=[w_r])
                yield
                op("act", lambda a: a.activation(out=exl[:], in_=lgt[:], func=AF.Exp, bias=nmx[:, 0:1]), reads=[w_r], writes=[w_r])
                yield
                op("pe", lambda t: t.matmul(ps[pbk][:, 0:NE], lhsT=SU[:], rhs=mskb[:, i, :], start=True, stop=(i == 0)),
                   reads=[mskb_r[i], cres], writes=[psr[pbk]], inc=(i == 0))
                yield
                for i2 in range(i):
                    op("pe", lambda t, i2=i2: t.matmul(ps[pbk][:, 0:NE], lhsT=ones_bf[:], rhs=mskb[:, i2, :], start=False,
                                                       stop=(i2 == i - 1)),
                       reads=[mskb_r[i2], cres], writes=[psr[pbk]], inc=(i2 == i - 1))
                    yield
                op("dve", lambda v: v.tensor_copy(out=posf[:], in_=ps[pbk][:, 0:NE]), reads=[psr[pbk]], writes=[w_r])
                yield
                op("dve", lambda v: v.tensor_tensor(out=exl[:], in0=exl[:], in1=msk[:], op=ALU.mult), reads=[w_r], writes=[w_r])
                yield
                op("dve", lambda v: v.reduce_sum(out=den[:], in_=exl[:], axis=mybir.AxisListType.X), reads=[w_r], writes=[w_r])
                yield
                op("dve", lambda v: v.reciprocal(out=den[:], in_=den[:]), reads=[w_r], writes=[w_r])
                yield
                op("dve", lambda v: v.tensor_scalar(out=G[:, i, :], in0=exl[:], scalar1=den[:, 0:1], scalar2=None, op0=ALU.mult),
                   reads=[w_r], writes=[G_r[i]])
                yield
                op("dve", lambda v: v.tensor_tensor(out=oh3[:], in0=iota3, in1=i8f[:, 0:4].unsqueeze(2).to_broadcast([128, 4, NE]),
                                                    op=ALU.is_equal),
                   reads=[w_r, cres], writes=[w_r])
                yield
                op("dve", lambda v: v.tensor_tensor(out=sc3[:], in0=oh3[:], in1=posf[:].unsqueeze(1).to_broadcast([128, 4, NE]), op=ALU.mult),
                   reads=[w_r], writes=[w_r])
                yield
                op("dve", lambda v: v.reduce_sum(out=pk[:], in_=sc3[:], axis=mybir.AxisListType.X), reads=[w_r], writes=[w_r])
                yield
                op("dve", lambda v: v.tensor_tensor(out=sc3[:], in0=oh3[:], in1=G[:, i, :].unsqueeze(1).to_broadcast([128, 4, NE]), op=ALU.mult),
                   reads=[w_r, G_r[i]], writes=[w_r])
                yield
                op("dve", lambda v: v.reduce_sum(out=gk[:, i, :], in_=sc3[:], axis=mybir.AxisListType.X), reads=[w_r], writes=[gk_r[i]])
                yield
                op("dve", lambda v: v.tensor_scalar(out=vk[:], in0=pk[:], scalar1=float(CAP), scalar2=None, op0=ALU.is_lt), reads=[w_r], writes=[w_r])
                yield
                op("dve", lambda v: v.tensor_tensor(out=gk[:, i, :], in0=gk[:, i, :], in1=vk[:], op=ALU.mult), reads=[w_r, gk_r[i]], writes=[gk_r[i]])
                yield
                op("dve", lambda v: v.scalar_tensor_tensor(out=dk[:], in0=i8f[:, 0:4], scalar=float(CAP), in1=pk[:], op0=ALU.mult, op1=ALU.add),
                   reads=[w_r], writes=[w_r])
                yield
                op("dve", lambda v: v.tensor_scalar(out=vk[:], in0=vk[:], scalar1=-1.0e6, scalar2=1.0e6, op0=ALU.mult, op1=ALU.add), reads=[w_r], writes=[w_r])
                yield
                op("dve", lambda v: v.tensor_tensor(out=dk[:], in0=dk[:], in1=vk[:], op=ALU.add), reads=[w_r], writes=[w_r])
                yield
                op("dve", lambda v: v.tensor_copy(out=dest[:, i, :], in_=dk[:]), reads=[w_r], writes=[dest_r[i]])
                yield
                for k in range(4):
                    op("pool", lambda g, k=k: g.indirect_dma_start(
                        out=xs_d, out_offset=bass.IndirectOffsetOnAxis(ap=dest[:, i, k:k + 1], axis=0),
                        in_=xn2all[:, i, :], in_offset=None, bounds_check=breg, oob_is_err=False),
                        reads=[dest_r[i], xn2all_r[i]], dma=True)
                    yield
                jj = i % 2
                op("pe", lambda t: t.transpose(out=ps[4 + jj][0:NE, 0:128], in_=G[:, i, :], identity=ident_f[:]),
                   reads=[G_r[i], cres], writes=[psr[4 + jj]])
                yield
                op("act", lambda a: a.copy(out=GT[jj][:], in_=ps[4 + jj][0:NE, 0:128]), reads=[psr[4 + jj]], writes=[GT_r[jj]])
                yield
                for half in range(2):
                    b = lb
                    op("pe", lambda t: t.matmul(ps[b][:], lhsT=GT[jj][:], rhs=bdn[:, half * 512:(half + 1) * 512], start=True, stop=True),
                       reads=[GT_r[jj], rt_r], writes=[psr[b]])
                    yield
                    op("dve", lambda v: v.tensor_tensor(out=hres[:, i, half * 512:(half + 1) * 512], in0=ps[b][:],
                                                        in1=hres[:, i, half * 512:(half + 1) * 512], op=ALU.add),
                       reads=[psr[b], h_r[i]], writes=[h_r[i]])
                    yield

            def route_pair(i0):
                gens = [route_tile(i) for i in (i0, i0 + 1)]
                alive = [True, True]
                while any(alive):
                    for gi, g in enumerate(gens):
                        if alive[gi]:
                            try:
                                next(g)
                            except StopIteration:
                                alive[gi] = False

            def hook5(i):
                if i >= 2 and i % 2 == 0:
                    route_pair(i - 2)

            def src5(i):
                return hres[:, i, :], [h_r[i]]
            norm_T(src5, 1, tok_out=lambda i: (xn2all[:, i, :], [xn2all_r[i]]), statrow=3, per_tile=hook5)
            kb.barrier()

        if stop == "ROUTE":
            dbg["G"] = nc.dram_tensor("dbg_G", [128, NT * NE], F32, kind="ExternalOutput").ap()
            op("sp", lambda q: q.dma_start(out=dbg["G"], in_=G[:].rearrange("p a b -> p (a b)")), reads=[rt_r], dma=True)
            dbg["dest"] = nc.dram_tensor("dbg_dest", [128, NT * 4], I32, kind="ExternalOutput").ap()
            op("sp", lambda q: q.dma_start(out=dbg["dest"], in_=dest[:].rearrange("p a b -> p (a b)")), reads=[rt_r], dma=True)
            dbg["gk"] = nc.dram_tensor("dbg_gk", [128, NT * 4], F32, kind="ExternalOutput").ap()
            op("sp", lambda q: q.dma_start(out=dbg["gk"], in_=gk[:].rearrange("p a b -> p (a b)")), reads=[rt_r], dma=True)
            kb.barrier()
            return nc, dbg

        with contextlib.ExitStack() as st:
            NR = 5
            NSTG = 8
            for t_ in range(10, 16):
                op("sp", lambda q, t_=t_: q.dma_start(out=hsp_d[(t_ - 10) * 128:(t_ - 9) * 128, :], in_=hres[:, t_, :]),
                   reads=[h_r[t_]], dma=True)
            kb.barrier()
            ring = [sbt(st, "ring%d" % j, [128, 8, D], BF16) for j in range(NR)]
            ring_r = [RL(8) for _ in range(NR)]
            xflat = xnT[:].rearrange("p a b -> p (a b)")
            xe = xflat[:, 0:4096].rearrange("p (a b) -> p a b", a=NA)
            xe_r = Res()
            xeT = xflat[:, 4096:8192].rearrange("p (a b) -> p a b", a=8)
            xeT_r = Res()
            actT = xflat[:, 8192:12288].rearrange("p (a b) -> p a b", a=8)
            actT_r = RL(8)
            stg = [sbt(st, "stg%d" % j, [128, D], F32) for j in range(2)]
            stg = [t[:] for t in stg] + [xflat[:, 12288 + j * 2048:12288 + (j + 1) * 2048].bitcast(F32) for j in range(2)]
            stg += [hres[:, 12 + j, :] for j in range(4)]
            stg_r = RL(NSTG)
            gc = sbt(st, "gc", [128, CAP], F32)
            sg = sbt(st, "sg", [128, CAP], F32)
            uc = sbt(st, "uc", [128, CAP], F32)
            gc_r, sg_r, uc_r = Res(), Res(), Res()
            yst = [gb[j][:] for j in range(2)] + [hres[:, 10 + j, :] for j in range(2)]
            yst_r = RL(4)
            w_up_v = w_up.rearrange("e (kc p) n -> e p kc n", p=128)
            w_dn_v = w_down.rearrange("e (kc p) n -> e p kc n", p=128)
            ys_res = Res()
            npiece = [0]

            def load_piece(mi, kc):
                e_, part = divmod(mi, 3)
                if e_ >= NE:
                    return
                j = mi % NR
                n = npiece[0]
                npiece[0] += 1
                sj = n % NSTG
                src = w_up_v[e_, :, kc, part * D:(part + 1) * D] if part < 2 else w_dn_v[e_, :, kc, :]
                op("sp", lambda q: q.dma_start(out=stg[sj], in_=src), writes=[stg_r[sj]], dma=True)
                op("act", lambda a: a.copy(out=ring[j][:, kc, :], in_=stg[sj]), reads=[stg_r[sj]], writes=[ring_r[j][kc]])

            def load_xe(e_):
                op("pool", lambda q: q.dma_start(out=xe[:, 0:3, :], in_=xs_d[e_ * CAP:e_ * CAP + 384, :].rearrange("(a p) n -> p a n", p=128)),
                   writes=[xe_r], dma=True)
                op("pool", lambda q: q.dma_start(out=xe[0:LAST, 3, :], in_=xs_d[e_ * CAP + 384:(e_ + 1) * CAP, :]),
                   writes=[xe_r], dma=True)

            load_xe(0)
            for mi in range(2):
                for kc in range(8):
                    load_piece(mi, kc)
            for e in range(NE):
                jg, ju, jd = (3 * e) % NR, (3 * e + 1) % NR, (3 * e + 2) % NR
                for a_ in range(NA):
                    b = a_ % 2
                    pb = ps[b][:].bitcast(BF16)
                    rows = 128
                    for kc in range(8):
                        op("pe", lambda t, kc=kc: t.transpose(out=pb[:, kc * 128:kc * 128 + rows], in_=xe[0:rows, a_, kc * 128:(kc + 1) * 128],
                                                              identity=ident_bf[0:rows, 0:rows]),
                           reads=[xe_r, cres], writes=[psr[b]], inc=(kc == 7))
                    o_ap = xeT[:, :, a_ * 128:a_ * 128 + rows]
                    i_ap = pb.rearrange("p (a b) -> p a b", a=8)[:, :, 0:rows]
                    op("dve", lambda v: v.tensor_copy(out=o_ap, in_=i_ap), reads=[psr[b]], writes=[xeT_r])
                if e + 1 < NE:
                    load_xe(e + 1)
                for nch in range(8):
                    bg = 2 + (nch % 2) * 2
                    bu = bg + 1
                    for kc in range(8):
                        op("pe", lambda t, kc=kc: t.matmul(ps[bg][:, 0:CAP], lhsT=ring[jg][:, kc, nch * 128:(nch + 1) * 128], rhs=xeT[:, kc, 0:CAP],
                                                           start=(kc == 0), stop=(kc == 7)),
                           reads=[ring_r[jg][kc], xeT_r], writes=[psr[bg]], inc=(kc == 7))
                    for kc in range(8):
                        op("pe", lambda t, kc=kc: t.matmul(ps[bu][:, 0:CAP], lhsT=ring[ju][:, kc, nch * 128:(nch + 1) * 128], rhs=xeT[:, kc, 0:CAP],
                                                           start=(kc == 0), stop=(kc == 7)),
                           reads=[ring_r[ju][kc], xeT_r], writes=[psr[bu]], inc=(kc == 7))
                    op("dve", lambda v: v.tensor_scalar(out=gc[:], in0=ps[bg][:, 0:CAP], scalar1=bupT[:, nch, e:e + 1], scalar2=7.0,
                                                        op0=ALU.add, op1=ALU.min),
                       reads=[psr[bg], rt_r], writes=[gc_r])
                    op("act", lambda a: a.activation(out=sg[:], in_=gc[:], func=AF.Sigmoid, scale=1.702),
                       reads=[gc_r], writes=[sg_r])
                    if e == 0:
                        load_piece(2, nch)
                    load_piece(3 * e + 3 + (nch // 4), (2 * nch) % 8)
                    load_piece(3 * e + 3 + (nch // 4), (2 * nch + 1) % 8)
                    op("dve", lambda v: v.tensor_scalar(out=uc[:], in0=ps[bu][:, 0:CAP], scalar1=bupT[:, 8 + nch, e:e + 1], scalar2=8.0,
                                                        op0=ALU.add, op1=ALU.min),
                       reads=[psr[bu], rt_r], writes=[uc_r])
                    op("dve", lambda v: v.tensor_tensor(out=sg[:], in0=gc[:], in1=sg[:], op=ALU.mult),
                       reads=[gc_r, sg_r], writes=[sg_r])
                    op("dve", lambda v: v.scalar_tensor_tensor(out=actT[:, nch, 0:CAP], in0=uc[:], scalar=-6.0, in1=sg[:],
                                                               op0=ALU.max, op1=ALU.mult),
                       reads=[sg_r, uc_r], writes=[actT_r[nch]])
                gi = 0
                for a_ in range(NA):
                    yp = (e * NA + a_) % 4
                    rows = 128
                    for half in range(2):
                        b = 6 + half
                        for nch in range(8):
                            op("pe", lambda t, nch=nch: t.matmul(ps[b][0:rows, :], lhsT=actT[:, nch, a_ * 128:a_ * 128 + rows],
                                                                 rhs=ring[jd][:, nch, half * 512:(half + 1) * 512],
                                                                 start=(nch == 0), stop=(nch == 7)),
                               reads=[actT_r[nch], ring_r[jd][nch]], writes=[psr[b]], inc=(nch == 7))
                        op("dve", lambda v: v.tensor_copy(out=yst[yp][0:rows, half * 512:(half + 1) * 512], in_=ps[b][0:rows, :]),
                           reads=[psr[b]], writes=[yst_r[yp]])
                        load_piece(3 * e + 5, gi)
                        gi += 1
                    r0 = e * CAP + a_ * 128
                    srows = 128 if a_ < 3 else LAST
                    op("pool", lambda q: q.dma_start(out=ys_d[r0:r0 + srows, :], in_=yst[yp][0:srows, :]), reads=[yst_r[yp]], dma=True)
            kb.barrier()

        with contextlib.ExitStack() as st:
            NYG = 8
            yg = [sbt(st, "yg%d" % j, [128, D], F32) for j in range(NYG)]
            yg_r = RL(NYG)
            for j in range(NYG):
                op("dve", lambda v, j=j: v.memset(yg[j][:], 0.0), writes=[yg_r[j]])
            load_gain(2)
            load_gain(3)
            junk = sbt(st, "fjunk", [128, D], BF16)
            junk_r = Res()
            xnb = [sbt(st, "fxnb%d" % j, [128, D], BF16) for j in range(2)]
            xnb_r = RL(2)
            fsq = sbt(st, "fsq", [128, 2 * NT], F32)
            wg = sbt(st, "wg", [128, 8, D], BF16)
            wp = sbt(st, "wp", [128, 2, D], BF16)
            wg_r = RL(10)
            wstg = [sbt(st, "wstg%d" % j, [128, D], F32) for j in range(2)]
            wstg_r = RL(2)
            wgv = w_ple_gate.rearrange("(kc p) n -> p kc n", p=128)
            wpv = w_ple.rearrange("(kc p) n -> p kc n", p=128)
            for n in range(10):
                src = wgv[:, n, :] if n < 8 else wpv[:, n - 8, :]
                dstw = wg[:, n, :] if n < 8 else wp[:, n - 8, :]
                op("sp", lambda q: q.dma_start(out=wstg[n % 2][:], in_=src), writes=[wstg_r[n % 2]], dma=True)
                op("act", lambda a: a.copy(out=dstw, in_=wstg[n % 2][:]), reads=[wstg_r[n % 2]], writes=[wg_r[n]])
            for t_ in range(10, 16):
                op("sp", lambda q, t_=t_: q.dma_start(out=hres[:, t_, :], in_=hsp_d[(t_ - 10) * 128:(t_ - 9) * 128, :]),
                   writes=[h_r[t_]], dma=True)
            pf = [sbt(st, "pf%d" % j, [128, 256], F32) for j in range(2)]
            pf_r = RL(2)
            pt = [sbt(st, "pt%d" % j, [128, 256], BF16) for j in range(2)]
            pt_r = RL(2)
            pT = [sbt(st, "pT%d" % j, [128, 2, 128], BF16) for j in range(2)]
            pT_r = RL(2)
            sgt = [sbt(st, "sgt%d" % j, [128, 512], F32) for j in range(2)]
            sgt_r = RL(2)
            ot = [sbt(st, "ot%d" % j, [128, D], F32) for j in range(2)]
            ot_r = RL(2)

            def combine(i):
                for k in range(4):
                    y = (4 * i + k) % NYG
                    op("pool", lambda g: g.indirect_dma_start(
                        out=yg[y][:, :], out_offset=None, in_=ys_d,
                        in_offset=bass.IndirectOffsetOnAxis(ap=dest[:, i, k:k + 1], axis=0),
                        bounds_check=breg, oob_is_err=False),
                        reads=[dest_r[i], ys_res], writes=[yg_r[y]], dma=True)
                    op("dve", lambda v: v.scalar_tensor_tensor(out=hres[:, i, :], in0=yg[y][:], scalar=gk[:, i, k:k + 1],
                                                               in1=hres[:, i, :], op0=ALU.mult, op1=ALU.add),
                       reads=[yg_r[y], gk_r[i], h_r[i]], writes=[h_r[i]])

            sA_r, sC_r = RL(NT), RL(NT)

            def pleA(i):
                j = i % 2
                ssc = stat[:, 4, i:i + 1]
                op("act", lambda a: a.activation(out=junk[:], in_=hres[:, i, :], func=AF.Square, accum_out=ssc),
                   reads=[h_r[i]], writes=[junk_r, sA_r[i]])
                op("act", lambda a: a.activation(out=fsq[:, i:i + 1], in_=ssc, func=AF.Sqrt, scale=1.0 / D, bias=EPS),
                   reads=[sA_r[i]], writes=[sA_r[i]])
                op("dve", lambda v: v.reciprocal(out=ssc, in_=fsq[:, i:i + 1]), reads=[sA_r[i]], writes=[sA_r[i]])
                op("dve", lambda v: v.scalar_tensor_tensor(out=xnb[j][:], in0=hres[:, i, :], scalar=ssc, in1=gb[0][:],
                                                           op0=ALU.mult, op1=ALU.mult),
                   reads=[h_r[i], sA_r[i], gb_r[0]], writes=[xnb_r[j]])
                op("sp", lambda q: q.dma_start(out=pf[j][:], in_=pin[i * 128:(i + 1) * 128, :]), writes=[pf_r[j]], dma=True)
                op("act", lambda a: a.copy(out=pt[j][:], in_=pf[j][:]), reads=[pf_r[j]], writes=[pt_r[j]])

            def pleA2(i):
                j = i % 2
                pb = ps[j][:].bitcast(BF16)
                for kc in range(8):
                    op("pe", lambda t, kc=kc: t.transpose(out=pb[:, kc * 128:(kc + 1) * 128], in_=xnb[j][:, kc * 128:(kc + 1) * 128],
                                                          identity=ident_bf[:]),
                       reads=[xnb_r[j], cres], writes=[psr[j]], inc=(kc == 7))
                op("act", lambda a: a.copy(out=xnT[:, :, i * 128:(i + 1) * 128], in_=pb.rearrange("p (a b) -> p a b", a=8)),
                   reads=[psr[j]], writes=[xnT_r[i]])
                pb2 = ps[2 + j][:].bitcast(BF16)
                for kc in range(2):
                    op("pe", lambda t, kc=kc: t.transpose(out=pb2[:, kc * 128:(kc + 1) * 128], in_=pt[j][:, kc * 128:(kc + 1) * 128],
                                                          identity=ident_bf[:]),
                       reads=[pt_r[j], cres], writes=[psr[2 + j]], inc=(kc == 1))
                op("act", lambda a: a.copy(out=pT[j][:], in_=pb2[:, 0:256].rearrange("p (a b) -> p a b", a=2)),
                   reads=[psr[2 + j]], writes=[pT_r[j]])

            def pleB(i):
                j = i % 2
                for half in range(2):
                    n0 = half * 512
                    bg = 4 + 2 * half
                    bp = bg + 1
                    for kc in range(8):
                        op("pe", lambda t, kc=kc: t.matmul(ps[bg][:], lhsT=xnT[:, kc, i * 128:(i + 1) * 128], rhs=wg[:, kc, n0:n0 + 512],
                                                           start=(kc == 0), stop=(kc == 7)),
                           reads=[xnT_r[i], wg_r[kc]], writes=[psr[bg]], inc=(kc == 7))
                    for kc in range(2):
                        op("pe", lambda t, kc=kc: t.matmul(ps[bp][:], lhsT=pT[j][:, kc, :], rhs=wp[:, kc, n0:n0 + 512],
                                                           start=(kc == 0), stop=(kc == 1)),
                           reads=[pT_r[j], wg_r[8 + kc]], writes=[psr[bp]], inc=(kc == 1))

            def pleB2(i):
                for half in range(2):
                    n0 = half * 512
                    bg = 4 + 2 * half
                    bp = bg + 1
                    op("act", lambda a: a.activation(out=sgt[half][:], in_=ps[bg][:], func=AF.Sigmoid), reads=[psr[bg]], writes=[sgt_r[half]])
                    op("dve", lambda v: v.tensor_tensor(out=sgt[half][:], in0=ps[bp][:], in1=sgt[half][:], op=ALU.mult),
                       reads=[psr[bp], sgt_r[half]], writes=[sgt_r[half]])
                    op("dve", lambda v: v.tensor_tensor(out=hres[:, i, n0:n0 + 512], in0=hres[:, i, n0:n0 + 512], in1=sgt[half][:], op=ALU.add),
                       reads=[sgt_r[half], h_r[i]], writes=[h_r[i]])

            def pleC(i):
                j = i % 2
                ssf = stat[:, 5, i:i + 1]
                op("act", lambda a: a.activation(out=junk[:], in_=hres[:, i, :], func=AF.Square, accum_out=ssf),
                   reads=[h_r[i]], writes=[junk_r, sC_r[i]])
                op("act", lambda a: a.activation(out=fsq[:, NT + i:NT + i + 1], in_=ssf, func=AF.Sqrt, scale=1.0 / D, bias=EPS),
                   reads=[sC_r[i]], writes=[sC_r[i]])
                op("dve", lambda v: v.reciprocal(out=ssf, in_=fsq[:, NT + i:NT + i + 1]), reads=[sC_r[i]], writes=[sC_r[i]])
                op("dve", lambda v: v.scalar_tensor_tensor(out=ot[j][:], in0=hres[:, i, :], scalar=ssf, in1=gb[1][:], op0=ALU.mult, op1=ALU.mult),
                   reads=[h_r[i], sC_r[i], gb_r[1]], writes=[ot_r[j]])
                op("sp", lambda q: q.dma_start(out=out[i * 128:(i + 1) * 128, :], in_=ot[j][:]), reads=[ot_r[j]], dma=True)

            combine(0)
            for r in range(NT + 2):
                if 0 <= r - 1 < NT:
                    pleB(r - 1)
                if r < NT:
                    pleA(r)
                if r + 1 < NT:
                    combine(r + 1)
                if 0 <= r - 1 < NT:
                    pleB2(r - 1)
                if r < NT:
                    pleA2(r)
                if 0 <= r - 2 < NT:
                    pleC(r - 2)
            kb.barrier()
        moe.close()
    return nc, dbg


_CACHE = {}


def _prep(inputs, b):
    f = lambda a: np.ascontiguousarray(np.asarray(a, dtype=np.float32))
    g = inputs
    lruvec = np.concatenate([g["conv_w"][0], g["conv_b"][0][None], g["lru_b_a"][0][None], g["lru_b_x"][0][None],
                             g["lru_lambda"][0][None], g["lru_out_g"][0][None]], axis=0)
    gains = np.stack([g["mix_norm_g"][0], g["ffn_norm_g"][0], g["ple_norm_g"][0], g["final_norm_g"]], axis=0)
    return {
        "x": f(g["x"][b]), "p": f(g["p"][0, b]), "gains": f(gains), "w_in": f(g["w_in"][0]), "lruvec": f(lruvec),
        "lru_w_a": f(g["lru_w_a"][0]), "lru_w_x": f(g["lru_w_x"][0]), "sb_out_g": f(g["sb_out_g"][0][None]),
        "w_out": f(g["w_out"][0]), "w_router": f(g["w_router"][0]), "b_router": f(g["b_router"][0][None]),
        "w_up": f(g["w_up"][0]), "b_up": f(g["b_up"][0]), "w_down": f(g["w_down"][0]), "b_down": f(g["b_down"][0]),
        "w_ple_gate": f(g["w_ple_gate"][0]), "w_ple": f(g["w_ple"][0]),
    }


def kernel(**inputs):
    inputs = {k: np.asarray(v) for k, v in inputs.items()}
    if "nc" not in _CACHE:
        _CACHE["nc"] = build("FULL")[0]
    nc = _CACHE["nc"]
    shared = _prep(inputs, 0)
    in_maps = []
    for b in range(8):
        m = dict(shared)
        m["x"] = np.ascontiguousarray(inputs["x"][b], dtype=np.float32)
        m["p"] = np.ascontiguousarray(inputs["p"][0, b], dtype=np.float32)
        in_maps.append(m)
    res = run_bass_kernel_spmd(nc, in_maps, core_ids=list(range(8)))
    return np.stack([np.asarray(r["out"], dtype=np.float32) for r in res.results], axis=0)
```

```python
import contextlib
import numpy as np
import concourse.bass as bass
import concourse.mybir as mybir
from concourse.bass_utils import run_bass_kernel_spmd

F32 = mybir.dt.float32
BF16 = mybir.dt.bfloat16
I32 = mybir.dt.int32
U32 = mybir.dt.uint32
AF = mybir.ActivationFunctionType
ALU = mybir.AluOpType

S = 2048
D = 1024
NT = 16
NE = 32
CAP = 448
NA = 4
LAST = CAP - 384
NSLOT = NE * CAP
EPS = 1e-6
NEG = -30000.0


class Res:
    __slots__ = ("w", "r", "const")

    def __init__(self, const=False):
        self.w = None
        self.r = {}
        self.const = const


def RL(n):
    return [Res() for _ in range(n)]


class KB:
    NDS = 48
    NPRE = 40

    def __init__(self, nc):
        self.nc = nc
        self.es = contextlib.ExitStack()
        self.E = {}
        for nm, h in (("pe", nc.tensor), ("act", nc.scalar), ("dve", nc.vector),
                      ("pool", nc.gpsimd), ("sp", nc.sync)):
            self.E[nm] = {"h": h, "sem": self.es.enter_context(nc.semaphore("c_" + nm)),
                          "n": 0, "seen": {}, "hist": []}
        self.ds = [[self.es.enter_context(nc.semaphore("d%d" % i)), 0] for i in range(self.NDS + self.NPRE)]
        self.dhist = {}
        self.dn = {"sp": 0, "pool": 0, "act": 0}
        self.drange = {"sp": (0, 28), "pool": (28, 44), "act": (44, 48)}
        self.pn = 0
        self.ninst = 0

    def _wait(self, e, ev):
        E = self.E[e]
        kind, key, val = ev
        if kind == "e":
            if key == e and e == "pe":
                return
            sem = self.E[key]["sem"]
            hist = self.E[key]["hist"]
            snap = hist[val - 1] if val - 1 < len(hist) else {}
        else:
            sem = self.ds[key][0]
            snap = self.dhist.get((key, val), {})
        k = (kind, key)
        if E["seen"].get(k, 0) >= val:
            return
        E["h"].wait_ge(sem, val)
        new = dict(E["seen"])
        for kk, vv in snap.items():
            if new.get(kk, 0) < vv:
                new[kk] = vv
        new[k] = val
        E["seen"] = new

    def op(self, e, fn, reads=(), writes=(), dma=False, inc=True, pre=False):
        E = self.E[e]
        evs = []
        for r in reads:
            if r.w is not None:
                evs.append(r.w)
        for w in writes:
            if w.w is not None:
                evs.append(w.w)
            for (kind, key), val in w.r.items():
                evs.append((kind, key, val))
        if dma:
            if pre:
                i = self.NDS + self.pn
                self.pn = (self.pn + 1) % self.NPRE
            else:
                lo, hi = self.drange[e]
                i = lo + self.dn[e]
                self.dn[e] = (self.dn[e] + 1) % (hi - lo)
            if self.ds[i][1] > 0:
                evs.append(("d", i, self.ds[i][1]))
        for ev in evs:
            self._wait(e, ev)
        inst = fn(E["h"])
        self.ninst += 1
        if dma:
            self.ds[i][1] += 16
            inst.then_inc(self.ds[i][0], 16)
            me = ("d", i, self.ds[i][1])
            self.dhist[(i, self.ds[i][1])] = E["seen"]
        elif inc:
            E["n"] += 1
            inst.then_inc(E["sem"], 1)
            me = ("e", e, E["n"])
            E["hist"].append(E["seen"])
        else:
            me = ("e", e, E["n"] + 1)
        for r in reads:
            if not r.const:
                k = (me[0], me[1])
                if r.r.get(k, 0) < me[2]:
                    r.r[k] = me[2]
        for w in writes:
            w.w = me
            w.r = {}
        return inst

    def barrier(self):
        evs = [("e", nm, E["n"]) for nm, E in self.E.items() if E["n"] > 0]
        evs += [("d", i, v) for i, (s, v) in enumerate(self.ds) if v > 0 and i < self.NDS]
        for e in self.E:
            for ev in evs:
                self._wait(e, ev)


def build(stop="FULL"):
    nc = bass.Bass("TRN2", target_bir_lowering=False)

    def din(name, shape, dtype=F32):
        return nc.dram_tensor(name, shape, dtype, kind="ExternalInput").ap()

    x = din("x", [S, D])
    pin = din("p", [S, 256])
    gains = din("gains", [4, D])
    w_in = din("w_in", [D, 2560])
    lruvec = din("lruvec", [9, 512])
    lru_w_a = din("lru_w_a", [8, 64, 64])
    lru_w_x = din("lru_w_x", [8, 64, 64])
    sb_out_g = din("sb_out_g", [1, 512])
    w_out = din("w_out", [D, D])
    w_router = din("w_router", [D, NE])
    b_router = din("b_router", [1, NE])
    w_up = din("w_up", [NE, D, 2 * D])
    b_up = din("b_up", [NE, 2 * D])
    w_down = din("w_down", [NE, D, D])
    b_down = din("b_down", [NE, D])
    w_ple_gate = din("w_ple_gate", [D, D])
    w_ple = din("w_ple", [256, D])
    out = nc.dram_tensor("out", [S, D], F32, kind="ExternalOutput").ap()
    xs_d = nc.dram_tensor("xs_scratch", [NSLOT, D], BF16, kind="Internal").ap()
    ys_d = nc.dram_tensor("ys_scratch", [NSLOT, D], F32, kind="Internal").ap()
    hsp_d = nc.dram_tensor("h_spill", [6 * 128, D], F32, kind="Internal").ap()
    dbg = {}

    kb = KB(nc)
    op = kb.op

    with kb.es:
        top = kb.es

        uniq = [0]

        def sbt(stack, name, shape, dtype):
            uniq[0] += 1
            return stack.enter_context(nc.sbuf_tensor("%s_%d" % (name, uniq[0]), shape, dtype))

        ps = [top.enter_context(nc.psum_tensor("ps%d" % i, [128, 512], F32)) for i in range(8)]
        psr = RL(8)

        cres = Res(const=True)
        dmat = sbt(top, "dmat", [128, 128], I32)
        ident_bf = sbt(top, "ident_bf", [128, 128], BF16)
        ident_f = sbt(top, "ident_f", [128, 128], F32)
        NU = sbt(top, "NU", [128, 128], BF16)
        NL = sbt(top, "NL", [128, 128], BF16)
        maskneg = sbt(top, "maskneg", [128, 128], BF16)
        SU = sbt(top, "SU", [128, 128], BF16)
        ones_bf = sbt(top, "ones_bf", [128, 128], BF16)
        zeros_bf = sbt(top, "zeros_bf", [128, 128], BF16)
        ones_f = sbt(top, "ones_f", [128, 1], F32)
        iota_e = sbt(top, "iota_e", [128, NE], F32)
        iota_ei = sbt(top, "iota_ei", [128, NE], I32)
        gb = [sbt(top, "gb%d" % i, [128, D], F32) for i in range(2)]
        gb_r = RL(2)
        hres = sbt(top, "hres", [128, NT, D], F32)
        h_r = RL(NT)
        xnT = sbt(top, "xnT", [128, 8, S], BF16)
        xnT_r = RL(NT)
        stat = sbt(top, "stat", [128, 8, NT], F32)
        stat_r = Res()

        op("pool", lambda g: g.iota(dmat[:], pattern=[[1, 128]], base=0, channel_multiplier=-1), writes=[cres])
        op("pool", lambda g: g.iota(iota_ei[:], pattern=[[1, NE]], base=0, channel_multiplier=0), writes=[cres])

        def cmat(dst, cmp_op, mul):
            op("dve", lambda v: v.tensor_scalar(out=dst[:], in0=dmat[:], scalar1=0.0, scalar2=mul,
                                                 op0=cmp_op, op1=ALU.mult), reads=[cres], writes=[cres])
        cmat(ident_bf, ALU.is_equal, 1.0)
        cmat(ident_f, ALU.is_equal, 1.0)
        cmat(NU, ALU.is_lt, -1.0)
        cmat(NL, ALU.is_ge, -1.0)
        cmat(maskneg, ALU.is_le, NEG)
        cmat(SU, ALU.is_gt, 1.0)
        op("dve", lambda v: v.memset(ones_bf[:], 1.0), writes=[cres])
        op("dve", lambda v: v.memset(zeros_bf[:], 0.0), writes=[cres])
        op("dve", lambda v: v.memset(ones_f[:], 1.0), writes=[cres])
        op("dve", lambda v: v.tensor_copy(out=iota_e[:], in_=iota_ei[:]), reads=[cres], writes=[cres])
        def load_gain(gidx):
            op("sp", lambda q: q.dma_start(out=gb[gidx % 2][:], in_=gains[gidx:gidx + 1, :].partition_broadcast(128)),
               writes=[gb_r[gidx % 2]], dma=True)

        breg = nc.gpsimd.to_reg(NSLOT - 1)

        def norm_T(src_fn, gidx, tok_out=None, banks=(0, 1), statrow=0, per_tile=None):
            load_gain(gidx)
            with contextlib.ExitStack() as st:
                junk = sbt(st, "nt_junk", [128, D], BF16)
                junk_r = Res()
                xnb = [sbt(st, "nt_xnb%d" % j, [128, D], BF16) for j in range(2)]
                xnb_r = RL(2)
                sq = sbt(st, "nt_sq", [128, NT], F32)
                sr = RL(NT)
                pend = []

                def flush():
                    while pend:
                        pi, pbank = pend.pop(0)
                        pbv = ps[pbank][:].bitcast(BF16)
                        o_ap = xnT[:, :, pi * 128:(pi + 1) * 128]
                        i_ap = pbv.rearrange("p (a b) -> p a b", a=8)
                        if pi % 2 == 0:
                            op("act", lambda a: a.copy(out=o_ap, in_=i_ap), reads=[psr[pbank]], writes=[xnT_r[pi]])
                        else:
                            op("dve", lambda v: v.tensor_copy(out=o_ap, in_=i_ap), reads=[psr[pbank]], writes=[xnT_r[pi]])

                for i in range(NT):
                    src, sres = src_fn(i)
                    ssc = stat[:, statrow, i:i + 1]
                    op("act", lambda a: a.activation(out=junk[:], in_=src, func=AF.Square, accum_out=ssc),
                       reads=sres + [cres], writes=[junk_r, sr[i]])
                    op("act", lambda a: a.activation(out=sq[:, i:i + 1], in_=ssc, func=AF.Sqrt,
                                                     scale=1.0 / D, bias=EPS),
                       reads=[sr[i]], writes=[sr[i]])
                    op("dve", lambda v: v.reciprocal(out=ssc, in_=sq[:, i:i + 1]), reads=[sr[i]], writes=[sr[i]])
                    if tok_out is not None:
                        dst, dres = tok_out(i)
                    else:
                        dst, dres = xnb[i % 2][:], [xnb_r[i % 2]]
                    op("dve", lambda v: v.scalar_tensor_tensor(out=dst, in0=src, scalar=ssc, in1=gb[gidx % 2][:],
                                                               op0=ALU.mult, op1=ALU.mult),
                       reads=sres + [sr[i], gb_r[gidx % 2]], writes=dres)
                    flush()
                    b = banks[i % 2]
                    pb = ps[b][:].bitcast(BF16)
                    for kc in range(8):
                        op("pe", lambda t, kc=kc: t.transpose(out=pb[:, kc * 128:(kc + 1) * 128],
                                                              in_=dst[:, kc * 128:(kc + 1) * 128],
                                                              identity=ident_bf[:]),
                           reads=dres + [cres], writes=[psr[b]], inc=(kc == 7))
                    pend.append((i, b))
                    if per_tile is not None:
                        per_tile(i)
                flush()
                if per_tile is not None:
                    per_tile(NT)
                kb.barrier()

        def halias(a0, a1, inner):
            v = hres[:, a0:a1, :].bitcast(BF16)
            return v.rearrange("p a (b c) -> p (a b) c", c=inner)

        mix = contextlib.ExitStack()
        top.enter_context(mix)
        w_in_v = w_in.rearrange("(kc p) n -> p kc n", p=128)
        wq2 = sbt(mix, "wq2", [128, 8, 512], BF16)
        wl = sbt(mix, "wl", [128, 8, 1024], BF16)
        wq = [halias(12, 14, 512), halias(14, 16, 512), wq2[:]]
        wq_r = RL(3)
        wl_r = Res()
        for j, c0 in enumerate((1024, 1536, 2048)):
            for kc in range(8):
                op("pool", lambda g, j=j, kc=kc, c0=c0: g.dma_start(out=wq[j][:, kc, :], in_=w_in_v[:, kc, c0:c0 + 512]),
                   writes=[wq_r[j]], dma=True, pre=True)
        for kc in range(8):
            op("pool", lambda g, kc=kc: g.dma_start(out=wl[:, kc, :], in_=w_in_v[:, kc, 0:1024]), writes=[wl_r], dma=True, pre=True)

        with contextlib.ExitStack() as st:
            xt = [sbt(st, "xt%d" % j, [128, D], F32) for j in range(3)]
            xt_r = RL(3)

            def src1(i):
                j = i % 3
                op("sp", lambda q: q.dma_start(out=xt[j][:], in_=x[i * 128:(i + 1) * 128, :]),
                   writes=[xt_r[j]], dma=True)
                return xt[j][:], [xt_r[j]]
            norm_T(src1, 0)

        if stop == "P1":
            dbg["xnT"] = nc.dram_tensor("dbg_xnT", [128, 8 * S], BF16, kind="ExternalOutput").ap()
            op("sp", lambda q: q.dma_start(out=dbg["xnT"], in_=xnT[:].rearrange("p a b -> p (a b)")),
               reads=xnT_r, dma=True)
            kb.barrier()
            return nc, dbg

        sbyT = sbt(mix, "sbyT", [128, 4, S], BF16)
        sby_r = Res()
        gsb = sbt(mix, "gsb", [128, 4], F32)
        gstg = sbt(mix, "gstg", [1, 512], F32)
        gsb_r = Res()
        op("sp", lambda q: q.dma_start(out=gstg[:], in_=sb_out_g), writes=[gsb_r], dma=True)
        for c in range(4):
            op("pe", lambda t, c=c: t.transpose(out=ps[0][:, c:c + 1], in_=gstg[0:1, c * 128:(c + 1) * 128],
                                                identity=ident_f[0:1, 0:1]),
               reads=[gsb_r, cres], writes=[psr[0]])
        op("dve", lambda v: v.tensor_copy(out=gsb[:], in_=ps[0][:, 0:4]), reads=[psr[0]], writes=[gsb_r])
        kb.barrier()

        lyT = sbt(mix, "lyT", [128, 4, S], BF16)
        ly_r = Res()
        with contextlib.ExitStack() as st:
            qT = halias(0, 4, S)
            kT = halias(4, 8, S)
            vtok = halias(8, 12, 512)
            qkv_r = Res()
            bi = 0
            for j, dstT in ((0, qT), (1, kT)):
                for ch in range(4):
                    for tb in range(4):
                        b = bi % 4
                        bi += 1
                        for kc in range(8):
                            op("pe", lambda t, kc=kc: t.matmul(ps[b][:], lhsT=wq[j][:, kc, ch * 128:(ch + 1) * 128],
                                                               rhs=xnT[:, kc, tb * 512:(tb + 1) * 512],
                                                               start=(kc == 0), stop=(kc == 7)),
                               reads=[wq_r[j]] + xnT_r[tb * 4:tb * 4 + 4], writes=[psr[b]], inc=(kc == 7))
                        sc = 0.125 if j == 0 else 1.0
                        if bi % 2 == 0:
                            op("act", lambda a: a.activation(out=dstT[:, ch, tb * 512:(tb + 1) * 512], in_=ps[b][:],
                                                             func=AF.Copy, scale=sc),
                               reads=[psr[b]], writes=[qkv_r])
                        else:
                            op("dve", lambda v: v.tensor_scalar(out=dstT[:, ch, tb * 512:(tb + 1) * 512], in0=ps[b][:],
                                                                scalar1=sc, scalar2=None, op0=ALU.mult),
                               reads=[psr[b]], writes=[qkv_r])
            for i in range(NT):
                b = bi % 4
                bi += 1
                for kc in range(8):
                    op("pe", lambda t, kc=kc: t.matmul(ps[b][:], lhsT=xnT[:, kc, i * 128:(i + 1) * 128],
                                                       rhs=wq[2][:, kc, :], start=(kc == 0), stop=(kc == 7)),
                       reads=[wq_r[2], xnT_r[i]], writes=[psr[b]], inc=(kc == 7))
                if i % 2 == 0:
                    op("act", lambda a: a.copy(out=vtok[:, i, :], in_=ps[b][:]), reads=[psr[b]], writes=[qkv_r])
                else:
                    op("dve", lambda v: v.tensor_copy(out=vtok[:, i, :], in_=ps[b][:]), reads=[psr[b]], writes=[qkv_r])
            kb.barrier()

            NSTR = 2
            wk = []
            for s_ in range(NSTR):
                per = []
                for par in range(2):
                    per.append({
                        "sp": sbt(st, "at_sp%d%d" % (s_, par), [128, 512], F32),
                        "spb": sbt(st, "at_spb%d%d" % (s_, par), [128, 512], BF16),
                        "d": sbt(st, "at_d%d%d" % (s_, par), [128, 512], F32),
                        "w": sbt(st, "at_w%d%d" % (s_, par), [128, 512], BF16),
                        "r": {k: Res() for k in ("sp", "spb", "d", "w")},
                    })
                wk.append(per)
            osq = sbt(st, "at_osq", [128, 512], F32)
            osq_r = Res()
            sqacc = sbt(st, "at_sqacc", [128, S], F32)
            sqacc_r = Res()
            steps = []
            for hp in range(4):
                for j in range(4):
                    cs = list(range(4 * j + 3, -1, -1))
                    for idx, c in enumerate(cs):
                        steps.append({"hp": hp, "j": j, "c": c, "first": idx == 0, "last": idx == len(cs) - 1,
                                      "par": len(steps) % 2})

            def geo(st_, s_):
                h = 2 * st_["hp"] + s_
                prt = slice((h % 2) * 64, (h % 2) * 64 + 64)
                m = st_["c"] - 4 * st_["j"]
                c0 = 128 * m if m > 0 else 0
                return h, prt, h // 2, m, c0, slice(c0, 512), st_["j"] * 512

            def stage1(s_, st_):
                h, prt, ch, m, c0, cols, t0 = geo(st_, s_)
                c, j = st_["c"], st_["j"]
                bz = 4 * s_ + st_["par"]
                diag = m >= 0
                op("pe", lambda t: t.matmul(ps[bz][:, cols], lhsT=kT[prt, ch, c * 128:(c + 1) * 128],
                                            rhs=qT[prt, ch, t0 + c0:t0 + 512], start=True, stop=not diag),
                   reads=[qkv_r], writes=[psr[bz]], inc=not diag)
                if diag:
                    op("pe", lambda t: t.matmul(ps[bz][:, c0:c0 + 128], lhsT=ident_bf[:], rhs=maskneg[:],
                                                start=False, stop=True),
                       reads=[cres], writes=[psr[bz]])

            def stage1b(s_, st_):
                h, prt, ch, m, c0, cols, t0 = geo(st_, s_)
                W = wk[s_][st_["par"]]
                R = W["r"]
                bz = 4 * s_ + st_["par"]
                op("act", lambda a: a.activation(out=W["sp"][:, cols], in_=ps[bz][:, cols], func=AF.Exp),
                   reads=[psr[bz]], writes=[R["sp"]])
                op("act", lambda a: a.activation(out=W["sp"][:, cols], in_=W["sp"][:, cols], func=AF.Ln, bias=1.0),
                   reads=[R["sp"]], writes=[R["sp"]])
                op("dve", lambda g: g.tensor_copy(out=W["spb"][:, cols], in_=W["sp"][:, cols]),
                   reads=[R["sp"]], writes=[R["spb"]])
                op("dve", lambda v: v.tensor_tensor(out=W["d"][:, cols], in0=ps[bz][:, cols], in1=W["sp"][:, cols],
                                                    op=ALU.subtract),
                   reads=[psr[bz], R["sp"]], writes=[R["d"]])

            def stage2a(s_, st_):
                h, prt, ch, m, c0, cols, t0 = geo(st_, s_)
                W = wk[s_][st_["par"]]
                R = W["r"]
                bx = 4 * s_ + 2
                if st_["first"]:
                    op("pe", lambda t: t.matmul(ps[bx][:, :], lhsT=zeros_bf[:, :], rhs=qT[:, 0, t0:t0 + 512],
                                                start=True, stop=False, skip_group_check=True),
                       reads=[cres, qkv_r], writes=[psr[bx]])
                op("pe", lambda t: t.matmul(ps[bx][:, cols], lhsT=NU[:], rhs=W["spb"][:, cols],
                                            start=False, stop=False, skip_group_check=True),
                   reads=[cres, R["spb"]], writes=[psr[bx]])
                op("dve", lambda v: v.tensor_tensor(out=W["d"][:, cols], in0=ps[bx][:, cols], in1=W["d"][:, cols],
                                                    op=ALU.add),
                   reads=[psr[bx], R["d"]], writes=[R["d"]])

            def stage2w(s_, st_):
                h, prt, ch, m, c0, cols, t0 = geo(st_, s_)
                W = wk[s_][st_["par"]]
                R = W["r"]
                op("act", lambda a: a.activation(out=W["w"][:, cols], in_=W["d"][:, cols], func=AF.Exp),
                   reads=[R["d"]], writes=[R["w"]])

            def stage2b(s_, st_):
                h, prt, ch, m, c0, cols, t0 = geo(st_, s_)
                W = wk[s_][st_["par"]]
                R = W["r"]
                bx = 4 * s_ + 2
                if st_["c"] > 0:
                    op("pe", lambda t: t.matmul(ps[bx][:, cols], lhsT=NL[:], rhs=W["spb"][:, cols],
                                                start=False, stop=False, skip_group_check=True),
                       reads=[cres, R["spb"]], writes=[psr[bx]])

            def stage2c(s_, st_):
                h, prt, ch, m, c0, cols, t0 = geo(st_, s_)
                W = wk[s_][st_["par"]]
                R = W["r"]
                bo = 4 * s_ + 3
                c, hp = st_["c"], st_["hp"]
                if st_["first"]:
                    op("pe", lambda t: t.matmul(ps[bo][prt, :], lhsT=zeros_bf[:, 0:64], rhs=qT[:, 0, t0:t0 + 512],
                                                start=True, stop=False, skip_group_check=True),
                       reads=[cres, qkv_r], writes=[psr[bo]])
                op("pe", lambda t: t.matmul(ps[bo][prt, cols], lhsT=vtok[:, c, h * 64:(h + 1) * 64],
                                            rhs=W["w"][:, cols], start=False, stop=(c == 0),
                                            skip_group_check=True),
                   reads=[qkv_r, R["w"]], writes=[psr[bo]])
                if st_["last"]:
                    op("act", lambda a: a.activation(out=sbyT[prt, hp, t0:t0 + 512], in_=ps[bo][prt, :], func=AF.Copy,
                                                     scale=gsb[prt, hp:hp + 1]),
                       reads=[psr[bo], gsb_r], writes=[sby_r])
                    if hp == 0:
                        op("act", lambda a: a.activation(out=sqacc[prt, t0:t0 + 512], in_=ps[bo][prt, :], func=AF.Square),
                           reads=[psr[bo]], writes=[sqacc_r])
                    else:
                        op("act", lambda a: a.activation(out=osq[prt, :], in_=ps[bo][prt, :], func=AF.Square),
                           reads=[psr[bo]], writes=[osq_r])
                        op("pool", lambda g: g.tensor_tensor(out=sqacc[prt, t0:t0 + 512], in0=sqacc[prt, t0:t0 + 512],
                                                             in1=osq[prt, :], op=ALU.add),
                           reads=[osq_r, sqacc_r], writes=[sqacc_r])

            nst = len(steps)
            for s_ in range(2):
                stage1(s_, steps[0])
            for r in range(nst + 1):
                if r > 0:
                    for s_ in range(2):
                        stage2a(s_, steps[r - 1])
                if r + 1 < nst:
                    for s_ in range(2):
                        stage1(s_, steps[r + 1])
                if r < nst:
                    for s_ in range(2):
                        stage1b(s_, steps[r])
                if r > 0:
                    for s_ in range(2):
                        stage2w(s_, steps[r - 1])
                    for s_ in range(2):
                        stage2b(s_, steps[r - 1])
                    for s_ in range(2):
                        stage2c(s_, steps[r - 1])
            kb.barrier()
            for i in range(NT):
                op("pe", lambda t, i=i: t.matmul(ps[0][:, i:i + 1], lhsT=sqacc[:, i * 128:(i + 1) * 128], rhs=ones_f[:, :],
                                                 start=True, stop=True),
                   reads=[sqacc_r, cres], writes=[psr[0]])
            op("dve", lambda v: v.tensor_copy(out=stat[:, 1, :], in_=ps[0][:, 0:NT]), reads=[psr[0]], writes=[stat_r])
            kb.barrier()

        if stop == "ATT":
            dbg["sbyT"] = nc.dram_tensor("dbg_sbyT", [128, 4 * S], BF16, kind="ExternalOutput").ap()
            op("sp", lambda q: q.dma_start(out=dbg["sbyT"], in_=sbyT[:].rearrange("p a b -> p (a b)")),
               reads=[sby_r], dma=True)
            dbg["stat"] = nc.dram_tensor("dbg_stat", [128, 8 * NT], F32, kind="ExternalOutput").ap()
            op("sp", lambda q: q.dma_start(out=dbg["stat"], in_=stat[:].rearrange("p a b -> p (a b)")),
               reads=[stat_r], dma=True)
            kb.barrier()
            return nc, dbg

        mix2 = contextlib.ExitStack()
        top.enter_context(mix2)
        wo = sbt(mix2, "wo", [128, 8, D], BF16)
        wo_r = Res()
        w_out_v = w_out.rearrange("(c p) n -> p c n", p=128)
        for c in range(8):
            op("pool", lambda g, c=c: g.dma_start(out=wo[:, c, :], in_=w_out_v[:, c, :]), writes=[wo_r], dma=True, pre=True)
        with contextlib.ExitStack() as st:
            lstg = sbt(st, "lstg", [9, 512], F32)
            lv = sbt(st, "lv", [128, 4, 9], F32)
            lsc = sbt(st, "lsc", [128, 4, 2], F32)
            ltmp = sbt(st, "ltmp", [128, 4], F32)
            lv_r = Res()
            op("sp", lambda q: q.dma_start(out=lstg[:], in_=lruvec), writes=[lv_r], dma=True)
            for cc in range(4):
                op("pe", lambda t, cc=cc: t.transpose(out=ps[0][:, cc * 16:cc * 16 + 9], in_=lstg[0:9, cc * 128:(cc + 1) * 128],
                                                      identity=ident_f[0:9, 0:9]),
                   reads=[lv_r, cres], writes=[psr[0]])
            op("dve", lambda v: v.tensor_copy(out=lv[:], in_=ps[0][:, 0:64].rearrange("p (a b) -> p a b", a=4)[:, :, 0:9]),
               reads=[psr[0]], writes=[lv_r])
            op("act", lambda a: a.activation(out=ltmp[:], in_=lv[:, :, 7], func=AF.Exp, scale=-1.0), reads=[lv_r], writes=[lv_r])
            op("act", lambda a: a.activation(out=ltmp[:], in_=ltmp[:], func=AF.Ln, bias=1.0), reads=[lv_r], writes=[lv_r])
            op("dve", lambda v: v.tensor_scalar(out=lsc[:, :, 0], in0=ltmp[:], scalar1=-8.0, scalar2=None, op0=ALU.mult),
               reads=[lv_r], writes=[lv_r])
            op("dve", lambda v: v.tensor_scalar(out=lsc[:, :, 1], in0=ltmp[:], scalar1=-16.0, scalar2=None, op0=ALU.mult),
               reads=[lv_r], writes=[lv_r])
            wabd = sbt(st, "wabd", [128, 4, 128], F32)
            wxbd = sbt(st, "wxbd", [128, 4, 128], F32)
            wbd_r = Res()
            op("pool", lambda g: g.memset(wabd[:], 0.0), writes=[wbd_r])
            op("pool", lambda g: g.memset(wxbd[:], 0.0), writes=[wbd_r])
            for n in range(8):
                pr = slice((n % 2) * 64, (n % 2) * 64 + 64)
                op("sp", lambda q, n=n, pr=pr: q.dma_start(out=wabd[pr, n // 2, pr], in_=lru_w_a[n]), writes=[wbd_r], dma=True)
                op("sp", lambda q, n=n, pr=pr: q.dma_start(out=wxbd[pr, n // 2, pr], in_=lru_w_x[n]), writes=[wbd_r], dma=True)

            lxp = sbt(st, "lxp", [128, S + 3], F32)
            def hf32(a0):
                return hres[:, a0:a0 + 2, :].rearrange("p a b -> p (a b)")
            lg, cx, b1, b2, b3, b4 = (hf32(a0) for a0 in (4, 6, 8, 10, 12, 14))
            gbuf = hf32(0)
            gbuf_r = Res()
            ysq = sbt(st, "ysq", [128, S], F32)
            lxp_r, lg_r, cx_r, b1_r, b2_r, b3_r, b4_r, ysq_r = RL(8)
            op("pool", lambda g: g.memset(lxp[:, 0:3], 0.0), writes=[lxp_r])
            bi = 0
            for cc in range(4):
                for which, col0 in ((0, cc * 128), (1, 512 + cc * 128)):
                    for tb in range(4):
                        b = bi % 4
                        bi += 1
                        for kc in range(8):
                            op("pe", lambda t, kc=kc: t.matmul(ps[b][:], lhsT=wl[:, kc, col0:col0 + 128],
                                                               rhs=xnT[:, kc, tb * 512:(tb + 1) * 512],
                                                               start=(kc == 0), stop=(kc == 7)),
                               reads=[wl_r] + xnT_r[tb * 4:tb * 4 + 4], writes=[psr[b]], inc=(kc == 7))
                        if which == 0:
                            op("act", lambda a: a.copy(out=lxp[:, 3 + tb * 512:3 + (tb + 1) * 512], in_=ps[b][:]),
                               reads=[psr[b]], writes=[lxp_r])
                        else:
                            op("act", lambda a: a.copy(out=lg[:, tb * 512:(tb + 1) * 512], in_=ps[b][:]),
                               reads=[psr[b]], writes=[lg_r])
                op("act", lambda a: a.activation(out=gbuf[:], in_=lg[:], func=AF.Square), reads=[lg_r], writes=[gbuf_r])
                op("dve", lambda v: v.tensor_scalar(out=cx[:], in0=lxp[:, 3:3 + S], scalar1=lv[:, cc, 3:4], scalar2=lv[:, cc, 4:5],
                                                    op0=ALU.mult, op1=ALU.add),
                   reads=[lxp_r, lv_r], writes=[cx_r])
                for jt in range(3):
                    op("dve", lambda v, jt=jt: v.scalar_tensor_tensor(out=cx[:], in0=lxp[:, jt:jt + S], scalar=lv[:, cc, jt:jt + 1],
                                                                      in1=cx[:], op0=ALU.mult, op1=ALU.add),
                       reads=[lxp_r, lv_r, cx_r], writes=[cx_r])
                op("dve", lambda g: g.tensor_scalar(out=gbuf[:], in0=gbuf[:], scalar1=0.044715, scalar2=1.0, op0=ALU.mult, op1=ALU.add),
                   reads=[gbuf_r], writes=[gbuf_r])
                op("dve", lambda g: g.tensor_tensor(out=gbuf[:], in0=gbuf[:], in1=lg[:], op=ALU.mult), reads=[gbuf_r, lg_r], writes=[gbuf_r])
                for wmat, bcol, dst, dres in ((wabd, 5, b1, b1_r), (wxbd, 6, b4, b4_r)):
                    for tb in range(4):
                        b = 4 + (bi % 4)
                        bi += 1
                        op("pe", lambda t: t.matmul(ps[b][:], lhsT=wmat[:, cc, :], rhs=cx[:, tb * 512:(tb + 1) * 512],
                                                    start=True, stop=True),
                           reads=[wbd_r, cx_r], writes=[psr[b]])
                        op("act", lambda a: a.activation(out=dst[:, tb * 512:(tb + 1) * 512], in_=ps[b][:], func=AF.Sigmoid,
                                                         bias=lv[:, cc, bcol:bcol + 1]),
                           reads=[psr[b], lv_r], writes=[dres])
                op("act", lambda a: a.activation(out=gbuf[:], in_=gbuf[:], func=AF.Sigmoid, scale=1.5957691216057308),
                   reads=[gbuf_r], writes=[gbuf_r])
                op("dve", lambda g: g.tensor_tensor(out=gbuf[:], in0=gbuf[:], in1=lg[:], op=ALU.mult), reads=[gbuf_r, lg_r], writes=[gbuf_r])
                op("act", lambda a: a.activation(out=b2[:], in_=b1[:], func=AF.Exp, scale=lsc[:, cc, 0:1]),
                   reads=[b1_r, lv_r], writes=[b2_r])
                op("act", lambda a: a.activation(out=b3[:], in_=b1[:], func=AF.Exp, scale=lsc[:, cc, 1:2]),
                   reads=[b1_r, lv_r], writes=[b3_r])
                op("act", lambda a: a.activation(out=b3[:], in_=b3[:], func=AF.Sqrt, scale=-1.0, bias=1.0),
                   reads=[b3_r], writes=[b3_r])
                op("dve", lambda g: g.tensor_tensor(out=b4[:], in0=b4[:], in1=cx[:], op=ALU.mult), reads=[b4_r, cx_r], writes=[b4_r])
                op("dve", lambda g: g.tensor_tensor(out=b4[:], in0=b4[:], in1=b3[:], op=ALU.mult), reads=[b4_r, b3_r], writes=[b4_r])
                op("dve", lambda v: v.tensor_tensor_scan(out=b1[:], data0=b2[:], data1=b4[:], initial=0.0,
                                                         op0=ALU.mult, op1=ALU.add),
                   reads=[b2_r, b4_r], writes=[b1_r])
                op("dve", lambda v: v.tensor_tensor(out=b1[:], in0=b1[:], in1=gbuf[:], op=ALU.mult), reads=[b1_r, gbuf_r], writes=[b1_r])
                if cc == 0:
                    op("act", lambda a: a.activation(out=ysq[:], in_=b1[:], func=AF.Square), reads=[b1_r], writes=[ysq_r])
                else:
                    op("act", lambda a: a.activation(out=b2[:], in_=b1[:], func=AF.Square), reads=[b1_r], writes=[b2_r])
                    op("dve", lambda g: g.tensor_tensor(out=ysq[:], in0=ysq[:], in1=b2[:], op=ALU.add), reads=[ysq_r, b2_r], writes=[ysq_r])
                op("dve", lambda v: v.tensor_scalar(out=lyT[:, cc, :], in0=b1[:], scalar1=lv[:, cc, 8:9], scalar2=None, op0=ALU.mult),
                   reads=[b1_r, lv_r], writes=[ly_r])
            kb.barrier()
            for i in range(NT):
                op("pe", lambda t, i=i: t.matmul(ps[0][:, i:i + 1], lhsT=ysq[:, i * 128:(i + 1) * 128], rhs=ones_f[:, :],
                                                 start=True, stop=True),
                   reads=[ysq_r, cres], writes=[psr[0]])
            op("dve", lambda v: v.tensor_copy(out=stat[:, 2, :], in_=ps[0][:, 0:NT]), reads=[psr[0]], writes=[stat_r])
            op("act", lambda a: a.activation(out=stat[:, 1:3, :], in_=stat[:, 1:3, :], func=AF.Sqrt, scale=1.0 / 512, bias=EPS),
               reads=[stat_r], writes=[stat_r])
            op("dve", lambda v: v.reciprocal(out=stat[:, 1:3, :], in_=stat[:, 1:3, :]), reads=[stat_r], writes=[stat_r])
            kb.barrier()

        if stop == "LRU":
            dbg["lyT"] = nc.dram_tensor("dbg_lyT", [128, 4 * S], BF16, kind="ExternalOutput").ap()
            op("sp", lambda q: q.dma_start(out=dbg["lyT"], in_=lyT[:].rearrange("p a b -> p (a b)")), reads=[ly_r], dma=True)
            dbg["stat"] = nc.dram_tensor("dbg_stat", [128, 8 * NT], F32, kind="ExternalOutput").ap()
            op("sp", lambda q: q.dma_start(out=dbg["stat"], in_=stat[:].rearrange("p a b -> p (a b)")), reads=[stat_r], dma=True)
            kb.barrier()
            return nc, dbg

        with contextlib.ExitStack() as st:
            xt = [sbt(st, "xo%d" % j, [128, D], F32) for j in range(2)]
            xt_r = RL(2)
            tmp = [sbt(st, "otmp%d" % j, [128, 512], F32) for j in range(2)]
            tmp_r = RL(2)
            for i in range(NT):
                j = i % 2
                op("sp", lambda q: q.dma_start(out=xt[j][:], in_=x[i * 128:(i + 1) * 128, :]), writes=[xt_r[j]], dma=True)
                for half in range(2):
                    n0 = half * 512
                    ba = 2 * ((2 * i + half) % 2)
                    bb = ba + 1
                    for c in range(4):
                        op("pe", lambda t, c=c: t.matmul(ps[ba][:], lhsT=lyT[:, c, i * 128:(i + 1) * 128], rhs=wo[:, c, n0:n0 + 512],
                                                         start=(c == 0), stop=(c == 3)),
                           reads=[ly_r, wo_r], writes=[psr[ba]], inc=(c == 3))
                    for c in range(4):
                        op("pe", lambda t, c=c: t.matmul(ps[bb][:], lhsT=sbyT[:, c, i * 128:(i + 1) * 128], rhs=wo[:, 4 + c, n0:n0 + 512],
                                                         start=(c == 0), stop=(c == 3)),
                           reads=[sby_r, wo_r], writes=[psr[bb]], inc=(c == 3))
                    op("dve", lambda v: v.scalar_tensor_tensor(out=tmp[half][:], in0=ps[ba][:], scalar=stat[:, 2, i:i + 1],
                                                               in1=xt[j][:, n0:n0 + 512], op0=ALU.mult, op1=ALU.add),
                       reads=[psr[ba], stat_r, xt_r[j]], writes=[tmp_r[half]])
                    op("dve", lambda v: v.scalar_tensor_tensor(out=hres[:, i, n0:n0 + 512], in0=ps[bb][:], scalar=stat[:, 1, i:i + 1],
                                                               in1=tmp[half][:], op0=ALU.mult, op1=ALU.add),
                       reads=[psr[bb], stat_r, tmp_r[half]], writes=[h_r[i]])
            kb.barrier()
        mix2.close()
        mix.close()

        def dump_h(name):
            dbg[name] = nc.dram_tensor("dbg_" + name, [S, D], F32, kind="ExternalOutput").ap()
            for i in range(NT):
                op("sp", lambda q, i=i: q.dma_start(out=dbg[name][i * 128:(i + 1) * 128, :], in_=hres[:, i, :]),
                   reads=[h_r[i]], dma=True)
            kb.barrier()

        if stop == "OUT":
            dump_h("h1")
            return nc, dbg

        moe = contextlib.ExitStack()
        top.enter_context(moe)
        G = sbt(moe, "G", [128, NT, NE], F32)
        dest = sbt(moe, "dest", [128, NT, 4], I32)
        gk = sbt(moe, "gk", [128, NT, 4], F32)
        bupT = sbt(moe, "bupT", [128, 16, NE], F32)
        rt_r = Res()
        G_r, dest_r, gk_r = RL(NT), RL(NT), RL(NT)
        with contextlib.ExitStack() as st:
            GT = [sbt(st, "GT%d" % j, [NE, 128], F32) for j in range(2)]
            GT_r = RL(2)
            bdn = sbt(st, "bdn", [NE, D], F32)
            xn2all = sbt(st, "xn2all", [128, NT, D], BF16)
            xn2all_r = RL(NT)
            wr = sbt(st, "wr", [128, 8, NE], BF16)
            brb = sbt(st, "brb", [128, NE], F32)
            bstg = sbt(st, "bstg", [NE, 2 * D], F32)
            op("pool", lambda g: g.dma_start(out=wr[:], in_=w_router.rearrange("(kc p) n -> p kc n", p=128)), writes=[rt_r], dma=True)
            op("sp", lambda q: q.dma_start(out=brb[:], in_=b_router.partition_broadcast(128)), writes=[rt_r], dma=True)
            op("sp", lambda q: q.dma_start(out=bstg[:], in_=b_up), writes=[rt_r], dma=True)
            op("sp", lambda q: q.dma_start(out=bdn[:], in_=b_down), writes=[rt_r], dma=True)
            for c in range(16):
                op("pe", lambda t, c=c: t.transpose(out=ps[4 + c // 8][:, (c % 8) * NE:(c % 8 + 1) * NE],
                                                    in_=bstg[0:NE, c * 128:(c + 1) * 128], identity=ident_f[0:NE, 0:NE]),
                   reads=[rt_r, cres], writes=[psr[4 + c // 8]])
            for hh in range(2):
                op("dve", lambda v, hh=hh: v.tensor_copy(out=bupT[:, hh * 8:(hh + 1) * 8, :],
                                                         in_=ps[4 + hh][:, 0:8 * NE].rearrange("p (a b) -> p a b", a=8)),
                   reads=[psr[4 + hh]], writes=[rt_r])
            op("dve", lambda v: v.tensor_scalar(out=bupT[:, 8:16, :], in0=bupT[:, 8:16, :], scalar1=1.0, scalar2=None, op0=ALU.add),
               reads=[rt_r], writes=[rt_r])
            mskb = sbt(st, "mskb", [128, NT, NE], BF16)
            mskb_r = RL(NT)
            T2 = []
            for par in range(2):
                T2.append({
                    "lgt": sbt(st, "lgt", [128, NE], F32), "v8": sbt(st, "v8", [128, 8], F32), "i8": sbt(st, "i8", [128, 8], U32),
                    "i8f": sbt(st, "i8f", [128, 8], F32), "nmx": sbt(st, "nmx", [128, 1], F32), "exl": sbt(st, "exl", [128, NE], F32),
                    "msk": sbt(st, "msk", [128, NE], F32), "den": sbt(st, "den", [128, 1], F32), "posf": sbt(st, "posf", [128, NE], F32),
                    "oh3": sbt(st, "oh3", [128, 4, NE], F32), "sc3": sbt(st, "sc3", [128, 4, NE], F32), "pk": sbt(st, "pk", [128, 4], F32),
                    "vk": sbt(st, "vk", [128, 4], F32), "dk": sbt(st, "dk", [128, 4], F32), "w_r": Res()})
            iota3 = iota_e[:].unsqueeze(1).to_broadcast([128, 4, NE])

            def route_tile(i):
                T = T2[i % 2]
                lgt, v8, i8, i8f, nmx, exl, msk, den = (T[k] for k in ("lgt", "v8", "i8", "i8f", "nmx", "exl", "msk", "den"))
                posf, oh3, sc3, pk, vk, dk, w_r = (T[k] for k in ("posf", "oh3", "sc3", "pk", "vk", "dk", "w_r"))
                lb, pbk = 2 + (i % 2), 6 + (i % 2)
                for kc in range(8):
                    op("pe", lambda t, kc=kc: t.matmul(ps[lb][:, 0:NE], lhsT=xnT[:, kc, i * 128:(i + 1) * 128], rhs=wr[:, kc, :],
                                                       start=(kc == 0), stop=(kc == 7)),
                       reads=[xnT_r[i], rt_r], writes=[psr[lb]], inc=(kc == 7))
                    yield
                op("dve", lambda v: v.tensor_tensor(out=lgt[:], in0=ps[lb][:, 0:NE], in1=brb[:], op=ALU.add),
                   reads=[psr[lb], rt_r], writes=[w_r])
                yield
                op("dve", lambda v: v.max(out=v8[:], in_=lgt[:]), reads=[w_r], writes=[w_r])
                yield
                op("dve", lambda v: v.max_index(out=i8[:], in_max=v8[:], in_values=lgt[:]), reads=[w_r], writes=[w_r])
                yield
                op("dve", lambda v: v.tensor_copy(out=i8f[:], in_=i8[:]), reads=[w_r], writes=[w_r])
                yield
                op("dve", lambda v: v.tensor_scalar(out=msk[:], in0=lgt[:], scalar1=v8[:, 3:4], scalar2=None, op0=ALU.is_ge),
                   reads=[w_r], writes=[w_r])
                yield
                op("dve", lambda v: v.tensor_copy(out=mskb[:, i, :], in_=msk[:]), reads=[w_r], writes=[mskb_r[i]])
                yield
                op("dve", lambda v: v.tensor_scalar(out=nmx[:], in0=v8[:, 0:1], scalar1=-1.0, scalar2=None, op0=ALU.mult),
                   reads=[w_r], writes=[w_r])
                yield
                op("act", lambda a: a.activation(out=exl[:], in_=lgt[:], func=AF.Exp, bias=nmx[:, 0:1]), reads=[w_r], writes=[w_r])
                yield
                op("pe", lambda t: t.matmul(ps[pbk][:, 0:NE], lhsT=SU[:], rhs=mskb[:, i, :], start=True, stop=(i == 0)),
                   reads=[mskb_r[i], cres], writes=[psr[pbk]], inc=(i == 0))
                yield
                for i2 in range(i):
                    op("pe", lambda t, i2=i2: t.matmul(ps[pbk][:, 0:NE], lhsT=ones_bf[:], rhs=mskb[:, i2, :], start=False,
                                                       stop=(i2 == i - 1)),
                       reads=[mskb_r[i2], cres], writes=[psr[pbk]], inc=(i2 == i - 1))
                    yield
                op("dve", lambda v: v.tensor_copy(out=posf[:], in_=ps[pbk][:, 0:NE]), reads=[psr[pbk]], writes=[w_r])
                yield
                op("dve", lambda v: v.tensor_tensor(out=exl[:], in0=exl[:], in1=msk[:], op=ALU.mult), reads=[w_r], writes=[w_r])
                yield
                op("dve", lambda v: v.reduce_sum(out=den[:], in_=exl[:], axis=mybir.AxisListType.X), reads=[w_r], writes=[w_r])
                yield
                op("dve", lambda v: v.reciprocal(out=den[:], in_=den[:]), reads=[w_r], writes=[w_r])
                yield
                op("dve", lambda v: v.tensor_scalar(out=G[:, i, :], in0=exl[:], scalar1=den[:, 0:1], scalar2=None, op0=ALU.mult),
                   reads=[w_r], writes=[G_r[i]])
                yield
                op("dve", lambda v: v.tensor_tensor(out=oh3[:], in0=iota3, in1=i8f[:, 0:4].unsqueeze(2).to_broadcast([128, 4, NE]),
                                                    op=ALU.is_equal),
                   reads=[w_r, cres], writes=[w_r])
                yield
                op("dve", lambda v: v.tensor_tensor(out=sc3[:], in0=oh3[:], in1=posf[:].unsqueeze(1).to_broadcast([128, 4, NE]), op=ALU.mult),
                   reads=[w_r], writes=[w_r])
                yield
                op("dve", lambda v: v.reduce_sum(out=pk[:], in_=sc3[:], axis=mybir.AxisListType.X), reads=[w_r], writes=[w_r])
                yield
                op("dve", lambda v: v.tensor_tensor(out=sc3[:], in0=oh3[:], in1=G[:, i, :].unsqueeze(1).to_broadcast([128, 4, NE]), op=ALU.mult),
                   reads=[w_r, G_r[i]], writes=[w_r])
                yield
                op("dve", lambda v: v.reduce_sum(out=gk[:, i, :], in_=sc3[:], axis=mybir.AxisListType.X), reads=[w_r], writes=[gk_r[i]])
                yield
                op("dve", lambda v: v.tensor_scalar(out=vk[:], in0=pk[:], scalar1=float(CAP), scalar2=None, op0=ALU.is_lt), reads=[w_r], writes=[w_r])
                yield
                op("dve", lambda v: v.tensor_tensor(out=gk[:, i, :], in0=gk[:, i, :], in1=vk[:], op=ALU.mult), reads=[w_r, gk_r[i]], writes=[gk_r[i]])
                yield
                op("dve", lambda v: v.scalar_tensor_tensor(out=dk[:], in0=i8f[:, 0:4], scalar=float(CAP), in1=pk[:], op0=ALU.mult, op1=ALU.add),
                   reads=[w_r], writes=[w_r])
                yield
                op("dve", lambda v: v.tensor_scalar(out=vk[:], in0=vk[:], scalar1=-1.0e6, scalar2=1.0e6, op0=ALU.mult, op1=ALU.add), reads=[w_r], writes=[w_r])
                yield
                op("dve", lambda v: v.tensor_tensor(out=dk[:], in0=dk[:], in1=vk[:], op=ALU.add), reads=[w_r], writes=[w_r])
                yield
                op("dve", lambda v: v.tensor_copy(out=dest[:, i, :], in_=dk[:]), reads=[w_r], writes=[dest_r[i]])
                yield
                for k in range(4):
                    op("pool", lambda g, k=k: g.indirect_dma_start(
                        out=xs_d, out_offset=bass.IndirectOffsetOnAxis(ap=dest[:, i, k:k + 1], axis=0),
                        in_=xn2all[:, i, :], in_offset=None, bounds_check=breg, oob_is_err=False),
                        reads=[dest_r[i], xn2all_r[i]], dma=True)
                    yield
                jj = i % 2
                op("pe", lambda t: t.transpose(out=ps[4 + jj][0:NE, 0:128], in_=G[:, i, :], identity=ident_f[:]),
                   reads=[G_r[i], cres], writes=[psr[4 + jj]])
                yield
                op("act", lambda a: a.copy(out=GT[jj][:], in_=ps[4 + jj][0:NE, 0:128]), reads=[psr[4 + jj]], writes=[GT_r[jj]])
                yield
                for half in range(2):
                    b = lb
                    op("pe", lambda t: t.matmul(ps[b][:], lhsT=GT[jj][:], rhs=bdn[:, half * 512:(half + 1) * 512], start=True, stop=True),
                       reads=[GT_r[jj], rt_r], writes=[psr[b]])
                    yield
                    op("dve", lambda v: v.tensor_tensor(out=hres[:, i, half * 512:(half + 1) * 512], in0=ps[b][:],
                                                        in1=hres[:, i, half * 512:(half + 1) * 512], op=ALU.add),
                       reads=[psr[b], h_r[i]], writes=[h_r[i]])
                    yield

            def route_pair(i0):
                gens = [route_tile(i) for i in (i0, i0 + 1)]
                alive = [True, True]
                while any(alive):
                    for gi, g in enumerate(gens):
                        if alive[gi]:
                            try:
                                next(g)
                            except StopIteration:
                                alive[gi] = False

            def hook5(i):
                if i >= 2 and i % 2 == 0:
                    route_pair(i - 2)

            def src5(i):
                return hres[:, i, :], [h_r[i]]
            norm_T(src5, 1, tok_out=lambda i: (xn2all[:, i, :], [xn2all_r[i]]), statrow=3, per_tile=hook5)
            kb.barrier()

        if stop == "ROUTE":
            dbg["G"] = nc.dram_tensor("dbg_G", [128, NT * NE], F32, kind="ExternalOutput").ap()
            op("sp", lambda q: q.dma_start(out=dbg["G"], in_=G[:].rearrange("p a b -> p (a b)")), reads=[rt_r], dma=True)
            dbg["dest"] = nc.dram_tensor("dbg_dest", [128, NT * 4], I32, kind="ExternalOutput").ap()
            op("sp", lambda q: q.dma_start(out=dbg["dest"], in_=dest[:].rearrange("p a b -> p (a b)")), reads=[rt_r], dma=True)
            dbg["gk"] = nc.dram_tensor("dbg_gk", [128, NT * 4], F32, kind="ExternalOutput").ap()
            op("sp", lambda q: q.dma_start(out=dbg["gk"], in_=gk[:].rearrange("p a b -> p (a b)")), reads=[rt_r], dma=True)
            kb.barrier()
            return nc, dbg

        with contextlib.ExitStack() as st:
            NR = 5
            NSTG = 8
            for t_ in range(10, 16):
                op("sp", lambda q, t_=t_: q.dma_start(out=hsp_d[(t_ - 10) * 128:(t_ - 9) * 128, :], in_=hres[:, t_, :]),
                   reads=[h_r[t_]], dma=True)
            kb.barrier()
            ring = [sbt(st, "ring%d" % j, [128, 8, D], BF16) for j in range(NR)]
            ring_r = [RL(8) for _ in range(NR)]
            xflat = xnT[:].rearrange("p a b -> p (a b)")
            xe = xflat[:, 0:4096].rearrange("p (a b) -> p a b", a=NA)
            xe_r = Res()
            xeT = xflat[:, 4096:8192].rearrange("p (a b) -> p a b", a=8)
            xeT_r = Res()
            actT = xflat[:, 8192:12288].rearrange("p (a b) -> p a b", a=8)
            actT_r = RL(8)
            stg = [sbt(st, "stg%d" % j, [128, D], F32) for j in range(2)]
            stg = [t[:] for t in stg] + [xflat[:, 12288 + j * 2048:12288 + (j + 1) * 2048].bitcast(F32) for j in range(2)]
            stg += [hres[:, 12 + j, :] for j in range(4)]
            stg_r = RL(NSTG)
            gc = sbt(st, "gc", [128, CAP], F32)
            sg = sbt(st, "sg", [128, CAP], F32)
            uc = sbt(st, "uc", [128, CAP], F32)
            gc_r, sg_r, uc_r = Res(), Res(), Res()
            yst = [gb[j][:] for j in range(2)] + [hres[:, 10 + j, :] for j in range(2)]
            yst_r = RL(4)
            w_up_v = w_up.rearrange("e (kc p) n -> e p kc n", p=128)
            w_dn_v = w_down.rearrange("e (kc p) n -> e p kc n", p=128)
            ys_res = Res()
            npiece = [0]

            def load_piece(mi, kc):
                e_, part = divmod(mi, 3)
                if e_ >= NE:
                    return
                j = mi % NR
                n = npiece[0]
                npiece[0] += 1
                sj = n % NSTG
                src = w_up_v[e_, :, kc, part * D:(part + 1) * D] if part < 2 else w_dn_v[e_, :, kc, :]
                op("sp", lambda q: q.dma_start(out=stg[sj], in_=src), writes=[stg_r[sj]], dma=True)
                op("act", lambda a: a.copy(out=ring[j][:, kc, :], in_=stg[sj]), reads=[stg_r[sj]], writes=[ring_r[j][kc]])

            def load_xe(e_):
                op("pool", lambda q: q.dma_start(out=xe[:, 0:3, :], in_=xs_d[e_ * CAP:e_ * CAP + 384, :].rearrange("(a p) n -> p a n", p=128)),
                   writes=[xe_r], dma=True)
                op("pool", lambda q: q.dma_start(out=xe[0:LAST, 3, :], in_=xs_d[e_ * CAP + 384:(e_ + 1) * CAP, :]),
                   writes=[xe_r], dma=True)

            def xpose(a_):
                b = a_ % 2
                pb = ps[b][:].bitcast(BF16)
                for kc in range(8):
                    op("pe", lambda t, kc=kc: t.transpose(out=pb[:, kc * 128:(kc + 1) * 128], in_=xe[:, a_, kc * 128:(kc + 1) * 128],
                                                          identity=ident_bf[:]),
                       reads=[xe_r, cres], writes=[psr[b]], inc=(kc == 7))
                op("dve", lambda v: v.tensor_copy(out=xeT[:, :, a_ * 128:(a_ + 1) * 128], in_=pb.rearrange("p (a b) -> p a b", a=8)),
                   reads=[psr[b]], writes=[xeT_r])

            load_xe(0)
            for mi in range(2):
                for kc in range(8):
                    load_piece(mi, kc)
            for e in range(NE):
                jg, ju, jd = (3 * e) % NR, (3 * e + 1) % NR, (3 * e + 2) % NR
                if e == 0:
                    for a_ in range(NA):
                        xpose(a_)
                if e + 1 < NE:
                    load_xe(e + 1)
                for nch in range(8):
                    bg = 2 + (nch % 2) * 2
                    bu = bg + 1
                    for kc in range(8):
                        op("pe", lambda t, kc=kc: t.matmul(ps[bg][:, 0:CAP], lhsT=ring[jg][:, kc, nch * 128:(nch + 1) * 128], rhs=xeT[:, kc, 0:CAP],
                                                           start=(kc == 0), stop=(kc == 7)),
                           reads=[ring_r[jg][kc], xeT_r], writes=[psr[bg]], inc=(kc == 7))
                    for kc in range(8):
                        op("pe", lambda t, kc=kc: t.matmul(ps[bu][:, 0:CAP], lhsT=ring[ju][:, kc, nch * 128:(nch + 1) * 128], rhs=xeT[:, kc, 0:CAP],
                                                           start=(kc == 0), stop=(kc == 7)),
                           reads=[ring_r[ju][kc], xeT_r], writes=[psr[bu]], inc=(kc == 7))
                    op("dve", lambda v: v.tensor_scalar(out=gc[:], in0=ps[bg][:, 0:CAP], scalar1=bupT[:, nch, e:e + 1], scalar2=7.0,
                                                        op0=ALU.add, op1=ALU.min),
                       reads=[psr[bg], rt_r], writes=[gc_r])
                    op("act", lambda a: a.activation(out=sg[:], in_=gc[:], func=AF.Sigmoid, scale=1.702),
                       reads=[gc_r], writes=[sg_r])
                    if e == 0:
                        load_piece(2, nch)
                    load_piece(3 * e + 3 + (nch // 4), (2 * nch) % 8)
                    load_piece(3 * e + 3 + (nch // 4), (2 * nch + 1) % 8)
                    op("dve", lambda v: v.tensor_scalar(out=uc[:], in0=ps[bu][:, 0:CAP], scalar1=bupT[:, 8 + nch, e:e + 1], scalar2=8.0,
                                                        op0=ALU.add, op1=ALU.min),
                       reads=[psr[bu], rt_r], writes=[uc_r])
                    op("dve", lambda v: v.tensor_tensor(out=sg[:], in0=gc[:], in1=sg[:], op=ALU.mult),
                       reads=[gc_r, sg_r], writes=[sg_r])
                    op("dve", lambda v: v.scalar_tensor_tensor(out=actT[:, nch, 0:CAP], in0=uc[:], scalar=-6.0, in1=sg[:],
                                                               op0=ALU.max, op1=ALU.mult),
                       reads=[sg_r, uc_r], writes=[actT_r[nch]])
                gi = 0
                for a_ in range(NA):
                    yp = (e * NA + a_) % 4
                    rows = 128
                    for half in range(2):
                        b = 6 + half
                        for nch in range(8):
                            op("pe", lambda t, nch=nch: t.matmul(ps[b][0:rows, :], lhsT=actT[:, nch, a_ * 128:a_ * 128 + rows],
                                                                 rhs=ring[jd][:, nch, half * 512:(half + 1) * 512],
                                                                 start=(nch == 0), stop=(nch == 7)),
                               reads=[actT_r[nch], ring_r[jd][nch]], writes=[psr[b]], inc=(nch == 7))
                        op("dve", lambda v: v.tensor_copy(out=yst[yp][0:rows, half * 512:(half + 1) * 512], in_=ps[b][0:rows, :]),
                           reads=[psr[b]], writes=[yst_r[yp]])
                        load_piece(3 * e + 5, gi)
                        gi += 1
                    if e + 1 < NE:
                        xpose(a_)
                    r0 = e * CAP + a_ * 128
                    srows = 128 if a_ < 3 else LAST
                    op("pool", lambda q: q.dma_start(out=ys_d[r0:r0 + srows, :], in_=yst[yp][0:srows, :]), reads=[yst_r[yp]], dma=True)
            kb.barrier()

        with contextlib.ExitStack() as st:
            NYG = 8
            yg = [sbt(st, "yg%d" % j, [128, D], F32) for j in range(NYG)]
            yg_r = RL(NYG)
            for j in range(NYG):
                op("dve", lambda v, j=j: v.memset(yg[j][:], 0.0), writes=[yg_r[j]])
            load_gain(2)
            load_gain(3)
            junk = sbt(st, "fjunk", [128, D], BF16)
            junk_r = Res()
            xnb = [sbt(st, "fxnb%d" % j, [128, D], BF16) for j in range(2)]
            xnb_r = RL(2)
            fsq = sbt(st, "fsq", [128, 2 * NT], F32)
            wg = sbt(st, "wg", [128, 8, D], BF16)
            wp = sbt(st, "wp", [128, 2, D], BF16)
            wg_r = RL(10)
            wstg = [sbt(st, "wstg%d" % j, [128, D], F32) for j in range(2)]
            wstg_r = RL(2)
            wgv = w_ple_gate.rearrange("(kc p) n -> p kc n", p=128)
            wpv = w_ple.rearrange("(kc p) n -> p kc n", p=128)
            for n in range(10):
                src = wgv[:, n, :] if n < 8 else wpv[:, n - 8, :]
                dstw = wg[:, n, :] if n < 8 else wp[:, n - 8, :]
                op("sp", lambda q: q.dma_start(out=wstg[n % 2][:], in_=src), writes=[wstg_r[n % 2]], dma=True)
                op("act", lambda a: a.copy(out=dstw, in_=wstg[n % 2][:]), reads=[wstg_r[n % 2]], writes=[wg_r[n]])
            for t_ in range(10, 16):
                op("sp", lambda q, t_=t_: q.dma_start(out=hres[:, t_, :], in_=hsp_d[(t_ - 10) * 128:(t_ - 9) * 128, :]),
                   writes=[h_r[t_]], dma=True)
            pf = [sbt(st, "pf%d" % j, [128, 256], F32) for j in range(2)]
            pf_r = RL(2)
            pt = [sbt(st, "pt%d" % j, [128, 256], BF16) for j in range(2)]
            pt_r = RL(2)
            pT = [sbt(st, "pT%d" % j, [128, 2, 128], BF16) for j in range(2)]
            pT_r = RL(2)
            sgt = [sbt(st, "sgt%d" % j, [128, 512], F32) for j in range(2)]
            sgt_r = RL(2)
            ot = [sbt(st, "ot%d" % j, [128, D], F32) for j in range(2)]
            ot_r = RL(2)

            def combine(i):
                for k in range(4):
                    y = (4 * i + k) % NYG
                    op("pool", lambda g: g.indirect_dma_start(
                        out=yg[y][:, :], out_offset=None, in_=ys_d,
                        in_offset=bass.IndirectOffsetOnAxis(ap=dest[:, i, k:k + 1], axis=0),
                        bounds_check=breg, oob_is_err=False),
                        reads=[dest_r[i], ys_res], writes=[yg_r[y]], dma=True)
                    op("dve", lambda v: v.scalar_tensor_tensor(out=hres[:, i, :], in0=yg[y][:], scalar=gk[:, i, k:k + 1],
                                                               in1=hres[:, i, :], op0=ALU.mult, op1=ALU.add),
                       reads=[yg_r[y], gk_r[i], h_r[i]], writes=[h_r[i]])

            sA_r, sC_r = RL(NT), RL(NT)

            def pleA(i):
                j = i % 2
                ssc = stat[:, 4, i:i + 1]
                op("act", lambda a: a.activation(out=junk[:], in_=hres[:, i, :], func=AF.Square, accum_out=ssc),
                   reads=[h_r[i]], writes=[junk_r, sA_r[i]])
                op("act", lambda a: a.activation(out=fsq[:, i:i + 1], in_=ssc, func=AF.Sqrt, scale=1.0 / D, bias=EPS),
                   reads=[sA_r[i]], writes=[sA_r[i]])
                op("dve", lambda v: v.reciprocal(out=ssc, in_=fsq[:, i:i + 1]), reads=[sA_r[i]], writes=[sA_r[i]])
                op("dve", lambda v: v.scalar_tensor_tensor(out=xnb[j][:], in0=hres[:, i, :], scalar=ssc, in1=gb[0][:],
                                                           op0=ALU.mult, op1=ALU.mult),
                   reads=[h_r[i], sA_r[i], gb_r[0]], writes=[xnb_r[j]])
                op("sp", lambda q: q.dma_start(out=pf[j][:], in_=pin[i * 128:(i + 1) * 128, :]), writes=[pf_r[j]], dma=True)
                op("act", lambda a: a.copy(out=pt[j][:], in_=pf[j][:]), reads=[pf_r[j]], writes=[pt_r[j]])

            def pleA2(i):
                j = i % 2
                pb = ps[j][:].bitcast(BF16)
                for kc in range(8):
                    op("pe", lambda t, kc=kc: t.transpose(out=pb[:, kc * 128:(kc + 1) * 128], in_=xnb[j][:, kc * 128:(kc + 1) * 128],
                                                          identity=ident_bf[:]),
                       reads=[xnb_r[j], cres], writes=[psr[j]], inc=(kc == 7))
                op("act", lambda a: a.copy(out=xnT[:, :, i * 128:(i + 1) * 128], in_=pb.rearrange("p (a b) -> p a b", a=8)),
                   reads=[psr[j]], writes=[xnT_r[i]])
                pb2 = ps[2 + j][:].bitcast(BF16)
                for kc in range(2):
                    op("pe", lambda t, kc=kc: t.transpose(out=pb2[:, kc * 128:(kc + 1) * 128], in_=pt[j][:, kc * 128:(kc + 1) * 128],
                                                          identity=ident_bf[:]),
                       reads=[pt_r[j], cres], writes=[psr[2 + j]], inc=(kc == 1))
                op("act", lambda a: a.copy(out=pT[j][:], in_=pb2[:, 0:256].rearrange("p (a b) -> p a b", a=2)),
                   reads=[psr[2 + j]], writes=[pT_r[j]])

            def pleB(i):
                j = i % 2
                for half in range(2):
                    n0 = half * 512
                    bg = 4 + 2 * half
                    bp = bg + 1
                    for kc in range(8):
                        op("pe", lambda t, kc=kc: t.matmul(ps[bg][:], lhsT=xnT[:, kc, i * 128:(i + 1) * 128], rhs=wg[:, kc, n0:n0 + 512],
                                                           start=(kc == 0), stop=(kc == 7)),
                           reads=[xnT_r[i], wg_r[kc]], writes=[psr[bg]], inc=(kc == 7))
                    for kc in range(2):
                        op("pe", lambda t, kc=kc: t.matmul(ps[bp][:], lhsT=pT[j][:, kc, :], rhs=wp[:, kc, n0:n0 + 512],
                                                           start=(kc == 0), stop=(kc == 1)),
                           reads=[pT_r[j], wg_r[8 + kc]], writes=[psr[bp]], inc=(kc == 1))

            def pleB2(i):
                for half in range(2):
                    n0 = half * 512
                    bg = 4 + 2 * half
                    bp = bg + 1
                    op("act", lambda a: a.activation(out=sgt[half][:], in_=ps[bg][:], func=AF.Sigmoid), reads=[psr[bg]], writes=[sgt_r[half]])
                    op("dve", lambda v: v.tensor_tensor(out=sgt[half][:], in0=ps[bp][:], in1=sgt[half][:], op=ALU.mult),
                       reads=[psr[bp], sgt_r[half]], writes=[sgt_r[half]])
                    op("dve", lambda v: v.tensor_tensor(out=hres[:, i, n0:n0 + 512], in0=hres[:, i, n0:n0 + 512], in1=sgt[half][:], op=ALU.add),
                       reads=[sgt_r[half], h_r[i]], writes=[h_r[i]])

            def pleC(i):
                j = i % 2
                ssf = stat[:, 5, i:i + 1]
                op("act", lambda a: a.activation(out=junk[:], in_=hres[:, i, :], func=AF.Square, accum_out=ssf),
                   reads=[h_r[i]], writes=[junk_r, sC_r[i]])
                op("act", lambda a: a.activation(out=fsq[:, NT + i:NT + i + 1], in_=ssf, func=AF.Sqrt, scale=1.0 / D, bias=EPS),
                   reads=[sC_r[i]], writes=[sC_r[i]])
                op("dve", lambda v: v.reciprocal(out=ssf, in_=fsq[:, NT + i:NT + i + 1]), reads=[sC_r[i]], writes=[sC_r[i]])
                op("dve", lambda v: v.scalar_tensor_tensor(out=ot[j][:], in0=hres[:, i, :], scalar=ssf, in1=gb[1][:], op0=ALU.mult, op1=ALU.mult),
                   reads=[h_r[i], sC_r[i], gb_r[1]], writes=[ot_r[j]])
                op("sp", lambda q: q.dma_start(out=out[i * 128:(i + 1) * 128, :], in_=ot[j][:]), reads=[ot_r[j]], dma=True)

            combine(0)
            for r in range(NT + 2):
                if 0 <= r - 1 < NT:
                    pleB(r - 1)
                if r < NT:
                    pleA(r)
                if r + 1 < NT:
                    combine(r + 1)
                if 0 <= r - 1 < NT:
                    pleB2(r - 1)
                if r < NT:
                    pleA2(r)
                if 0 <= r - 2 < NT:
                    pleC(r - 2)
            kb.barrier()
        moe.close()
    return nc, dbg


_CACHE = {}


def _prep(inputs, b):
    f = lambda a: np.ascontiguousarray(np.asarray(a, dtype=np.float32))
    g = inputs
    lruvec = np.concatenate([g["conv_w"][0], g["conv_b"][0][None], g["lru_b_a"][0][None], g["lru_b_x"][0][None],
                             g["lru_lambda"][0][None], g["lru_out_g"][0][None]], axis=0)
    gains = np.stack([g["mix_norm_g"][0], g["ffn_norm_g"][0], g["ple_norm_g"][0], g["final_norm_g"]], axis=0)
    return {
        "x": f(g["x"][b]), "p": f(g["p"][0, b]), "gains": f(gains), "w_in": f(g["w_in"][0]), "lruvec": f(lruvec),
        "lru_w_a": f(g["lru_w_a"][0]), "lru_w_x": f(g["lru_w_x"][0]), "sb_out_g": f(g["sb_out_g"][0][None]),
        "w_out": f(g["w_out"][0]), "w_router": f(g["w_router"][0]), "b_router": f(g["b_router"][0][None]),
        "w_up": f(g["w_up"][0]), "b_up": f(g["b_up"][0]), "w_down": f(g["w_down"][0]), "b_down": f(g["b_down"][0]),
        "w_ple_gate": f(g["w_ple_gate"][0]), "w_ple": f(g["w_ple"][0]),
    }


def kernel(**inputs):
    inputs = {k: np.asarray(v) for k, v in inputs.items()}
    if "nc" not in _CACHE:
        _CACHE["nc"] = build("FULL")[0]
    nc = _CACHE["nc"]
    shared = _prep(inputs, 0)
    in_maps = []
    for b in range(8):
        m = dict(shared)
        m["x"] = np.ascontiguousarray(inputs["x"][b], dtype=np.float32)
        m["p"] = np.ascontiguousarray(inputs["p"][0, b], dtype=np.float32)
        in_maps.append(m)
    res = run_bass_kernel_spmd(nc, in_maps, core_ids=list(range(8)))
    return np.stack([np.asarray(r["out"], dtype=np.float32) for r in res.results], axis=0)
```
